# Optimizing a Trainium2 kernel written in Bass

```python
import jax, jax.numpy as jnp
from jax import lax
import numpy as np

D_MODEL = 1024
BATCH = 8
SEQ = 2048
DEPTH = 1

RET_HEADS = 4
RET_DK = 256
RET_DV = 256
RET_CHUNK = 128
MLA_HEADS = 8
MLA_NOPE = 128
MLA_ROPE = 64
MLA_V = 128
MLA_Q_LORA = 384
MLA_KV_LORA = 256
ATTN_BLOCK = 128
ROPE_THETA = 10000.0
N_GROUPS = 4
EXPERTS_PER_GROUP = 8
TOP_K = 2
D_EXPERT = 256
EPS = 1e-6

RET_QK_W = RET_HEADS * RET_DK
RET_V_W = RET_HEADS * RET_DV
MLA_V_W = MLA_HEADS * MLA_V
SPLITS = [RET_QK_W, RET_QK_W, RET_V_W, RET_V_W, MLA_Q_LORA, MLA_KV_LORA, MLA_ROPE, D_MODEL, D_MODEL]
IN_W = sum(SPLITS)

kernel_name = "hybrid_retention_mla_hiermoe_adaln"


def rmsnorm(x, g):
    xf = x.astype(jnp.float32)
    y = xf * lax.rsqrt(jnp.mean(xf * xf, axis=-1, keepdims=True) + EPS)
    return (y * g.astype(jnp.float32)).astype(x.dtype)


def modulate(x, shift, scale):
    return x * (1.0 + scale) + shift


def rope_tables(pos, dim):
    inv = ROPE_THETA ** (-jnp.arange(0, dim, 2, dtype=jnp.float32) / dim)
    ang = pos[:, None] * inv[None, :]
    return jnp.cos(ang), jnp.sin(ang)


def apply_rope(x, cos, sin):
    x1, x2 = jnp.split(x.astype(jnp.float32), 2, axis=-1)
    cos = cos[None, :, None, :]
    sin = sin[None, :, None, :]
    return jnp.concatenate([x1 * cos - x2 * sin, x2 * cos + x1 * sin], axis=-1).astype(x.dtype)


def retention(q, k, v):
    b, s, h, dk = q.shape
    dv = v.shape[-1]
    c = RET_CHUNK
    n = s // c
    log_gamma = jnp.log1p(-jnp.exp2(-5.0 - jnp.arange(h, dtype=jnp.float32)))
    idx = jnp.arange(c, dtype=jnp.float32)
    rel = idx[:, None] - idx[None, :]
    decay_in = jnp.where(rel[None] >= 0, jnp.exp(log_gamma[:, None, None] * jnp.maximum(rel, 0.0)[None]), 0.0)
    xi = jnp.exp(log_gamma[:, None] * (idx[None, :] + 1.0))
    zeta = jnp.exp(log_gamma[:, None] * (c - 1.0 - idx[None, :]))
    chunk_decay = jnp.exp(log_gamma * c)

    def to_chunks(t):
        return t.astype(jnp.float32).reshape(b, n, c, h, t.shape[-1]).transpose(1, 0, 2, 3, 4)

    qc = to_chunks(q)
    kc = to_chunks(k) * (dk ** -0.5)
    vc = to_chunks(v)

    def step(state, inp):
        qi, ki, vi = inp
        scores = jnp.einsum('bihd,bjhd->bhij', qi, ki) * decay_in[None]
        y = jnp.einsum('bhij,bjhe->bihe', scores, vi) + jnp.einsum('bihd,hi,bhde->bihe', qi, xi, state)
        state = state * chunk_decay[None, :, None, None] + jnp.einsum('bjhd,hj,bjhe->bhde', ki, zeta, vi)
        return state, y

    state0 = jnp.zeros((b, h, dk, dv), jnp.float32)
    _, ys = lax.scan(step, state0, (qc, kc, vc))
    return ys.transpose(1, 0, 2, 3, 4).reshape(b, s, h, dv)


def head_norm(y):
    mu = jnp.mean(y, axis=-1, keepdims=True)
    var = jnp.mean(jnp.square(y - mu), axis=-1, keepdims=True)
    return (y - mu) * lax.rsqrt(var + EPS)


def mla(c_q, c_kv, k_pe, q_norm, kv_norm, w_uq, w_ukv, cos, sin):
    b, s, _ = c_q.shape
    q = (rmsnorm(c_q, q_norm) @ w_uq).reshape(b, s, MLA_HEADS, MLA_NOPE + MLA_ROPE)
    q_nope, q_pe = q[..., :MLA_NOPE], q[..., MLA_NOPE:]
    q_pe = apply_rope(q_pe, cos, sin)
    kv = (rmsnorm(c_kv, kv_norm) @ w_ukv).reshape(b, s, MLA_HEADS, MLA_NOPE + MLA_V)
    k_nope, v = kv[..., :MLA_NOPE], kv[..., MLA_NOPE:]
    k_pe = apply_rope(k_pe[:, :, None, :], cos, sin)[:, :, 0, :]
    scale = (MLA_NOPE + MLA_ROPE) ** -0.5
    outs = []
    for blk in range(s // ATTN_BLOCK):
        lo = blk * ATTN_BLOCK
        hi = lo + ATTN_BLOCK
        scores = (jnp.einsum('bqhd,bkhd->bhqk', q_nope[:, lo:hi], k_nope[:, :hi])
                  + jnp.einsum('bqhr,bkr->bhqk', q_pe[:, lo:hi], k_pe[:, :hi])).astype(jnp.float32) * scale
        mask = (lo + jnp.arange(ATTN_BLOCK))[:, None] >= jnp.arange(hi)[None, :]
        p = jax.nn.softmax(jnp.where(mask, scores, -jnp.inf), axis=-1).astype(v.dtype)
        outs.append(jnp.einsum('bhqk,bkhd->bqhd', p, v[:, :hi]))
    return jnp.concatenate(outs, axis=1).reshape(b, s, MLA_V_W)


def token_mixer(u, w_in, w_ret_o, q_norm, kv_norm, w_uq, w_ukv, w_mla_o, w_out, ret_rope, mla_rope):
    b, s, _ = u.shape
    proj = u @ w_in
    offs = np.cumsum(SPLITS)[:-1].tolist()
    rq, rk, rv, rg, c_q, c_kv, k_pe, gate_ret, gate_mla = jnp.split(proj, offs, axis=-1)
    rq = apply_rope(rq.reshape(b, s, RET_HEADS, RET_DK), *ret_rope)
    rk = apply_rope(rk.reshape(b, s, RET_HEADS, RET_DK), *ret_rope)
    rv = rv.reshape(b, s, RET_HEADS, RET_DV)
    y_ret = head_norm(retention(rq, rk, rv)).reshape(b, s, RET_V_W).astype(u.dtype)
    y_ret = (jax.nn.silu(rg) * y_ret) @ w_ret_o
    y_mla = mla(c_q, c_kv, k_pe, q_norm, kv_norm, w_uq, w_ukv, *mla_rope) @ w_mla_o
    merged = jax.nn.sigmoid(gate_ret) * y_ret + jax.nn.sigmoid(gate_mla) * y_mla
    return merged @ w_out


def hier_moe(u, w_grp, b_grp, w_exp, b_exp, w1, w3, w2):
    b, s, d = u.shape
    t = u.reshape(-1, d)
    n_tok = t.shape[0]
    grp_logits = (t @ w_grp).astype(jnp.float32) + b_grp.astype(jnp.float32)
    grp_prob = jax.nn.softmax(grp_logits, axis=-1)
    _, g_sel = lax.top_k(grp_logits, 1)
    p_grp = jnp.take_along_axis(grp_prob, g_sel, axis=-1)
    exp_logits = ((t @ w_exp).astype(jnp.float32) + b_exp.astype(jnp.float32)).reshape(n_tok, N_GROUPS, EXPERTS_PER_GROUP)
    in_grp = jnp.take_along_axis(exp_logits, g_sel[:, :, None], axis=1)[:, 0]
    top_val, top_idx = lax.top_k(in_grp, TOP_K)
    top_w = jax.nn.softmax(top_val, axis=-1) * p_grp
    exp_w = jnp.sum(jax.nn.one_hot(top_idx, EXPERTS_PER_GROUP, dtype=jnp.float32) * top_w[..., None], axis=1)
    gate = jax.nn.one_hot(g_sel[:, 0], N_GROUPS, dtype=jnp.float32)[:, :, None] * exp_w[:, None, :]
    out = jnp.zeros_like(t)
    for gi in range(N_GROUPS):
        hid = jax.nn.silu(jnp.einsum('td,edf->tef', t, w1[gi])) * jnp.einsum('td,edf->tef', t, w3[gi])
        hid = hid * gate[:, gi, :, None].astype(hid.dtype)
        out = out + jnp.einsum('tef,efd->td', hid, w2[gi])
    return out.reshape(b, s, d)


def setup_inputs(seed: int = 0) -> dict:
    key = jax.random.key(seed)
    ks = jax.random.split(key, 24)
    L = DEPTH
    G, E, F = N_GROUPS, EXPERTS_PER_GROUP, D_EXPERT

    def nrm(k, shape, fan_in, scale=1.0):
        return jax.random.normal(k, shape, jnp.float32) * (scale * fan_in ** -0.5)

    def gain(k, shape):
        return 1.0 + 0.02 * jax.random.normal(k, shape, jnp.float32)

    return {
        "x": jax.random.normal(ks[0], (BATCH, SEQ, D_MODEL), jnp.float32),
        "c": jax.random.normal(ks[1], (BATCH, D_MODEL), jnp.float32),
        "w_ada": nrm(ks[2], (L, D_MODEL, 6 * D_MODEL), D_MODEL, 0.5),
        "b_ada": 0.02 * jax.random.normal(ks[3], (L, 6 * D_MODEL), jnp.float32),
        "norm1": gain(ks[4], (L, D_MODEL)),
        "norm2": gain(ks[5], (L, D_MODEL)),
        "w_in": nrm(ks[6], (L, D_MODEL, IN_W), D_MODEL),
        "w_ret_o": nrm(ks[7], (L, RET_V_W, D_MODEL), RET_V_W),
        "q_norm": gain(ks[8], (L, MLA_Q_LORA)),
        "kv_norm": gain(ks[9], (L, MLA_KV_LORA)),
        "w_uq": nrm(ks[10], (L, MLA_Q_LORA, MLA_HEADS * (MLA_NOPE + MLA_ROPE)), MLA_Q_LORA),
        "w_ukv": nrm(ks[11], (L, MLA_KV_LORA, MLA_HEADS * (MLA_NOPE + MLA_V)), MLA_KV_LORA),
        "w_mla_o": nrm(ks[12], (L, MLA_V_W, D_MODEL), MLA_V_W),
        "w_out": nrm(ks[13], (L, D_MODEL, D_MODEL), D_MODEL),
        "w_grp": nrm(ks[14], (L, D_MODEL, G), D_MODEL),
        "b_grp": 0.01 * jax.random.normal(ks[15], (L, G), jnp.float32),
        "w_exp": nrm(ks[16], (L, D_MODEL, G * E), D_MODEL),
        "b_exp": 0.01 * jax.random.normal(ks[17], (L, G * E), jnp.float32),
        "w1": nrm(ks[18], (L, G, E, D_MODEL, F), D_MODEL),
        "w3": nrm(ks[19], (L, G, E, D_MODEL, F), D_MODEL),
        "w2": nrm(ks[20], (L, G, E, F, D_MODEL), F),
        "final_norm": gain(ks[21], (D_MODEL,)),
    }


def reference(x, c, w_ada, b_ada, norm1, norm2, w_in, w_ret_o, q_norm, kv_norm, w_uq, w_ukv, w_mla_o, w_out,
              w_grp, b_grp, w_exp, b_exp, w1, w3, w2, final_norm):
    s = x.shape[1]
    pos = jnp.arange(s, dtype=jnp.float32)
    ret_rope = rope_tables(pos, RET_DK)
    mla_rope = rope_tables(pos, MLA_ROPE)
    c_act = jax.nn.silu(c)
    h = x
    for l in range(DEPTH):
        mod = (c_act @ w_ada[l] + b_ada[l])[:, None, :]
        sh1, sc1, g1, sh2, sc2, g2 = jnp.split(mod, 6, axis=-1)
        u = modulate(rmsnorm(h, norm1[l]), sh1, sc1)
        h = h + g1 * token_mixer(u, w_in[l], w_ret_o[l], q_norm[l], kv_norm[l], w_uq[l], w_ukv[l],
                                 w_mla_o[l], w_out[l], ret_rope, mla_rope)
        u = modulate(rmsnorm(h, norm2[l]), sh2, sc2)
        h = h + g2 * hier_moe(u, w_grp[l], b_grp[l], w_exp[l], b_exp[l], w1[l], w3[l], w2[l])
    return rmsnorm(h, final_norm)
```

```python
import contextlib
import os
import numpy as np
import concourse.bass as bass
import concourse.mybir as mybir
from concourse.bass_utils import run_bass_kernel_spmd

F32 = mybir.dt.float32
BF16 = mybir.dt.bfloat16
AF = mybir.ActivationFunctionType
ALU = mybir.AluOpType

D = 1024
S = 2048
NT = 16
NCORES = 8
EPS = 1e-6
RET_H = 4
MLA_H = 8
IN_W = 6848
N_EXP = 32

ENGS = ("pe", "act", "dve", "pool", "sp")
SAME_ENGINE_SYNC = True


def _conflict(k1, k2):
    n = min(len(k1), len(k2))
    return k1[:n] == k2[:n]


class Sched:
    def __init__(self, nc, es, ndma=12):
        self.nc = nc
        self.sem = {}
        for e in ENGS:
            self.sem[("c", e)] = es.enter_context(nc.semaphore(f"c_{e}"))
        self.cnt = {e: 0 for e in ENGS}
        self.ndma = ndma
        self.dcnt = {}
        self.drr = {}
        for q in ("sp", "pool", "act"):
            self.dcnt[q] = [0] * ndma
            self.drr[q] = 0
            for i in range(ndma):
                self.sem[("d", q, i)] = es.enter_context(nc.semaphore(f"d_{q}{i}"))
        self.seen = {e: {} for e in ENGS}
        self.prog = {e: [] for e in ENGS}
        self.lastw = {}
        self.readers = {}
        self.out_tokens = []
        self.nins = 0

    def _collect(self, eng, reads, writes):
        toks = set()
        for k in reads:
            for (k2, tok) in self.lastw.get(k[0], ()):
                if _conflict(k, k2):
                    toks.add(tok)
        for k in writes:
            for (k2, tok) in self.lastw.get(k[0], ()):
                if _conflict(k, k2):
                    toks.add(tok)
            for (k2, tok) in self.readers.get(k[0], ()):
                if _conflict(k, k2):
                    toks.add(tok)
        waits = []
        for (s, v) in sorted(toks, key=lambda t: (str(t[0]), t[1])):
            if s == ("c", eng) and (eng == "pe" or eng == "sp" or not SAME_ENGINE_SYNC):
                continue
            if self.seen[eng].get(s, 0) >= v:
                continue
            waits.append((s, v))
        best = {}
        for (s, v) in waits:
            best[s] = max(best.get(s, 0), v)
        for s, v in best.items():
            self.seen[eng][s] = v
        return list(best.items())

    def _commit(self, tok, reads, writes):
        for k in writes:
            lw = self.lastw.setdefault(k[0], [])
            lw[:] = [(k2, t) for (k2, t) in lw if not (len(k2) >= len(k) and k2[:len(k)] == k)]
            lw.append((k, tok))
            rd = self.readers.setdefault(k[0], [])
            rd[:] = [(k2, t) for (k2, t) in rd if not (len(k2) >= len(k) and k2[:len(k)] == k)]
        for k in reads:
            rd = self.readers.setdefault(k[0], [])
            rd[:] = [(k2, t) for (k2, t) in rd if not (k2 == k and t[0] == tok[0])]
            rd.append((k, tok))

    def op(self, eng, fn, reads=(), writes=()):
        reads = [tuple(k) if isinstance(k, (tuple, list)) else (k,) for k in reads]
        writes = [tuple(k) if isinstance(k, (tuple, list)) else (k,) for k in writes]
        reads = [k[:2] if k[0] == "ps" else k for k in reads]
        writes = [k[:2] if k[0] == "ps" else k for k in writes]
        waits = self._collect(eng, reads, writes)
        self.cnt[eng] += 1
        tok = (("c", eng), self.cnt[eng])
        self._commit(tok, reads, writes)
        self.prog[eng].append((waits, fn, ("c", eng), 1))
        self.nins += 1
        return tok

    def dma(self, q, out, in_, reads=(), writes=(), is_output=False):
        reads = [tuple(k) if isinstance(k, (tuple, list)) else (k,) for k in reads]
        writes = [tuple(k) if isinstance(k, (tuple, list)) else (k,) for k in writes]
        waits = self._collect(q, reads, writes)
        i = self.drr[q]
        self.drr[q] = (i + 1) % self.ndma
        s = ("d", q, i)
        if self.dcnt[q][i] > 0:
            v = 16 * self.dcnt[q][i]
            if self.seen[q].get(s, 0) < v:
                self.seen[q][s] = v
                waits = [w for w in waits if w[0] != s] + [(s, v)]
        self.dcnt[q][i] += 1
        tok = (s, 16 * self.dcnt[q][i])
        self._commit(tok, reads, writes)

        def fn(e, out=out, in_=in_):
            return e.dma_start(out=out, in_=in_)
        self.prog[q].append((waits, fn, s, 16))
        if is_output:
            self.out_tokens.append(tok)
        self.nins += 1
        return tok

    def alias(self, new, old):
        toks = set(t for (_, t) in self.lastw.get(old, ())) | set(t for (_, t) in self.readers.get(old, ()))
        lw = self.lastw.setdefault(new, [])
        for t in sorted(toks, key=lambda t: (str(t[0]), t[1])):
            lw.append(((new,), t))

    def barrier(self):
        snap = dict(self.cnt)
        for e in ENGS:
            waits = []
            for f in ENGS:
                if f == e or snap[f] == 0:
                    continue
                s = ("c", f)
                if self.seen[e].get(s, 0) < snap[f]:
                    self.seen[e][s] = snap[f]
                    waits.append((s, snap[f]))
            if waits:
                self.prog[e].append((waits, None, None, 0))

    def finish(self):
        waits = {}
        for (s, v) in self.out_tokens:
            waits[s] = max(waits.get(s, 0), v)
        for q in ("sp", "pool", "act"):
            for i in range(self.ndma):
                if self.dcnt[q][i] > 0:
                    waits[("d", q, i)] = 16 * self.dcnt[q][i]
        for e in ENGS:
            if self.cnt[e] > 0:
                waits[("c", e)] = self.cnt[e]
        self.prog["sp"].append((list(waits.items()), None, None, 0))

    def emit(self, block):
        nc = self.nc
        sem = self.sem

        def replay(eng_name):
            def run(e):
                for (waits, fn, s, inc) in self.prog[eng_name]:
                    for (ws, wv) in waits:
                        e.wait_ge(sem[ws], wv)
                    if fn is not None:
                        ins = fn(e)
                        ins.then_inc(sem[s], inc)
            return run

        block.tensor(replay("pe"))
        block.scalar(replay("act"))
        block.vector(replay("dve"))
        block.gpsimd(replay("pool"))
        block.sync(replay("sp"))


class Arena:
    def __init__(self, base_ap, nbytes, sched):
        self.base = base_ap
        self.nbytes = nbytes
        self.off = 0
        self.peak = 0
        self.sched = sched
        self.history = []

    def mark(self):
        return self.off

    def release(self, m):
        self.off = m

    def alloc(self, name, shape, dtype):
        n = int(np.prod(shape))
        esz = 4 if dtype == F32 else 2
        nb = (n * esz + 31) // 32 * 32
        assert self.off + nb <= self.nbytes, f"arena overflow {self.off}+{nb}>{self.nbytes}"
        lo, hi = self.off, self.off + nb
        olds = set(nm for (l, h, nm) in self.history if l < hi and lo < h and nm != name)
        for o in sorted(olds):
            self.sched.alias(name, o)
        self.history.append((lo, hi, name))
        self.offs = getattr(self, "offs", {})
        self.offs.setdefault(name, lo)
        a = self.base[:, self.off // 4:(self.off + nb) // 4]
        self.off += nb
        self.peak = max(self.peak, self.off)
        if dtype != F32:
            a = a.bitcast(dtype)
        a = a[:, 0:n]
        if len(shape) == 2:
            a = a.rearrange("p (a b) -> p a b", a=shape[0])
        elif len(shape) == 3:
            a = a.rearrange("p (a b c) -> p a b c", a=shape[0], b=shape[1])
        return a


def build_program(dbg=()):
    nc = bass.Bass("TRN2", target_bir_lowering=False)
    dbg = set(dbg)

    def din(name, shape, dt=F32):
        return nc.dram_tensor(name, list(shape), dt, kind="ExternalInput").ap()

    x = din("x", [S, D])
    rows_d = din("rows", [77, 128])
    b_ada = din("b_ada", [1, 6 * D])
    w_ada = din("w_ada", [D, 6 * D])
    w_in = din("w_in", [D, IN_W])
    w_ret_o = din("w_ret_o", [D, D])
    w_uq = din("w_uq", [384, 1536])
    w_ukv = din("w_ukv", [256, 2048])
    w_mla_o = din("w_mla_o", [D, D])
    w_out = din("w_out", [D, D])
    w_grp = din("w_grp", [D, 4])
    w_exp = din("w_exp", [D, 32])
    b_rt = din("b_rt", [1, 36])
    w1 = din("w1", [N_EXP, D, 256])
    w3 = din("w3", [N_EXP, D, 256])
    w2 = din("w2", [N_EXP, 256, D])
    fnorm = din("final_norm", [1, D])
    c_ident = din("c_ident", [128, 128])
    c_mask = din("c_mask", [128, 128])
    c_rcos = din("c_rcos", [128, S])
    c_rsin = din("c_rsin", [128, S])
    c_mcos64 = din("c_mcos64", [64, S])
    c_msin64 = din("c_msin64", [64, S])
    c_mcos_tok = din("c_mcos_tok", [128, NT, 32])
    c_msin_tok = din("c_msin_tok", [128, NT, 32])
    c_ret = din("c_ret", [128, 8])
    out_d = nc.dram_tensor("out", [S, D], F32, kind="ExternalOutput").ap()
    dbg_outs = {}

    log_gamma = [float(np.log1p(-np.exp2(-5.0 - h))) for h in range(RET_H)]
    gC = [float(np.exp(lg * 128.0)) for lg in log_gamma]

    with contextlib.ExitStack() as es:
        ARENA_BYTES = 204 * 1024
        arena_t = es.enter_context(nc.sbuf_tensor("arena", [128, ARENA_BYTES // 4], F32))
        psum_t = es.enter_context(nc.psum_tensor("psum", [128, 8, 512], F32))
        Sc = Sched(nc, es)
        A = Arena(arena_t[:, :], ARENA_BYTES, Sc)

        def bank(b):
            return psum_t[:, b, :]

        def bank_bf(b):
            return psum_t[:, b, :].bitcast(BF16)

        def PS(b):
            return ("ps", b)

        def dump(name, ap, shape, dt, key):
            if name not in dbg:
                return
            d = nc.dram_tensor("dbg_" + name, list(shape), dt, kind="ExternalOutput").ap()
            dbg_outs[name] = d
            Sc.dma("sp", d, ap, reads=[key], writes=[("dbgout", name)], is_output=True)

        def mm_group(out, pairs, reads, writes):
            def fn(e, out=out, pairs=list(pairs)):
                n = len(pairs)
                ins = None
                for i, (l, r) in enumerate(pairs):
                    ins = e.matmul(out=out, lhsT=l, rhs=r, start=(i == 0), stop=(i == n - 1))
                return ins
            return Sc.op("pe", fn, reads=reads, writes=writes)

        def transposes(items, reads, writes, ident):
            def fn(e, items=list(items), ident=ident):
                ins = None
                for (o, i) in items:
                    ins = e.transpose(out=o, in_=i, identity=ident)
                return ins
            return Sc.op("pe", fn, reads=reads, writes=writes)

        ident_bf = A.alloc("ident_bf", (128,), BF16)
        ident_f = A.alloc("ident_f", (128,), F32)
        mask01 = A.alloc("mask01", (128,), BF16)
        cols = A.alloc("cols", (80,), F32)
        modc = A.alloc("modc", (48,), F32)
        A1 = A.alloc("A1", (8,), F32)
        A2 = A.alloc("A2", (8,), F32)
        cact = A.alloc("cact", (8,), F32)
        cret = A.alloc("cret", (8,), F32)
        stat = A.alloc("stat", (64,), F32)
        g1b = A.alloc("g1b", (D,), F32)
        g2b = A.alloc("g2b", (D,), F32)
        uT = A.alloc("uT", (8, S), BF16)
        R_G = A.alloc("gatedT", (8, S), BF16)
        R_A = A.alloc("attnT", (8, S), BF16)
        gatedT = R_G
        attnT = R_A
        TMP0 = A.mark()

        Sc.dma("pool", ident_bf, c_ident, writes=[("ident_bf",)])
        Sc.dma("sp", ident_f, c_ident, writes=[("ident_f",)])
        Sc.dma("pool", mask01, c_mask, writes=[("mask01",)])
        Sc.dma("sp", cret, c_ret, writes=[("cret",)])

        m0 = A.mark()
        rows_in = A.alloc("rows_in", (128,), F32)
        cact_rep = A.alloc("cact_rep", (8, 128), F32)
        wa = [A.alloc("wa", (8, 512), F32) for _ in range(2)]
        xt = [A.alloc("xt", (D,), F32) for _ in range(2)]
        xn = [A.alloc("xn", (D,), BF16) for _ in range(2)]
        junk = A.alloc("junk", (D,), BF16)

        Sc.dma("sp", rows_in[0:77, :], rows_d, writes=[("rows_in",)])
        Sc.dma("sp", g1b, b_ada[0, 2 * D:3 * D].partition_broadcast(128), writes=[("g1b",)])
        Sc.dma("sp", g2b, b_ada[0, 5 * D:6 * D].partition_broadcast(128), writes=[("g2b",)])

        transposes([(bank(2)[:, 0:77], rows_in[0:77, :])], reads=[("rows_in",), ("ident_f",)], writes=[PS(2)],
                   ident=ident_f[0:77, 0:77])
        Sc.op("dve", lambda e: e.tensor_copy(out=cols[:, 0:77], in_=bank(2)[:, 0:77]), reads=[PS(2)], writes=[("cols",)])
        Sc.op("act", lambda e: e.activation(out=cact, in_=cols[:, 69:77], func=AF.Silu), reads=[("cols",)], writes=[("cact",)])
        for c in range(8):
            Sc.op("dve", lambda e, c=c: e.tensor_copy(out=cact_rep[:, c, :], in_=cact[:, c:c + 1].to_broadcast([128, 128])),
                  reads=[("cact",)], writes=[("cact_rep", c)])

        wa_view = w_ada.rearrange("(c p) n -> p c n", p=128)
        blk_order = [0, 1, 2, 3, 6, 7, 8, 9, 4, 5, 10, 11]
        for bi, blk in enumerate(blk_order):
            wb = wa[bi % 2]
            Sc.dma("sp", wb, wa_view[:, :, blk * 512:(blk + 1) * 512], writes=[("wa", bi % 2)])
            if blk in (4, 5, 10, 11):
                gb = g1b if blk in (4, 5) else g2b
                gk = ("g1b",) if blk in (4, 5) else ("g2b",)
                half = blk % 2 if blk in (4, 5) else (blk - 10)
                pb = 3 + (bi % 2)
                mm_group(bank(pb), [(cact_rep[:, c, :], wb[:, c, :]) for c in range(8)],
                         reads=[("cact_rep",), ("wa", bi % 2)], writes=[PS(pb)])
                Sc.op("dve", lambda e, gb=gb, half=half, pb=pb: e.tensor_tensor(
                    out=gb[:, half * 512:(half + 1) * 512], in0=bank(pb), in1=gb[:, half * 512:(half + 1) * 512], op=ALU.add),
                    reads=[PS(pb), gk], writes=[gk])
            else:
                for j in range(4):
                    J = blk * 4 + j
                    mm_group(bank(2)[:, 128 + J:129 + J], [(wb[:, c, j * 128:(j + 1) * 128], cact[:, c:c + 1]) for c in range(8)],
                             reads=[("cact",), ("wa", bi % 2)], writes=[("ps", 2, "col", J)])
            if bi == 3:
                Sc.op("dve", lambda e: e.tensor_tensor(out=modc[:, 0:16], in0=bank(2)[:, 128:144], in1=cols[:, 0:16], op=ALU.add),
                      reads=[("ps", 2, "col"), ("cols",)], writes=[("modc", 0)])
                Sc.op("dve", lambda e: e.scalar_tensor_tensor(out=A1, in0=modc[:, 8:16], scalar=1.0, in1=cols[:, 48:56],
                                                               op0=ALU.add, op1=ALU.mult),
                      reads=[("modc", 0), ("cols",)], writes=[("A1",)])
            if bi == 7:
                Sc.op("dve", lambda e: e.tensor_tensor(out=modc[:, 24:40], in0=bank(2)[:, 152:168], in1=cols[:, 24:40], op=ALU.add),
                      reads=[("ps", 2, "col"), ("cols",)], writes=[("modc", 1)])
                Sc.op("dve", lambda e: e.scalar_tensor_tensor(out=A2, in0=modc[:, 32:40], scalar=1.0, in1=cols[:, 56:64],
                                                               op0=ALU.add, op1=ALU.mult),
                      reads=[("modc", 1), ("cols",)], writes=[("A2",)])
        dump("modc", modc, [128, 48], F32, ("modc",))
        dump("g1b", g1b, [128, D], F32, ("g1b",))

        def norm_A(t, src_ap, src_key):
            b2 = t % 2
            junk_ = junk
            xn_ = xn[b2]
            Sc.op("act", lambda e: e.activation(out=junk_, in_=src_ap, func=AF.Square, accum_out=stat[:, t:t + 1]),
                  reads=[src_key], writes=[("junk",), ("stat", t)])
            Sc.op("act", lambda e: e.activation(out=stat[:, 16 + t:17 + t], in_=stat[:, t:t + 1], func=AF.Sqrt,
                                                scale=1.0 / D, bias=EPS),
                  reads=[("stat", t)], writes=[("stat", 16 + t)])
            Sc.op("dve", lambda e: e.reciprocal(out=stat[:, 32 + t:33 + t], in_=stat[:, 16 + t:17 + t]),
                  reads=[("stat", 16 + t)], writes=[("stat", 32 + t)])
            Sc.op("act", lambda e: e.activation(out=xn_, in_=src_ap, func=AF.Copy, scale=stat[:, 32 + t:33 + t]),
                  reads=[src_key, ("stat", 32 + t)], writes=[("xn", b2)])

        def norm_B(t, Acol, shcol, Akey, shkey, dstT, dst_name, pb):
            b2 = t % 2
            xn_ = xn[b2]
            pT = bank_bf(pb).rearrange("p (a b) -> p a b", a=8)
            transposes([(pT[:, c, :], xn_[:, c * 128:(c + 1) * 128]) for c in range(8)],
                       reads=[("xn", b2), ("ident_bf",)], writes=[PS(pb)], ident=ident_bf)
            for c in range(8):
                Sc.op("dve", lambda e, c=c: e.tensor_scalar(out=dstT[:, c, t * 128:(t + 1) * 128], in0=pT[:, c, :],
                                                            scalar1=Acol[:, c:c + 1], scalar2=shcol[:, c:c + 1],
                                                            op0=ALU.mult, op1=ALU.add),
                      reads=[PS(pb), Akey, shkey], writes=[(dst_name, c, t)])

        Sc.dma("sp", xt[0], x[0:128, :], writes=[("xt", 0)])
        norm_A(0, xt[0], ("xt", 0))
        for t in range(NT):
            if t + 1 < NT:
                Sc.dma("sp", xt[(t + 1) % 2], x[(t + 1) * 128:(t + 2) * 128, :], writes=[("xt", (t + 1) % 2)])
                norm_A(t + 1, xt[(t + 1) % 2], ("xt", (t + 1) % 2))
            norm_B(t, A1, modc[:, 0:8], ("A1",), ("modc", 0), uT, "uT", pb=t % 2)
        dump("uT", uT, [128, 8, S], BF16, ("uT",))
        A.release(m0)


        m1 = A.mark()
        rcos = A.alloc("rcos", (S,), BF16)
        rsin = A.alloc("rsin", (S,), BF16)
        Sc.dma("pool", rcos, c_rcos, writes=[("rcos",)])
        Sc.dma("pool", rsin, c_rsin, writes=[("rsin",)])
        NW = 8
        wslot = [A.alloc("wslot", (8, 256), BF16) for _ in range(NW)]
        qT = A.alloc("qT", (2, S), BF16)
        kT = A.alloc("kT", (2, S), BF16)
        dummy_ = A.alloc("dummy_", (512,), F32) if os.environ.get("MK_DUMMY") else None
        v_s = A.alloc("v_s", (NT, 256), BF16)
        sgT = A.alloc("sgT", (2, S), BF16)
        Tst = A.alloc("Tst", (512,), F32)
        st_bf = [A.alloc("st_bf", (2, 256), BF16) for _ in range(2)]
        sm = [A.alloc("sm", (128,), BF16) for _ in range(2)]
        ktok = [A.alloc("ktok", (256,), BF16) for _ in range(2)]
        yn = [A.alloc("yn", (256,), BF16) for _ in range(2)]
        rt = [A.alloc("rt", (512,), F32) for _ in range(4)]
        st6 = [A.alloc("st6", (8,), F32) for _ in range(2)]
        mv = [A.alloc("mv", (8,), F32) for _ in range(2)]
        mhalf = A.alloc("mhalf", (8,), F32)
        Sc.op("pool", lambda e: e.memset(mhalf, -0.5), reads=[], writes=[("mhalf",)])
        w_in_v = w_in.rearrange("(c p) n -> p c n", p=128)
        wstate = {"n": 0}

        def load_w(col0):
            slot = wstate["n"] % NW
            wstate["n"] += 1
            Sc.dma("pool", wslot[slot], w_in_v[:, :, col0:col0 + 256], writes=[("wslot", slot)])
            return slot

        def head_slots(h):
            return [load_w(m * 1024 + h * 256) for m in range(4)]

        def proj_units(h, tg, slots):
            sq, sk, sv, sg = slots
            tsl = slice(tg * 512, (tg + 1) * 512)
            units = []

            def qk_mm(nm, sl, j):
                def f():
                    mm_group(bank(j), [(wslot[sl][:, c, j * 128:(j + 1) * 128], uT[:, c, tsl]) for c in range(8)],
                             reads=[("wslot", sl), ("uT",)], writes=[PS(j)])
                return f

            def qk_rope(nm, dst):
                def f():
                    P0, P1 = bank(0), bank(1)
                    Sc.op("dve", lambda e: e.tensor_tensor(out=rt[0], in0=P0, in1=rcos[:, tsl], op=ALU.mult),
                          reads=[PS(0), ("rcos",)], writes=[("rt", 0)])
                    Sc.op("dve", lambda e: e.tensor_tensor(out=rt[1], in0=P1, in1=rsin[:, tsl], op=ALU.mult),
                          reads=[PS(1), ("rsin",)], writes=[("rt", 1)])
                    Sc.op("dve", lambda e: e.tensor_tensor(out=rt[2], in0=P1, in1=rcos[:, tsl], op=ALU.mult),
                          reads=[PS(1), ("rcos",)], writes=[("rt", 2)])
                    Sc.op("dve", lambda e: e.tensor_tensor(out=rt[3], in0=P0, in1=rsin[:, tsl], op=ALU.mult),
                          reads=[PS(0), ("rsin",)], writes=[("rt", 3)])
                    Sc.op("pool", lambda e: e.tensor_tensor(out=dst[:, 0, tsl], in0=rt[0], in1=rt[1], op=ALU.subtract),
                          reads=[("rt", 0), ("rt", 1)], writes=[(nm + "T", 0, tg)])
                    Sc.op("pool", lambda e: e.tensor_tensor(out=dst[:, 1, tsl], in0=rt[2], in1=rt[3], op=ALU.add),
                          reads=[("rt", 2), ("rt", 3)], writes=[(nm + "T", 1, tg)])
                return f

            def qk_units(nm, sl, dst):
                m1_ = qk_mm(nm, sl, 1)
                r_ = qk_rope(nm, dst)
                return [qk_mm(nm, sl, 0), (lambda: (m1_(), r_()))]

            def g_unit(j):
                def f():
                    mm_group(bank(j), [(wslot[sg][:, c, j * 128:(j + 1) * 128], uT[:, c, tsl]) for c in range(8)],
                             reads=[("wslot", sg), ("uT",)], writes=[PS(j)])
                    Sc.op("act", lambda e: e.activation(out=sgT[:, j, tsl], in_=bank(j), func=AF.Silu),
                          reads=[PS(j)], writes=[("sgT", j, tg)])
                return f

            def v_unit(t):
                def f():
                    vb_ = 2 if t % 2 == 0 else 5
                    rg_ = bank(vb_)[:, 0:256]
                    mm_group(rg_, [(uT[:, c, t * 128:(t + 1) * 128], wslot[sv][:, c, :]) for c in range(8)],
                             reads=[("wslot", sv), ("uT",)], writes=[PS(vb_)])
                    Sc.op("act", lambda e: e.activation(out=v_s[:, t, :], in_=rg_, func=AF.Copy, scale=cret[:, h:h + 1]),
                          reads=[PS(vb_), ("cret",)], writes=[("v_s", t)])
                return f

            g0_, g1_ = g_unit(0), g_unit(1)
            qu_ = qk_units("q", sq, qT)
            ku_ = qk_units("k", sk, kT)
            units += [qu_[0], v_unit(4 * tg), qu_[1], v_unit(4 * tg + 1), ku_[0], v_unit(4 * tg + 2), ku_[1], v_unit(4 * tg + 3)]
            units.append(lambda: (g0_(), g1_()))
            return units

        def ret_tail(h, n):
            p = n % 2
            tg = n // 4
            nsl = slice(n * 128, (n + 1) * 128)
            yT = bank_bf(7)[:, p * 256:(p + 1) * 256].rearrange("p (c t) -> p c t", c=2)
            transposes([(yT[:, c, :], yn[p][:, c * 128:(c + 1) * 128]) for c in range(2)],
                       reads=[("yn", p), ("ident_bf",)], writes=[("ps", 7, p)], ident=ident_bf)
            Sc.op("dve", lambda e: e.tensor_tensor(out=gatedT[:, 2 * h:2 * h + 2, nsl], in0=yT, in1=sgT[:, :, nsl], op=ALU.mult),
                  reads=[("ps", 7, p), ("sgT", 0, tg), ("sgT", 1, tg)], writes=[("gatedT", h, n)])

        def recur_units(h, n):
            p = n % 2
            tg = n // 4
            nsl = slice(n * 128, (n + 1) * 128)
            qk_keys = [("kT", 0, tg), ("kT", 1, tg), ("qT", 0, tg), ("qT", 1, tg)]
            scb = bank(4)[:, p * 128:(p + 1) * 128]
            ybk = 3
            yb = bank(ybk)[:, 0:256]

            def ua():
                mm_group(scb, [(kT[:, c, nsl], qT[:, c, nsl]) for c in range(2)], reads=qk_keys, writes=[("ps", 4, "s", p)])
                Sc.op("dve", lambda e: e.tensor_tensor(out=sm[p], in0=scb, in1=mask01, op=ALU.mult),
                      reads=[("ps", 4, "s", p), ("mask01",)], writes=[("sm", p)])
                if n < NT - 1:
                    kt = bank_bf(6)[:, 0:256]
                    transposes([(kt[:, c * 128:(c + 1) * 128], kT[:, c, nsl]) for c in range(2)],
                               reads=qk_keys[0:2] + [("ident_bf",)], writes=[PS(6)], ident=ident_bf)
                    Sc.op("act", lambda e: e.activation(out=ktok[p], in_=kt, func=AF.Copy),
                          reads=[PS(6)], writes=[("ktok", p)])

            def ukv():
                if n < NT - 1:
                    for c in range(2):
                        mm_group(bank(6)[:, c * 256:(c + 1) * 256], [(ktok[p][:, c * 128:(c + 1) * 128], v_s[:, n, :])],
                                 reads=[("ktok", p), ("v_s", n)], writes=[("ps", 6, c)])
                    if n == 0:
                        Sc.op("dve", lambda e: e.tensor_copy(out=Tst, in_=bank(6)), reads=[("ps", 6)], writes=[("Tst",)])
                    else:
                        Sc.op("dve", lambda e: e.scalar_tensor_tensor(out=Tst, in0=Tst, scalar=gC[h], in1=bank(6),
                                                                       op0=ALU.mult, op1=ALU.add),
                              reads=[("ps", 6), ("Tst",)], writes=[("Tst",)])
                    Sc.op("act", lambda e: e.activation(out=st_bf[p].rearrange("p a b -> p (a b)"), in_=Tst, func=AF.Copy, scale=gC[h]),
                          reads=[("Tst",)], writes=[("st_bf", p)])

            def uy():
                pairs = [(sm[p], v_s[:, n, :])]
                rds = [("sm", p), ("v_s", n)]
                if n > 0:
                    pairs += [(qT[:, c, nsl], st_bf[1 - p][:, c, :]) for c in range(2)]
                    rds += qk_keys[2:4] + [("st_bf", 1 - p)]
                mm_group(yb, pairs, reads=rds, writes=[PS(ybk)])

            def uc():
                Sc.op("dve", lambda e: e.bn_stats(out=st6[p][:, 0:6], in_=yb), reads=[PS(ybk)], writes=[("st6", p)])
                Sc.op("dve", lambda e: e.bn_aggr(out=mv[p][:, 0:2], in_=st6[p][:, 0:6]), reads=[("st6", p)], writes=[("mv", p, 0)])
                Sc.op("dve", lambda e: e.tensor_scalar(out=mv[p][:, 2:3], in0=mv[p][:, 1:2], scalar1=cret[:, 4 + h:5 + h], scalar2=None,
                                                       op0=ALU.add),
                      reads=[("mv", p, 0), ("cret",)], writes=[("mv", p, 2)])
                Sc.op("pool", lambda e: e.tensor_tensor(out=mv[p][:, 3:4], in0=mv[p][:, 2:3], in1=mhalf[:, 0:1], op=ALU.pow),
                      reads=[("mv", p, 2), ("mhalf",)], writes=[("mv", p, 3)])
                Sc.op("dve", lambda e: e.tensor_scalar(out=yn[p], in0=yb, scalar1=mv[p][:, 0:1], scalar2=mv[p][:, 3:4],
                                                       op0=ALU.subtract, op1=ALU.mult),
                      reads=[PS(ybk), ("mv", p)], writes=[("yn", p)])
                if n > 0:
                    ret_tail(h, n - 1)
                if n == NT - 1:
                    ret_tail(h, n)
            return dict(a=ua, kv=ukv, y=uy, c=uc)

        all_slots = {0: head_slots(0)}
        seq = [(h, tg) for h in range(RET_H) for tg in range(4)][:int(os.environ.get('MK_R', 16))]
        for u in proj_units(0, 0, all_slots[0])[:int(os.environ.get('MK_U', 99))]:
            u()
        if os.environ.get('MK_NOPF', '') == '':
            all_slots[1] = head_slots(1)
        for i, (h, tg) in enumerate(seq):
            if tg == 0 and h >= 1 and h + 1 < RET_H:
                all_slots[h + 1] = head_slots(h + 1)
            pu = []
            if i + 1 < len(seq):
                h2, tg2 = seq[i + 1]
                pu = proj_units(h2, tg2, all_slots[h2])
            ru = []
            for n in range(4 * tg, 4 * tg + 4):
                u_ = recur_units(h, n)
                ru += [u_["a"], u_["kv"], (lambda u_=u_: (u_["y"](), u_["c"]()))]
            k = 0
            for j, r in enumerate(ru):
                r()
                if j % 3 != 0 and k < len(pu):
                    pu[k]()
                    k += 1
            while k < len(pu):
                pu[k]()
                k += 1
        dump("gatedT", gatedT, [128, 8, S], BF16, ("gatedT",))
        A.release(m1)


        if os.environ.get("MK_STOP", "") == "1":
            A.release(m1)
            Sc.finish()
            block = es.enter_context(nc.Block())
            Sc.emit(block)
            return nc, dbg_outs
        m2 = A.mark()
        SCALE = float(192.0 ** -0.5)
        wuq = A.alloc("wuq", (3, 1536), BF16)
        wpad = [A.alloc("wpad", (3, 128), BF16) for _ in range(2)]
        wukv = A.alloc("wukv", (2, 2048), BF16)
        mcs = A.alloc("mcs", (S,), BF16)
        cqnT = A.alloc("cqnT", (3, S), BF16)
        ckvnT = A.alloc("ckvnT", (2, S), BF16)
        kpeT = A.alloc("kpeT", (S,), BF16)
        v_aug = A.alloc("v_aug", (NT, 4, 130), BF16)
        Sc.dma("pool", wuq, w_uq.rearrange("(c p) n -> p c n", p=128), writes=[("wuq",)])
        Sc.dma("pool", wukv, w_ukv.rearrange("(c p) n -> p c n", p=128), writes=[("wukv",)])
        wuq4 = wuq.rearrange("p c (h k) -> p c h k", h=8)
        Sc.dma("pool", mcs[0:64, :], c_mcos64, writes=[("mcs", 0)])
        Sc.dma("pool", mcs[64:128, :], c_msin64, writes=[("mcs", 1)])

        mA = A.mark()
        wl = A.alloc("wl", (8, 704), BF16)
        mcos_t = A.alloc("mcos_t", (NT, 32), F32)
        msin_t = A.alloc("msin_t", (NT, 32), F32)
        cqn = [A.alloc("cqn", (384,), BF16) for _ in range(2)]
        ckvn = [A.alloc("ckvn", (256,), BF16) for _ in range(2)]
        kper = [A.alloc("kper", (128,), BF16) for _ in range(2)]
        lst = [A.alloc("lst", (8,), F32) for _ in range(2)]
        kr = [A.alloc("kr", (4, 32), F32) for _ in range(2)]
        ljunk = A.alloc("ljunk", (384,), BF16)
        for i in range(2):
            Sc.op("dve", lambda e, i=i: e.memset(kper[i], 0.0), reads=[], writes=[("kper", i)])
        Sc.dma("pool", wl, w_in_v[:, :, 4096:4800], writes=[("wl",)])
        Sc.dma("sp", mcos_t, c_mcos_tok, writes=[("mcos_t",)])
        Sc.dma("sp", msin_t, c_msin_tok, writes=[("msin_t",)])
        _stop = os.environ.get("MK_STOP", "")
        for t in range(0 if _stop == "2pre" else int(os.environ.get("MK_NT", NT))):
            p = t % 2
            b0 = 4 * p
            tsl = slice(t * 128, (t + 1) * 128)
            mm_group(bank(b0)[:, 0:384], [(uT[:, c, tsl], wl[:, c, 0:384]) for c in range(8)],
                     reads=[("uT",), ("wl",)], writes=[PS(b0)])
            mm_group(bank(b0 + 1)[:, 0:320], [(uT[:, c, tsl], wl[:, c, 384:704]) for c in range(8)],
                     reads=[("uT",), ("wl",)], writes=[PS(b0 + 1)])
            cq_ps = bank(b0)[:, 0:384]
            ckv_ps = bank(b0 + 1)[:, 0:256]
            kx1 = bank(b0 + 1)[:, 256:288]
            kx2 = bank(b0 + 1)[:, 288:320]
            _A = os.environ.get("MK_A", "sq,rope,tr,ev")
            if "sq" not in _A:
                continue
            Sc.op("act", lambda e, p=p, cq_ps=cq_ps: e.activation(out=ljunk, in_=cq_ps, func=AF.Square, accum_out=lst[p][:, 0:1]),
                  reads=[PS(b0)], writes=[("ljunk",), ("lst", p, 0)])
            Sc.op("act", lambda e, p=p, ckv_ps=ckv_ps: e.activation(out=ljunk[:, 0:256], in_=ckv_ps, func=AF.Square, accum_out=lst[p][:, 1:2]),
                  reads=[PS(b0 + 1)], writes=[("ljunk",), ("lst", p, 1)])
            Sc.op("act", lambda e, p=p: e.activation(out=lst[p][:, 2:3], in_=lst[p][:, 0:1], func=AF.Sqrt, scale=1.0 / 384, bias=EPS),
                  reads=[("lst", p, 0)], writes=[("lst", p, 2)])
            Sc.op("act", lambda e, p=p: e.activation(out=lst[p][:, 3:4], in_=lst[p][:, 1:2], func=AF.Sqrt, scale=1.0 / 256, bias=EPS),
                  reads=[("lst", p, 1)], writes=[("lst", p, 3)])
            Sc.op("dve", lambda e, p=p: e.reciprocal(out=lst[p][:, 4:6], in_=lst[p][:, 2:4]),
                  reads=[("lst", p, 2), ("lst", p, 3)], writes=[("lst", p, 4)])
            Sc.op("act", lambda e, p=p, cq_ps=cq_ps: e.activation(out=cqn[p], in_=cq_ps, func=AF.Copy, scale=lst[p][:, 4:5]),
                  reads=[PS(b0), ("lst", p, 4)], writes=[("cqn", p)])
            Sc.op("act", lambda e, p=p, ckv_ps=ckv_ps: e.activation(out=ckvn[p], in_=ckv_ps, func=AF.Copy, scale=lst[p][:, 4 + 1:6]),
                  reads=[PS(b0 + 1), ("lst", p, 4)], writes=[("ckvn", p)])
            if "rope" not in _A:
                continue
            cs = mcos_t[:, t, :]
            sn = msin_t[:, t, :]
            Sc.op("dve", lambda e, p=p, kx1=kx1, cs=cs: e.tensor_tensor(out=kr[p][:, 0, :], in0=kx1, in1=cs, op=ALU.mult),
                  reads=[PS(b0 + 1), ("mcos_t",)], writes=[("kr", p, 0)])
            Sc.op("dve", lambda e, p=p, kx2=kx2, sn=sn: e.tensor_tensor(out=kr[p][:, 1, :], in0=kx2, in1=sn, op=ALU.mult),
                  reads=[PS(b0 + 1), ("msin_t",)], writes=[("kr", p, 1)])
            Sc.op("dve", lambda e, p=p, kx2=kx2, cs=cs: e.tensor_tensor(out=kr[p][:, 2, :], in0=kx2, in1=cs, op=ALU.mult),
                  reads=[PS(b0 + 1), ("mcos_t",)], writes=[("kr", p, 2)])
            Sc.op("dve", lambda e, p=p, kx1=kx1, sn=sn: e.tensor_tensor(out=kr[p][:, 3, :], in0=kx1, in1=sn, op=ALU.mult),
                  reads=[PS(b0 + 1), ("msin_t",)], writes=[("kr", p, 3)])
            Sc.op("dve", lambda e, p=p: e.tensor_tensor(out=kper[p][:, 0:32], in0=kr[p][:, 0, :], in1=kr[p][:, 1, :], op=ALU.subtract),
                  reads=[("kr", p)], writes=[("kper", p, 0)])
            Sc.op("dve", lambda e, p=p: e.tensor_tensor(out=kper[p][:, 64:96], in0=kr[p][:, 0, :], in1=kr[p][:, 1, :], op=ALU.subtract),
                  reads=[("kr", p)], writes=[("kper", p, 2)])
            Sc.op("dve", lambda e, p=p: e.tensor_tensor(out=kper[p][:, 96:128], in0=kr[p][:, 2, :], in1=kr[p][:, 3, :], op=ALU.add),
                  reads=[("kr", p)], writes=[("kper", p, 3)])
            Sc.op("dve", lambda e, p=p: e.tensor_tensor(out=kper[p][:, 32:64], in0=kr[p][:, 2, :], in1=kr[p][:, 3, :], op=ALU.add),
                  reads=[("kr", p)], writes=[("kper", p, 1)])
            if "tr" not in _A:
                continue
            pT = bank_bf(b0 + 2)[:, 0:768].rearrange("p (a b) -> p a b", a=6)
            items = [(pT[:, c, :], cqn[p][:, c * 128:(c + 1) * 128]) for c in range(3)]
            items += [(pT[:, 3 + c, :], ckvn[p][:, c * 128:(c + 1) * 128]) for c in range(2)]
            items += [(pT[:, 5, :], kper[p])]
            transposes(items, reads=[("cqn", p), ("ckvn", p), ("kper", p), ("ident_bf",)], writes=[PS(b0 + 2)], ident=ident_bf)
            if "ev" not in _A:
                continue
            for c in range(3):
                Sc.op("dve", lambda e, c=c, pT=pT, tsl=tsl: e.tensor_scalar(out=cqnT[:, c, tsl], in0=pT[:, c, :], scalar1=cols[:, 64 + c:65 + c],
                                                                           scalar2=None, op0=ALU.mult),
                      reads=[PS(b0 + 2), ("cols",)], writes=[("cqnT", c, t)])
            for c in range(2):
                Sc.op("dve", lambda e, c=c, pT=pT, tsl=tsl: e.tensor_scalar(out=ckvnT[:, c, tsl], in0=pT[:, 3 + c, :], scalar1=cols[:, 67 + c:68 + c],
                                                                           scalar2=None, op0=ALU.mult),
                      reads=[PS(b0 + 2), ("cols",)], writes=[("ckvnT", c, t)])
            if "nokpe" in _A:
                continue
            Sc.op("dve", lambda e, pT=pT, tsl=tsl: e.tensor_copy(out=kpeT[:, tsl], in_=pT[:, 5, :]),
                  reads=[PS(b0 + 2)], writes=[("kpeT", t)])
        dump("cqnT", cqnT, [128, 3, S], BF16, ("cqnT",))
        dump("ckvnT", ckvnT, [128, 2, S], BF16, ("ckvnT",))
        A.release(mA)

        _stop = os.environ.get("MK_STOP", "")
        qn = A.alloc("qn", (S,), BF16)
        kn = A.alloc("kn", (S,), BF16)
        qpe = A.alloc("qpe", (S,), BF16)
        qr = [A.alloc("qr", (512,), F32) for _ in range(2)]
        pTs = [A.alloc("pTs", (512,), BF16) for _ in range(6)]
        accP = [A.alloc("accP", (512,), F32) for _ in range(4)]
        ones_f = A.alloc("ones_f", (128,), F32)
        maskb = A.alloc("maskb", (128,), BF16)
        zeros_bf = A.alloc("zeros_bf", (512,), BF16)
        Sc.op("pool", lambda e: e.memset(zeros_bf, 0.0), reads=[], writes=[("zeros_bf",)])
        Sc.op("dve", lambda e: e.tensor_scalar(out=maskb, in0=mask01, scalar1=-1.0, scalar2=30000.0, op0=ALU.add, op1=ALU.mult),
              reads=[("mask01",)], writes=[("maskb",)])
        Sc.op("dve", lambda e: e.memset(ones_f, 1.0), reads=[], writes=[("ones_f",)])
        wukv4 = wukv.rearrange("p c (h k) -> p c h k", h=8)
        cnt_st = {"s": 0, "o": 0}

        def build_wpad(hh):
            wq_ = wpad[hh % 2]
            for c in range(3):
                Sc.op("act", lambda e, c=c: e.activation(out=wq_[:, c, 0:64], in_=wuq4[:, c, hh, 128:192], func=AF.Copy),
                      reads=[("wuq",)], writes=[("wpad", hh % 2, c, 0)])
                Sc.op("act", lambda e, c=c: e.activation(out=wq_[:, c, 64:96], in_=wuq4[:, c, hh, 160:192], func=AF.Copy, scale=-1.0),
                      reads=[("wuq",)], writes=[("wpad", hh % 2, c, 1)])
                Sc.op("act", lambda e, c=c: e.activation(out=wq_[:, c, 96:128], in_=wuq4[:, c, hh, 128:160], func=AF.Copy),
                      reads=[("wuq",)], writes=[("wpad", hh % 2, c, 2)])
        for hp in range(0 if _stop in ("2A", "2pre") else 2):
            Sc.op("dve", lambda e: e.memset(v_aug[:, :, :, 128:130], 1.0), reads=[], writes=[("v_aug",)])
            for t in range(NT):
                pb = t % 2
                tsl = slice(t * 128, (t + 1) * 128)
                mm_group(bank(pb), [(ckvnT[:, c, tsl], wukv4[:, c, 4 * hp:4 * hp + 4, 128:256]) for c in range(2)],
                         reads=[("ckvnT",), ("wukv",)], writes=[PS(pb)])
                Sc.op("act", lambda e, t=t, pb=pb: e.activation(out=v_aug[:, t, :, 0:128], in_=bank(pb).rearrange("p (h k) -> p h k", h=4),
                                                                 func=AF.Copy),
                      reads=[PS(pb)], writes=[("v_aug", t)])
            for hl in range(0 if _stop == "2V" else 4):
                h = 4 * hp + hl
                for tg in range(4):
                    tsl = slice(tg * 512, (tg + 1) * 512)
                    pb = 7 * (tg % 2)
                    mm_group(bank(pb), [(wuq[:, c, h * 192:h * 192 + 128], cqnT[:, c, tsl]) for c in range(3)],
                             reads=[("wuq",), ("cqnT",)], writes=[PS(pb)])
                    Sc.op("act", lambda e, pb=pb, tsl=tsl: e.activation(out=qn[:, tsl], in_=bank(pb), func=AF.Copy, scale=SCALE),
                          reads=[PS(pb)], writes=[("qn", tg)])
                for tg in range(4):
                    tsl = slice(tg * 512, (tg + 1) * 512)
                    pb = 7 * (tg % 2)
                    mm_group(bank(pb), [(wukv[:, c, h * 256:h * 256 + 128], ckvnT[:, c, tsl]) for c in range(2)],
                             reads=[("wukv",), ("ckvnT",)], writes=[PS(pb)])
                    Sc.op("act", lambda e, pb=pb, tsl=tsl: e.activation(out=kn[:, tsl], in_=bank(pb), func=AF.Copy),
                          reads=[PS(pb)], writes=[("kn", tg)])
                wp_ = wpad[h % 2]
                if h == 0:
                    build_wpad(0)
                for tg in range(4):
                    tsl = slice(tg * 512, (tg + 1) * 512)
                    pb = 7 * (tg % 2)
                    mm_group(bank(pb), [(wp_[:, c, :], cqnT[:, c, tsl]) for c in range(3)],
                             reads=[("wpad", h % 2), ("cqnT",)], writes=[PS(pb)])
                    Sc.op("dve", lambda e, tsl=tsl, pb=pb: e.tensor_tensor(out=qpe[:, tsl], in0=bank(pb), in1=mcs[:, tsl], op=ALU.mult),
                          reads=[PS(pb), ("mcs",)], writes=[("qpe", tg)])
                if h + 1 < MLA_H:
                    build_wpad(h + 1)
                its = [(G, jb) for G in range(0 if _stop == "2P" else 4) for jb in range(4 * G + 4)]
                info = []
                for (G, jb) in its:
                    td = jb - 4 * G
                    q0 = max(td, 0) * 128
                    k3 = cnt_st["s"] % 6
                    sb_ = 1 + cnt_st["s"] % 3
                    cnt_st["s"] += 1
                    info.append(dict(G=G, jb=jb, td=td, q0=q0, N=512 - q0, k3=k3, sb=sb_))

                def emit_st(it):
                    ksl = slice(it["jb"] * 128, (it["jb"] + 1) * 128)
                    qsl = slice(it["G"] * 512 + it["q0"], (it["G"] + 1) * 512)
                    sbk, N_ = it["sb"], it["N"]
                    diag = it["td"] >= 0

                    def stf(e):
                        e.matmul(out=bank(sbk)[:, 0:N_], lhsT=kn[:, ksl], rhs=qn[:, qsl], start=True, stop=False)
                        ins = e.matmul(out=bank(sbk)[:, 0:N_], lhsT=kpeT[:, ksl], rhs=qpe[:, qsl], start=False, stop=not diag)
                        if diag:
                            ins = e.matmul(out=bank(sbk)[:, 0:128], lhsT=ident_bf, rhs=maskb, start=False, stop=True)
                        return ins
                    Sc.op("pe", stf, reads=[("kn",), ("qn",), ("kpeT",), ("qpe",), ("ident_bf",), ("maskb",)], writes=[PS(sbk)])

                def emit_exp(it):
                    k3, sb, N = it["k3"], it["sb"], it["N"]
                    Sc.op("act", lambda e: e.activation(out=pTs[k3][:, 0:N], in_=bank(sb)[:, 0:N], func=AF.Exp),
                          reads=[PS(sb)], writes=[("pTs", k3)])

                def emit_pv(it, hl=hl):
                    k3, N, G, jb, q0 = it["k3"], it["N"], it["G"], it["jb"], it["q0"]
                    ob = 4 + G % 2
                    which = jb % 2
                    ai = 2 * (G % 2) + which
                    ap_ = accP[ai]
                    en_ = "pool" if which == 0 else "dve"

                    def pv(e):
                        return e.matmul(out=bank(ob)[:, q0:512], lhsT=v_aug[:, jb, hl, 0:128], rhs=pTs[k3][:, 0:N],
                                        start=(jb == 0), stop=(jb == 4 * G + 3))
                    Sc.op("pe", pv, reads=[("pTs", k3), ("v_aug",)], writes=[PS(ob)])
                    if jb < 2:
                        if q0 > 0:
                            Sc.op(en_, lambda e: e.memset(ap_[:, 0:q0], 0.0), reads=[], writes=[("accP", ai)])
                        if en_ == "pool":
                            Sc.op(en_, lambda e: e.tensor_tensor(out=ap_[:, q0:512], in0=pTs[k3][:, 0:N], in1=zeros_bf[:, 0:N], op=ALU.add),
                                  reads=[("pTs", k3), ("zeros_bf",)], writes=[("accP", ai)])
                        else:
                            Sc.op(en_, lambda e: e.tensor_copy(out=ap_[:, q0:512], in_=pTs[k3][:, 0:N]), reads=[("pTs", k3)], writes=[("accP", ai)])
                    else:
                        Sc.op(en_, lambda e: e.tensor_tensor(out=ap_[:, q0:512], in0=ap_[:, q0:512], in1=pTs[k3][:, 0:N], op=ALU.add),
                              reads=[("pTs", k3), ("accP", ai)], writes=[("accP", ai)])

                def fin_steps(G, h=h):
                    ob = 4 + G % 2

                    def s0():
                        mm_group(bank(6), [(ones_f, accP[2 * (G % 2)]), (ones_f, accP[2 * (G % 2) + 1])],
                                 reads=[("ones_f",), ("accP", 2 * (G % 2)), ("accP", 2 * (G % 2) + 1)], writes=[PS(6)])

                    def piece(j):
                        def f():
                            cs_ = slice(j * 128, (j + 1) * 128)
                            Sc.op("dve", lambda e: e.reciprocal(out=qr[0][:, cs_], in_=bank(6)[:, cs_]), reads=[PS(6)], writes=[("qr", 0, j)])
                            Sc.op("dve", lambda e: e.tensor_tensor(out=attnT[:, h, G * 512 + j * 128:G * 512 + (j + 1) * 128],
                                                                   in0=bank(ob)[:, cs_], in1=qr[0][:, cs_], op=ALU.mult),
                                  reads=[PS(ob), ("qr", 0, j)], writes=[("attnT", h, G, j)])
                        return f
                    return [s0] + [piece(j) for j in range(4)]

                pend = []
                for j in range(min(2, len(info))):
                    emit_st(info[j])
                for i, it in enumerate(info):
                    if i + 2 < len(info):
                        emit_st(info[i + 2])
                    emit_exp(it)
                    emit_pv(it)
                    if pend:
                        pend.pop(0)()
                    if it["jb"] == 4 * it["G"] + 3:
                        pend += [(lambda: None), (lambda: None)] + fin_steps(it["G"])
                while pend:
                    pend.pop(0)()
        dump("attnT", attnT, [128, 8, S], BF16, ("attnT",))
        A.release(m2)


        dump("gT2", gatedT, [128, 8, S], BF16, ("gatedT",))
        dump("aT2", attnT, [128, 8, S], BF16, ("attnT",))
        dump("uT2", uT, [128, 8, S], BF16, ("uT",))
        m3 = A.mark()
        mergedT = A.alloc("mergedT", (8, S), BF16)
        wout = A.alloc("wout", (8, D), BF16)
        Sc.dma("pool", wout, w_out.rearrange("(c p) n -> p c n", p=128), writes=[("wout",)])
        m3a = A.mark()
        wm = [[A.alloc("wm", (8, 128), BF16) for _ in range(4)] for _ in range(2)]
        sgA = [A.alloc("sgA", (512,), BF16) for _ in range(2)]
        sgB = [A.alloc("sgB", (512,), BF16) for _ in range(2)]
        mt1 = [A.alloc("mt1", (512,), F32) for _ in range(2)]
        mt2 = [A.alloc("mt2", (512,), F32) for _ in range(2)]
        w_ro_v = w_ret_o.rearrange("(c p) n -> p c n", p=128)
        w_mo_v = w_mla_o.rearrange("(c p) n -> p c n", p=128)

        def load_wm(fc):
            ws = fc % 2
            srcs = [w_ro_v[:, :, fc * 128:(fc + 1) * 128], w_mo_v[:, :, fc * 128:(fc + 1) * 128],
                    w_in_v[:, :, 4800 + fc * 128:4800 + (fc + 1) * 128], w_in_v[:, :, 5824 + fc * 128:5824 + (fc + 1) * 128]]
            for i in range(4):
                Sc.dma("pool", wm[ws][i], srcs[i], writes=[("wm", ws, i)])

        load_wm(0)
        for fc in range(8):
            ws = fc % 2
            if fc + 1 < 8:
                load_wm(fc + 1)
            for tg in range(4):
                it = fc * 4 + tg
                p = it % 2
                bs = p * 4
                tsl = slice(tg * 512, (tg + 1) * 512)
                srcT = [(gatedT, ("gatedT",)), (attnT, ("attnT",)), (uT, ("uT",)), (uT, ("uT",))]
                for i in range(4):
                    mm_group(bank(bs + i), [(wm[ws][i][:, c, :], srcT[i][0][:, c, tsl]) for c in range(8)],
                             reads=[("wm", ws, i), srcT[i][1]], writes=[PS(bs + i)])
                Sc.op("act", lambda e, p=p, bs=bs: e.activation(out=sgA[p], in_=bank(bs + 2), func=AF.Sigmoid),
                      reads=[PS(bs + 2)], writes=[("sgA", p)])
                Sc.op("act", lambda e, p=p, bs=bs: e.activation(out=sgB[p], in_=bank(bs + 3), func=AF.Sigmoid),
                      reads=[PS(bs + 3)], writes=[("sgB", p)])
                Sc.op("dve", lambda e, p=p, bs=bs: e.tensor_tensor(out=mt1[p], in0=bank(bs), in1=sgA[p], op=ALU.mult),
                      reads=[PS(bs), ("sgA", p)], writes=[("mt1", p)])
                Sc.op("dve", lambda e, p=p, bs=bs: e.tensor_tensor(out=mt2[p], in0=bank(bs + 1), in1=sgB[p], op=ALU.mult),
                      reads=[PS(bs + 1), ("sgB", p)], writes=[("mt2", p)])
                Sc.op("dve", lambda e, p=p, fc=fc, tsl=tsl: e.tensor_tensor(out=mergedT[:, fc, tsl], in0=mt1[p], in1=mt2[p], op=ALU.add),
                      reads=[("mt1", p), ("mt2", p)], writes=[("mergedT", fc, tg)])
        dump("mergedT", mergedT, [128, 8, S], BF16, ("mergedT",))
        dump("sgA1", sgA[1], [128, 512], BF16, ("sgA", 1))
        dump("mt11", mt1[1], [128, 512], F32, ("mt1", 1))
        dump("mt21", mt2[1], [128, 512], F32, ("mt2", 1))
        dump("wm1", wm[1][0], [128, 8, 128], BF16, ("wm", 1, 0))
        dump("gT3", gatedT, [128, 8, S], BF16, ("gatedT",))
        dump("aT3", attnT, [128, 8, S], BF16, ("attnT",))
        dump("uT3", uT, [128, 8, S], BF16, ("uT",))
        A.release(m3a)

        if os.environ.get("MK_STOP", "") == "3a":
            raise_stop = True
        else:
            raise_stop = False
        offG = A.offs["gatedT"]
        assert A.offs["attnT"] == offG + 32768
        acc = arena_t[:, offG // 4:offG // 4 + NT * D].rearrange("p (t d) -> p t d", t=NT)
        Sc.alias("acc", "gatedT")
        Sc.alias("acc", "attnT")
        xt = [A.alloc("xt", (D,), F32) for _ in range(2)]
        xn = [A.alloc("xn", (D,), BF16) for _ in range(2)]
        junk = A.alloc("junk", (D,), BF16)
        mtmp = [A.alloc("mtmp", (D,), F32) for _ in range(2)]
        def pre3b(t):
            p = t % 2
            tsl = slice(t * 128, (t + 1) * 128)
            Sc.dma("sp", xt[p], x[t * 128:(t + 1) * 128, :], writes=[("xt", p)])
            for half in range(2):
                hb = p * 2 + half
                hs = slice(half * 512, (half + 1) * 512)
                mm_group(bank(hb), [(mergedT[:, c, tsl], wout[:, c, hs]) for c in range(8)],
                         reads=[("mergedT",), ("wout",)], writes=[PS(hb)])
                Sc.op("dve", lambda e, p=p, hb=hb, hs=hs: e.tensor_tensor(out=mtmp[p][:, hs], in0=bank(hb), in1=g1b[:, hs], op=ALU.mult),
                      reads=[PS(hb), ("g1b",)], writes=[("mtmp", p, half)])
                Sc.op("dve", lambda e, p=p, t=t, hs=hs: e.tensor_tensor(out=acc[:, t, hs], in0=mtmp[p][:, hs], in1=xt[p][:, hs], op=ALU.add),
                      reads=[("mtmp", p, half), ("xt", p)], writes=[("acc", t, half)])

        if not raise_stop:
            pre3b(0)
            norm_A(0, acc[:, 0, :], ("acc", 0))
            for t in range(NT):
                if t + 1 < NT:
                    pre3b(t + 1)
                    norm_A(t + 1, acc[:, t + 1, :], ("acc", t + 1))
                norm_B(t, A2, modc[:, 24:32], ("A2",), ("modc", 1), uT, "uT", pb=4 + t % 2)
        dump("h1", acc, [128, NT, D], F32, ("acc",))
        dump("u2T", uT, [128, 8, S], BF16, ("uT",))
        A.release(m3)

        m4 = A.mark()
        wr = A.alloc("wr", (8, 36), BF16)
        brt = A.alloc("brt", (36,), F32)
        gate = A.alloc("gate", (NT, 32), F32)
        hid = [A.alloc("hid", (2, S), BF16) for _ in range(2)]
        sa = [A.alloc("sa", (512,), BF16) for _ in range(2)]
        NWS = 3
        w1e = [A.alloc("w1e", (8, 256), BF16) for _ in range(NWS)]
        w3e = [A.alloc("w3e", (8, 256), BF16) for _ in range(NWS)]
        w2e = [A.alloc("w2e", (2, D), BF16) for _ in range(NWS)]
        w2s = [A.alloc("w2s", (2, D), F32) for _ in range(2)]
        junk = A.alloc("junk", (D,), BF16)
        fnb = A.alloc("fnb", (D,), F32)
        Sc.dma("sp", fnb, fnorm[0, :].partition_broadcast(128), writes=[("fnb",)])
        Sc.dma("pool", wr[:, :, 0:4], w_grp.rearrange("(c p) n -> p c n", p=128), writes=[("wr", 0)])
        Sc.dma("pool", wr[:, :, 4:36], w_exp.rearrange("(c p) n -> p c n", p=128), writes=[("wr", 1)])
        Sc.dma("sp", brt, b_rt[0, :].partition_broadcast(128), writes=[("brt",)])

        def load_expert(e):
            sl = e % NWS
            Sc.dma("pool", w1e[sl], w1[e].rearrange("(c p) f -> p c f", p=128), writes=[("w1e", sl)])
            Sc.dma("pool", w3e[sl], w3[e].rearrange("(c p) f -> p c f", p=128), writes=[("w3e", sl)])
            Sc.dma("sp", w2s[e % 2], w2[e].rearrange("(c p) d -> p c d", p=128), writes=[("w2s", e % 2)])
            for c in range(2):
                Sc.op("pool", lambda en, c=c, sl=sl, e=e: en.tensor_tensor(out=w2e[sl][:, c, :], in0=w2s[e % 2][:, c, :], in1=g2b, op=ALU.mult),
                      reads=[("w2s", e % 2), ("g2b",)], writes=[("w2e", sl, c)])

        load_expert(0)
        X = mybir.AxisListType.X
        LG = A.alloc("LG", (NT, 36), F32)
        r16 = A.alloc("r16", (12, NT), F32)
        r4a = A.alloc("r4a", (NT, 4), F32)
        r4b = A.alloc("r4b", (NT, 4), F32)
        rml = A.alloc("rml", (NT, 32), F32)
        rml2 = A.alloc("rml2", (NT, 32), F32)
        rm1 = A.alloc("rm1", (NT, 32), F32)
        rm2 = A.alloc("rm2", (NT, 32), F32)
        for t in range(0 if raise_stop else NT):
            p = t % 2
            tsl = slice(t * 128, (t + 1) * 128)
            lg = bank(p)[:, 0:36]
            mm_group(lg, [(uT[:, c, tsl], wr[:, c, :]) for c in range(8)], reads=[("uT",), ("wr",)], writes=[PS(p)])
            Sc.op("dve", lambda e, t=t, lg=lg: e.tensor_tensor(out=LG[:, t, :], in0=lg, in1=brt, op=ALU.add),
                  reads=[PS(p), ("brt",)], writes=[("LG", t)])
        if not raise_stop:
            Lg_ = LG[:, :, 0:4]
            Le_ = LG[:, :, 4:36].rearrange("p t (g e) -> p t g e", g=4)

            def b3(row, n):
                return r16[:, row, :].unsqueeze(2).to_broadcast([128, NT, n])

            def dv(fn, reads, writes):
                Sc.op("dve", fn, reads=reads, writes=writes)
            dv(lambda e: e.tensor_reduce(out=r16[:, 0, :], in_=Lg_, axis=X, op=ALU.max), [("LG",)], [("r16", 0)])
            dv(lambda e: e.tensor_tensor(out=r4a, in0=Lg_, in1=b3(0, 4), op=ALU.is_equal), [("LG",), ("r16", 0)], [("r4a",)])
            dv(lambda e: e.tensor_tensor(out=r4b, in0=Lg_, in1=b3(0, 4), op=ALU.subtract), [("LG",), ("r16", 0)], [("r4b",)])
            Sc.op("act", lambda e: e.activation(out=r4b, in_=r4b, func=AF.Exp), reads=[("r4b",)], writes=[("r4b",)])
            dv(lambda e: e.tensor_reduce(out=r16[:, 1, :], in_=r4b, axis=X, op=ALU.add), [("r4b",)], [("r16", 1)])
            dv(lambda e: e.reciprocal(out=r16[:, 2, :], in_=r16[:, 1, :]), [("r16", 1)], [("r16", 2)])
            dv(lambda e: e.tensor_scalar(out=r4a, in0=r4a, scalar1=-1.0, scalar2=1.0e30, op0=ALU.add, op1=ALU.mult), [("r4a",)], [("r4a",)])
            dv(lambda e: e.tensor_tensor(out=rml.rearrange("p t (g e) -> p t g e", g=4), in0=Le_,
                                         in1=r4a.unsqueeze(3).to_broadcast([128, NT, 4, 8]), op=ALU.add), [("LG",), ("r4a",)], [("rml",)])
            dv(lambda e: e.tensor_reduce(out=r16[:, 3, :], in_=rml, axis=X, op=ALU.max), [("rml",)], [("r16", 3)])
            dv(lambda e: e.tensor_tensor(out=rm1, in0=rml, in1=b3(3, 32), op=ALU.is_equal), [("rml",), ("r16", 3)], [("rm1",)])
            dv(lambda e: e.scalar_tensor_tensor(out=rml2, in0=rm1, scalar=-1.0e30, in1=rml, op0=ALU.mult, op1=ALU.add),
               [("rm1",), ("rml",)], [("rml2",)])
            dv(lambda e: e.tensor_reduce(out=r16[:, 4, :], in_=rml2, axis=X, op=ALU.max), [("rml2",)], [("r16", 4)])
            dv(lambda e: e.tensor_tensor(out=rm2, in0=rml2, in1=b3(4, 32), op=ALU.is_equal), [("rml2",), ("r16", 4)], [("rm2",)])
            dv(lambda e: e.tensor_tensor(out=r16[:, 5, :], in0=r16[:, 4, :], in1=r16[:, 3, :], op=ALU.subtract), [("r16", 3), ("r16", 4)], [("r16", 5)])
            Sc.op("act", lambda e: e.activation(out=r16[:, 6, :], in_=r16[:, 5, :], func=AF.Exp), reads=[("r16", 5)], writes=[("r16", 6)])
            dv(lambda e: e.tensor_scalar(out=r16[:, 7, :], in0=r16[:, 6, :], scalar1=1.0, scalar2=None, op0=ALU.add), [("r16", 6)], [("r16", 7)])
            dv(lambda e: e.reciprocal(out=r16[:, 8, :], in_=r16[:, 7, :]), [("r16", 7)], [("r16", 8)])
            dv(lambda e: e.tensor_tensor(out=r16[:, 9, :], in0=r16[:, 8, :], in1=r16[:, 2, :], op=ALU.mult), [("r16", 8), ("r16", 2)], [("r16", 9)])
            dv(lambda e: e.tensor_tensor(out=r16[:, 10, :], in0=r16[:, 9, :], in1=r16[:, 6, :], op=ALU.mult), [("r16", 9), ("r16", 6)], [("r16", 10)])
            dv(lambda e: e.tensor_tensor(out=rm1, in0=rm1, in1=b3(9, 32), op=ALU.mult), [("rm1",), ("r16", 9)], [("rm1",)])
            dv(lambda e: e.tensor_tensor(out=rm2, in0=rm2, in1=b3(10, 32), op=ALU.mult), [("rm2",), ("r16", 10)], [("rm2",)])
            dv(lambda e: e.tensor_tensor(out=gate, in0=rm1, in1=rm2, op=ALU.add), [("rm1",), ("rm2",)], [("gate",)])
        dump("gate", gate, [128, NT, 32], F32, ("gate",))

        n_exp = 0 if raise_stop else int(os.environ.get("MK_NEXP", N_EXP))

        def up_steps(ex):
            sl = ex % NWS
            hp_ = ex % 2
            steps = []
            for fch in range(2):
                for tg in range(4):
                    def f(fch=fch, tg=tg):
                        fs = slice(fch * 128, (fch + 1) * 128)
                        pp = (fch * 4 + tg) % 2
                        tsl = slice(tg * 512, (tg + 1) * 512)
                        mm_group(bank(pp), [(w1e[sl][:, c, fs], uT[:, c, tsl]) for c in range(8)],
                                 reads=[("w1e", sl), ("uT",)], writes=[PS(pp)])
                        mm_group(bank(2 + pp), [(w3e[sl][:, c, fs], uT[:, c, tsl]) for c in range(8)],
                                 reads=[("w3e", sl), ("uT",)], writes=[PS(2 + pp)])
                        Sc.op("act", lambda e: e.activation(out=sa[pp], in_=bank(pp), func=AF.Silu), reads=[PS(pp)], writes=[("sa", pp)])
                        Sc.op("dve", lambda e: e.tensor_tensor(out=hid[hp_][:, fch, tsl], in0=bank(2 + pp), in1=sa[pp], op=ALU.mult),
                              reads=[PS(2 + pp), ("sa", pp)], writes=[("hid", hp_, fch, tg)])
                    steps.append(f)
            return steps

        def down_steps(ex):
            sl = ex % NWS
            hp_ = ex % 2
            steps = []
            for t in range(NT):
                def f(t=t):
                    ob = 4 + (t % 2) * 2
                    tsl = slice(t * 128, (t + 1) * 128)
                    for half in range(2):
                        mm_group(bank(ob + half), [(hid[hp_][:, fch, tsl], w2e[sl][:, fch, half * 512:(half + 1) * 512]) for fch in range(2)],
                                 reads=[("hid", hp_, 0, t // 4), ("hid", hp_, 1, t // 4), ("w2e", sl)], writes=[PS(ob + half)])
                    Sc.op("dve", lambda e: e.scalar_tensor_tensor(
                        out=acc[:, t, :].rearrange("p (a b) -> p a b", a=2), in0=psum_t[:, ob:ob + 2, :], scalar=gate[:, t, ex:ex + 1],
                        in1=acc[:, t, :].rearrange("p (a b) -> p a b", a=2), op0=ALU.mult, op1=ALU.add),
                        reads=[PS(ob), PS(ob + 1), ("gate", t), ("acc", t)], writes=[("acc", t)])
                steps.append(f)
            return steps

        if n_exp > 0:
            if n_exp > 1:
                load_expert(1)
            for st_ in up_steps(0):
                st_()
        for ex in range(n_exp):
            if ex + 2 < n_exp:
                load_expert(ex + 2)
            dn = down_steps(ex)
            up = up_steps(ex + 1) if ex + 1 < n_exp else []
            for i, d_ in enumerate(dn):
                d_()
                if i % 2 == 1 and (i // 2) < len(up):
                    up[i // 2]()
        dump("h2", acc, [128, NT, D], F32, ("acc",))

        for t in range(0 if raise_stop else NT):
            Sc.op("act", lambda e, t=t: e.activation(out=junk, in_=acc[:, t, :], func=AF.Square, accum_out=stat[:, t:t + 1]),
                  reads=[("acc", t)], writes=[("junk",), ("stat", t)])
        if not raise_stop:
            Sc.op("act", lambda e: e.activation(out=stat[:, 16:32], in_=stat[:, 0:16], func=AF.Sqrt, scale=1.0 / D, bias=EPS),
                  reads=[("stat",)], writes=[("stat", "sq")])
            Sc.op("dve", lambda e: e.reciprocal(out=stat[:, 32:48], in_=stat[:, 16:32]), reads=[("stat", "sq")], writes=[("stat", "rs")])
        for t in range(0 if raise_stop else NT):
            p = t % 2
            Sc.op("dve", lambda e, t=t, p=p: e.scalar_tensor_tensor(out=w2s[p][:, 0, :], in0=acc[:, t, :], scalar=stat[:, 32 + t:33 + t], in1=fnb,
                                                                   op0=ALU.mult, op1=ALU.mult),
                  reads=[("acc", t), ("stat", "rs"), ("fnb",)], writes=[("w2s", p)])
            Sc.dma("sp", out_d[t * 128:(t + 1) * 128, :], w2s[p][:, 0, :], reads=[("w2s", p)], writes=[("out", t)], is_output=True)
        A.release(m4)

        Sc.finish()
        block = es.enter_context(nc.Block())
        Sc.emit(block)
        print(f"[build] instructions={Sc.nins} arena_peak={A.peak}")
    return nc, dbg_outs


_CACHE = {}


def _consts():
    idx = np.arange(128, dtype=np.float64)
    ident = np.eye(128, dtype=np.float32)
    mask = (idx[None, :] >= idx[:, None]).astype(np.float32)
    pos = np.arange(S, dtype=np.float32)
    inv_r = (np.float32(10000.0) ** (-np.arange(0, 256, 2, dtype=np.float32) / np.float32(256))).astype(np.float32)
    ang_r = pos[:, None] * inv_r[None, :]
    rcos = np.cos(ang_r).T.astype(np.float32).copy()
    rsin = np.sin(ang_r).T.astype(np.float32).copy()
    inv_m = (np.float32(10000.0) ** (-np.arange(0, 64, 2, dtype=np.float32) / np.float32(64))).astype(np.float32)
    ang_m = pos[:, None] * inv_m[None, :]
    mcos = np.cos(ang_m).astype(np.float32)
    msin = np.sin(ang_m).astype(np.float32)
    scale = np.float32(192.0 ** -0.5)
    mcos64 = (np.concatenate([mcos, mcos], axis=1).T * scale).astype(np.float32).copy()
    msin64 = (np.concatenate([msin, msin], axis=1).T * scale).astype(np.float32).copy()
    mcos_tok = mcos.reshape(NT, 128, 32).transpose(1, 0, 2).copy()
    msin_tok = msin.reshape(NT, 128, 32).transpose(1, 0, 2).copy()
    lg = np.log1p(-np.exp2(-5.0 - np.arange(RET_H, dtype=np.float64)))
    zp = np.exp(-lg[None, :] * (idx[:, None] + 1.0)) * (256.0 ** -0.5)
    eox2 = EPS * np.exp(-2.0 * lg[None, :] * (idx[:, None] + 1.0))
    cret = np.concatenate([zp, eox2], axis=1).astype(np.float32)
    return dict(c_ident=ident, c_mask=mask, c_rcos=rcos, c_rsin=rsin, c_mcos64=mcos64, c_msin64=msin64,
                c_mcos_tok=mcos_tok, c_msin_tok=msin_tok, c_ret=cret)


def make_in_maps(inputs):
    f = lambda a: np.ascontiguousarray(np.asarray(a, dtype=np.float32))
    g = {k: f(v) for k, v in inputs.items()}
    shared = dict(
        b_ada=g["b_ada"].reshape(1, 6 * D), w_ada=g["w_ada"][0], w_in=g["w_in"][0], w_ret_o=g["w_ret_o"][0],
        w_uq=g["w_uq"][0], w_ukv=g["w_ukv"][0], w_mla_o=g["w_mla_o"][0], w_out=g["w_out"][0],
        w_grp=g["w_grp"][0], w_exp=g["w_exp"][0],
        b_rt=np.concatenate([g["b_grp"][0], g["b_exp"][0]]).reshape(1, 36),
        w1=g["w1"][0].reshape(N_EXP, D, 256), w3=g["w3"][0].reshape(N_EXP, D, 256), w2=g["w2"][0].reshape(N_EXP, 256, D),
        final_norm=g["final_norm"].reshape(1, D),
    )
    shared.update(_consts())
    maps = []
    for b in range(NCORES):
        rows = np.concatenate([g["b_ada"].reshape(48, 128), g["norm1"].reshape(8, 128), g["norm2"].reshape(8, 128),
                               g["q_norm"].reshape(3, 128), g["kv_norm"].reshape(2, 128), g["c"][b].reshape(8, 128)], axis=0)
        m = dict(shared)
        m["x"] = g["x"][b]
        m["rows"] = np.ascontiguousarray(rows)
        maps.append(m)
    return maps


def kernel(**inputs):
    if "nc" not in _CACHE:
        _CACHE["nc"] = build_program()[0]
    nc = _CACHE["nc"]
    maps = make_in_maps(inputs)
    res = run_bass_kernel_spmd(nc, maps, core_ids=list(range(NCORES)))
    return np.stack([np.asarray(r["out"], dtype=np.float32) for r in res.results], axis=0)
```

```python
import contextlib
import os
import numpy as np
import concourse.bass as bass
import concourse.mybir as mybir
from concourse.bass_utils import run_bass_kernel_spmd

F32 = mybir.dt.float32
BF16 = mybir.dt.bfloat16
AF = mybir.ActivationFunctionType
ALU = mybir.AluOpType

D = 1024
S = 2048
NT = 16
NCORES = 8
EPS = 1e-6
RET_H = 4
MLA_H = 8
IN_W = 6848
N_EXP = 32

ENGS = ("pe", "act", "dve", "pool", "sp")
SAME_ENGINE_SYNC = True


def _conflict(k1, k2):
    n = min(len(k1), len(k2))
    return k1[:n] == k2[:n]


class Sched:
    def __init__(self, nc, es, ndma=12):
        self.nc = nc
        self.sem = {}
        for e in ENGS:
            self.sem[("c", e)] = es.enter_context(nc.semaphore(f"c_{e}"))
        self.cnt = {e: 0 for e in ENGS}
        self.ndma = ndma
        self.dcnt = {}
        self.drr = {}
        for q in ("sp", "pool", "act"):
            self.dcnt[q] = [0] * ndma
            self.drr[q] = 0
            for i in range(ndma):
                self.sem[("d", q, i)] = es.enter_context(nc.semaphore(f"d_{q}{i}"))
        self.seen = {e: {} for e in ENGS}
        self.prog = {e: [] for e in ENGS}
        self.lastw = {}
        self.readers = {}
        self.out_tokens = []
        self.nins = 0

    def _collect(self, eng, reads, writes):
        toks = set()
        for k in reads:
            for (k2, tok) in self.lastw.get(k[0], ()):
                if _conflict(k, k2):
                    toks.add(tok)
        for k in writes:
            for (k2, tok) in self.lastw.get(k[0], ()):
                if _conflict(k, k2):
                    toks.add(tok)
            for (k2, tok) in self.readers.get(k[0], ()):
                if _conflict(k, k2):
                    toks.add(tok)
        waits = []
        for (s, v) in sorted(toks, key=lambda t: (str(t[0]), t[1])):
            if s == ("c", eng) and (eng == "pe" or eng == "sp" or not SAME_ENGINE_SYNC):
                continue
            if self.seen[eng].get(s, 0) >= v:
                continue
            waits.append((s, v))
        best = {}
        for (s, v) in waits:
            best[s] = max(best.get(s, 0), v)
        for s, v in best.items():
            self.seen[eng][s] = v
        return list(best.items())

    def _commit(self, tok, reads, writes):
        for k in writes:
            lw = self.lastw.setdefault(k[0], [])
            lw[:] = [(k2, t) for (k2, t) in lw if not (len(k2) >= len(k) and k2[:len(k)] == k)]
            lw.append((k, tok))
            rd = self.readers.setdefault(k[0], [])
            rd[:] = [(k2, t) for (k2, t) in rd if not (len(k2) >= len(k) and k2[:len(k)] == k)]
        for k in reads:
            rd = self.readers.setdefault(k[0], [])
            rd[:] = [(k2, t) for (k2, t) in rd if not (k2 == k and t[0] == tok[0])]
            rd.append((k, tok))

    def op(self, eng, fn, reads=(), writes=()):
        reads = [tuple(k) if isinstance(k, (tuple, list)) else (k,) for k in reads]
        writes = [tuple(k) if isinstance(k, (tuple, list)) else (k,) for k in writes]
        reads = [k[:2] if k[0] == "ps" else k for k in reads]
        writes = [k[:2] if k[0] == "ps" else k for k in writes]
        waits = self._collect(eng, reads, writes)
        self.cnt[eng] += 1
        tok = (("c", eng), self.cnt[eng])
        self._commit(tok, reads, writes)
        self.prog[eng].append((waits, fn, ("c", eng), 1))
        self.nins += 1
        return tok

    def dma(self, q, out, in_, reads=(), writes=(), is_output=False):
        reads = [tuple(k) if isinstance(k, (tuple, list)) else (k,) for k in reads]
        writes = [tuple(k) if isinstance(k, (tuple, list)) else (k,) for k in writes]
        waits = self._collect(q, reads, writes)
        i = self.drr[q]
        self.drr[q] = (i + 1) % self.ndma
        s = ("d", q, i)
        if self.dcnt[q][i] > 0:
            v = 16 * self.dcnt[q][i]
            if self.seen[q].get(s, 0) < v:
                self.seen[q][s] = v
                waits = [w for w in waits if w[0] != s] + [(s, v)]
        self.dcnt[q][i] += 1
        tok = (s, 16 * self.dcnt[q][i])
        self._commit(tok, reads, writes)

        def fn(e, out=out, in_=in_):
            return e.dma_start(out=out, in_=in_)
        self.prog[q].append((waits, fn, s, 16))
        if is_output:
            self.out_tokens.append(tok)
        self.nins += 1
        return tok

    def alias(self, new, old):
        toks = set(t for (_, t) in self.lastw.get(old, ())) | set(t for (_, t) in self.readers.get(old, ()))
        lw = self.lastw.setdefault(new, [])
        for t in sorted(toks, key=lambda t: (str(t[0]), t[1])):
            lw.append(((new,), t))

    def barrier(self):
        snap = dict(self.cnt)
        for e in ENGS:
            waits = []
            for f in ENGS:
                if f == e or snap[f] == 0:
                    continue
                s = ("c", f)
                if self.seen[e].get(s, 0) < snap[f]:
                    self.seen[e][s] = snap[f]
                    waits.append((s, snap[f]))
            if waits:
                self.prog[e].append((waits, None, None, 0))

    def finish(self):
        waits = {}
        for (s, v) in self.out_tokens:
            waits[s] = max(waits.get(s, 0), v)
        for q in ("sp", "pool", "act"):
            for i in range(self.ndma):
                if self.dcnt[q][i] > 0:
                    waits[("d", q, i)] = 16 * self.dcnt[q][i]
        for e in ENGS:
            if self.cnt[e] > 0:
                waits[("c", e)] = self.cnt[e]
        self.prog["sp"].append((list(waits.items()), None, None, 0))

    def emit(self, block):
        nc = self.nc
        sem = self.sem

        def replay(eng_name):
            def run(e):
                for (waits, fn, s, inc) in self.prog[eng_name]:
                    for (ws, wv) in waits:
                        e.wait_ge(sem[ws], wv)
                    if fn is not None:
                        ins = fn(e)
                        ins.then_inc(sem[s], inc)
            return run

        block.tensor(replay("pe"))
        block.scalar(replay("act"))
        block.vector(replay("dve"))
        block.gpsimd(replay("pool"))
        block.sync(replay("sp"))


class Arena:
    def __init__(self, base_ap, nbytes, sched):
        self.base = base_ap
        self.nbytes = nbytes
        self.off = 0
        self.peak = 0
        self.sched = sched
        self.history = []

    def mark(self):
        return self.off

    def release(self, m):
        self.off = m

    def alloc(self, name, shape, dtype):
        n = int(np.prod(shape))
        esz = 4 if dtype == F32 else 2
        nb = (n * esz + 31) // 32 * 32
        assert self.off + nb <= self.nbytes, f"arena overflow {self.off}+{nb}>{self.nbytes}"
        lo, hi = self.off, self.off + nb
        olds = set(nm for (l, h, nm) in self.history if l < hi and lo < h and nm != name)
        for o in sorted(olds):
            self.sched.alias(name, o)
        self.history.append((lo, hi, name))
        self.offs = getattr(self, "offs", {})
        self.offs.setdefault(name, lo)
        a = self.base[:, self.off // 4:(self.off + nb) // 4]
        self.off += nb
        self.peak = max(self.peak, self.off)
        if dtype != F32:
            a = a.bitcast(dtype)
        a = a[:, 0:n]
        if len(shape) == 2:
            a = a.rearrange("p (a b) -> p a b", a=shape[0])
        elif len(shape) == 3:
            a = a.rearrange("p (a b c) -> p a b c", a=shape[0], b=shape[1])
        return a


def build_program(dbg=()):
    nc = bass.Bass("TRN2", target_bir_lowering=False)
    dbg = set(dbg)

    def din(name, shape, dt=F32):
        return nc.dram_tensor(name, list(shape), dt, kind="ExternalInput").ap()

    x = din("x", [S, D])
    rows_d = din("rows", [77, 128])
    b_ada = din("b_ada", [1, 6 * D])
    w_ada = din("w_ada", [D, 6 * D])
    w_in = din("w_in", [D, IN_W])
    w_ret_o = din("w_ret_o", [D, D])
    w_uq = din("w_uq", [384, 1536])
    w_ukv = din("w_ukv", [256, 2048])
    w_mla_o = din("w_mla_o", [D, D])
    w_out = din("w_out", [D, D])
    w_grp = din("w_grp", [D, 4])
    w_exp = din("w_exp", [D, 32])
    b_rt = din("b_rt", [1, 36])
    w1 = din("w1", [N_EXP, D, 256])
    w3 = din("w3", [N_EXP, D, 256])
    w2 = din("w2", [N_EXP, 256, D])
    fnorm = din("final_norm", [1, D])
    c_ident = din("c_ident", [128, 128])
    c_mask = din("c_mask", [128, 128])
    c_rcos = din("c_rcos", [128, S])
    c_rsin = din("c_rsin", [128, S])
    c_mcos64 = din("c_mcos64", [64, S])
    c_msin64 = din("c_msin64", [64, S])
    c_mcos_tok = din("c_mcos_tok", [128, NT, 32])
    c_msin_tok = din("c_msin_tok", [128, NT, 32])
    c_ret = din("c_ret", [128, 8])
    out_d = nc.dram_tensor("out", [S, D], F32, kind="ExternalOutput").ap()
    dbg_outs = {}

    log_gamma = [float(np.log1p(-np.exp2(-5.0 - h))) for h in range(RET_H)]
    gC = [float(np.exp(lg * 128.0)) for lg in log_gamma]

    with contextlib.ExitStack() as es:
        ARENA_BYTES = 204 * 1024
        arena_t = es.enter_context(nc.sbuf_tensor("arena", [128, ARENA_BYTES // 4], F32))
        psum_t = es.enter_context(nc.psum_tensor("psum", [128, 8, 512], F32))
        Sc = Sched(nc, es)
        A = Arena(arena_t[:, :], ARENA_BYTES, Sc)

        def bank(b):
            return psum_t[:, b, :]

        def bank_bf(b):
            return psum_t[:, b, :].bitcast(BF16)

        def PS(b):
            return ("ps", b)

        def dump(name, ap, shape, dt, key):
            if name not in dbg:
                return
            d = nc.dram_tensor("dbg_" + name, list(shape), dt, kind="ExternalOutput").ap()
            dbg_outs[name] = d
            Sc.dma("sp", d, ap, reads=[key], writes=[("dbgout", name)], is_output=True)

        def mm_group(out, pairs, reads, writes):
            def fn(e, out=out, pairs=list(pairs)):
                n = len(pairs)
                ins = None
                for i, (l, r) in enumerate(pairs):
                    ins = e.matmul(out=out, lhsT=l, rhs=r, start=(i == 0), stop=(i == n - 1))
                return ins
            return Sc.op("pe", fn, reads=reads, writes=writes)

        def transposes(items, reads, writes, ident):
            def fn(e, items=list(items), ident=ident):
                ins = None
                for (o, i) in items:
                    ins = e.transpose(out=o, in_=i, identity=ident)
                return ins
            return Sc.op("pe", fn, reads=reads, writes=writes)

        ident_bf = A.alloc("ident_bf", (128,), BF16)
        ident_f = A.alloc("ident_f", (128,), F32)
        mask01 = A.alloc("mask01", (128,), BF16)
        cols = A.alloc("cols", (80,), F32)
        modc = A.alloc("modc", (48,), F32)
        A1 = A.alloc("A1", (8,), F32)
        A2 = A.alloc("A2", (8,), F32)
        cact = A.alloc("cact", (8,), F32)
        cret = A.alloc("cret", (8,), F32)
        stat = A.alloc("stat", (64,), F32)
        g1b = A.alloc("g1b", (D,), F32)
        g2b = A.alloc("g2b", (D,), F32)
        uT = A.alloc("uT", (8, S), BF16)
        R_G = A.alloc("gatedT", (8, S), BF16)
        R_A = A.alloc("attnT", (8, S), BF16)
        gatedT = R_G
        attnT = R_A
        TMP0 = A.mark()

        Sc.dma("pool", ident_bf, c_ident, writes=[("ident_bf",)])
        Sc.dma("sp", ident_f, c_ident, writes=[("ident_f",)])
        Sc.dma("pool", mask01, c_mask, writes=[("mask01",)])
        Sc.dma("sp", cret, c_ret, writes=[("cret",)])

        m0 = A.mark()
        rows_in = A.alloc("rows_in", (128,), F32)
        cact_rep = A.alloc("cact_rep", (8, 128), F32)
        wa = [A.alloc("wa", (8, 512), F32) for _ in range(2)]
        xt = [A.alloc("xt", (D,), F32) for _ in range(2)]
        xn = [A.alloc("xn", (D,), BF16) for _ in range(2)]
        junk = A.alloc("junk", (D,), BF16)

        Sc.dma("sp", rows_in[0:77, :], rows_d, writes=[("rows_in",)])
        Sc.dma("sp", g1b, b_ada[0, 2 * D:3 * D].partition_broadcast(128), writes=[("g1b",)])
        Sc.dma("sp", g2b, b_ada[0, 5 * D:6 * D].partition_broadcast(128), writes=[("g2b",)])

        transposes([(bank(2)[:, 0:77], rows_in[0:77, :])], reads=[("rows_in",), ("ident_f",)], writes=[PS(2)],
                   ident=ident_f[0:77, 0:77])
        Sc.op("dve", lambda e: e.tensor_copy(out=cols[:, 0:77], in_=bank(2)[:, 0:77]), reads=[PS(2)], writes=[("cols",)])
        Sc.op("act", lambda e: e.activation(out=cact, in_=cols[:, 69:77], func=AF.Silu), reads=[("cols",)], writes=[("cact",)])
        for c in range(8):
            Sc.op("dve", lambda e, c=c: e.tensor_copy(out=cact_rep[:, c, :], in_=cact[:, c:c + 1].to_broadcast([128, 128])),
                  reads=[("cact",)], writes=[("cact_rep", c)])

        wa_view = w_ada.rearrange("(c p) n -> p c n", p=128)
        blk_order = [0, 1, 2, 3, 6, 7, 8, 9, 4, 5, 10, 11]
        for bi, blk in enumerate(blk_order):
            wb = wa[bi % 2]
            Sc.dma("sp", wb, wa_view[:, :, blk * 512:(blk + 1) * 512], writes=[("wa", bi % 2)])
            if blk in (4, 5, 10, 11):
                gb = g1b if blk in (4, 5) else g2b
                gk = ("g1b",) if blk in (4, 5) else ("g2b",)
                half = blk % 2 if blk in (4, 5) else (blk - 10)
                pb = 3 + (bi % 2)
                mm_group(bank(pb), [(cact_rep[:, c, :], wb[:, c, :]) for c in range(8)],
                         reads=[("cact_rep",), ("wa", bi % 2)], writes=[PS(pb)])
                Sc.op("dve", lambda e, gb=gb, half=half, pb=pb: e.tensor_tensor(
                    out=gb[:, half * 512:(half + 1) * 512], in0=bank(pb), in1=gb[:, half * 512:(half + 1) * 512], op=ALU.add),
                    reads=[PS(pb), gk], writes=[gk])
            else:
                for j in range(4):
                    J = blk * 4 + j
                    mm_group(bank(2)[:, 128 + J:129 + J], [(wb[:, c, j * 128:(j + 1) * 128], cact[:, c:c + 1]) for c in range(8)],
                             reads=[("cact",), ("wa", bi % 2)], writes=[("ps", 2, "col", J)])
            if bi == 3:
                Sc.op("dve", lambda e: e.tensor_tensor(out=modc[:, 0:16], in0=bank(2)[:, 128:144], in1=cols[:, 0:16], op=ALU.add),
                      reads=[("ps", 2, "col"), ("cols",)], writes=[("modc", 0)])
                Sc.op("dve", lambda e: e.scalar_tensor_tensor(out=A1, in0=modc[:, 8:16], scalar=1.0, in1=cols[:, 48:56],
                                                               op0=ALU.add, op1=ALU.mult),
                      reads=[("modc", 0), ("cols",)], writes=[("A1",)])
            if bi == 7:
                Sc.op("dve", lambda e: e.tensor_tensor(out=modc[:, 24:40], in0=bank(2)[:, 152:168], in1=cols[:, 24:40], op=ALU.add),
                      reads=[("ps", 2, "col"), ("cols",)], writes=[("modc", 1)])
                Sc.op("dve", lambda e: e.scalar_tensor_tensor(out=A2, in0=modc[:, 32:40], scalar=1.0, in1=cols[:, 56:64],
                                                               op0=ALU.add, op1=ALU.mult),
                      reads=[("modc", 1), ("cols",)], writes=[("A2",)])
        dump("modc", modc, [128, 48], F32, ("modc",))
        dump("g1b", g1b, [128, D], F32, ("g1b",))

        def norm_A(t, src_ap, src_key):
            b2 = t % len(xn)
            junk_ = junk
            xn_ = xn[b2]
            Sc.op("act", lambda e: e.activation(out=junk_, in_=src_ap, func=AF.Square, accum_out=stat[:, t:t + 1]),
                  reads=[src_key], writes=[("junk",), ("stat", t)])
            Sc.op("act", lambda e: e.activation(out=stat[:, 16 + t:17 + t], in_=stat[:, t:t + 1], func=AF.Sqrt,
                                                scale=1.0 / D, bias=EPS),
                  reads=[("stat", t)], writes=[("stat", 16 + t)])
            Sc.op("dve", lambda e: e.reciprocal(out=stat[:, 32 + t:33 + t], in_=stat[:, 16 + t:17 + t]),
                  reads=[("stat", 16 + t)], writes=[("stat", 32 + t)])
            Sc.op("act", lambda e: e.activation(out=xn_, in_=src_ap, func=AF.Copy, scale=stat[:, 32 + t:33 + t]),
                  reads=[src_key, ("stat", 32 + t)], writes=[("xn", b2)])

        def norm_B(t, Acol, shcol, Akey, shkey, dstT, dst_name, pb):
            b2 = t % len(xn)
            xn_ = xn[b2]
            pT = bank_bf(pb).rearrange("p (a b) -> p a b", a=8)
            transposes([(pT[:, c, :], xn_[:, c * 128:(c + 1) * 128]) for c in range(8)],
                       reads=[("xn", b2), ("ident_bf",)], writes=[PS(pb)], ident=ident_bf)
            for c in range(8):
                Sc.op("dve", lambda e, c=c: e.tensor_scalar(out=dstT[:, c, t * 128:(t + 1) * 128], in0=pT[:, c, :],
                                                            scalar1=Acol[:, c:c + 1], scalar2=shcol[:, c:c + 1],
                                                            op0=ALU.mult, op1=ALU.add),
                      reads=[PS(pb), Akey, shkey], writes=[(dst_name, c, t)])

        Sc.dma("sp", xt[0], x[0:128, :], writes=[("xt", 0)])
        norm_A(0, xt[0], ("xt", 0))
        for t in range(NT):
            if t + 1 < NT:
                Sc.dma("sp", xt[(t + 1) % 2], x[(t + 1) * 128:(t + 2) * 128, :], writes=[("xt", (t + 1) % 2)])
                norm_A(t + 1, xt[(t + 1) % 2], ("xt", (t + 1) % 2))
            norm_B(t, A1, modc[:, 0:8], ("A1",), ("modc", 0), uT, "uT", pb=t % 2)
        dump("uT", uT, [128, 8, S], BF16, ("uT",))
        A.release(m0)


        m1 = A.mark()
        rcos = A.alloc("rcos", (S,), BF16)
        rsin = A.alloc("rsin", (S,), BF16)
        Sc.dma("pool", rcos, c_rcos, writes=[("rcos",)])
        Sc.dma("pool", rsin, c_rsin, writes=[("rsin",)])
        NW = 8
        wslot = [A.alloc("wslot", (8, 256), BF16) for _ in range(NW)]
        qT = A.alloc("qT", (2, S), BF16)
        kT = A.alloc("kT", (2, S), BF16)
        dummy_ = A.alloc("dummy_", (512,), F32) if os.environ.get("MK_DUMMY") else None
        v_s = A.alloc("v_s", (NT, 256), BF16)
        sgT = A.alloc("sgT", (2, S), BF16)
        Tst = A.alloc("Tst", (512,), F32)
        st_bf = [A.alloc("st_bf", (2, 256), BF16) for _ in range(2)]
        sm = [A.alloc("sm", (128,), BF16) for _ in range(2)]
        ktok = [A.alloc("ktok", (256,), BF16) for _ in range(2)]
        yn = [A.alloc("yn", (256,), BF16) for _ in range(2)]
        rt = [A.alloc("rt", (512,), F32) for _ in range(4)]
        st6 = [A.alloc("st6", (8,), F32) for _ in range(2)]
        mv = [A.alloc("mv", (8,), F32) for _ in range(2)]
        mhalf = A.alloc("mhalf", (8,), F32)
        Sc.op("pool", lambda e: e.memset(mhalf, -0.5), reads=[], writes=[("mhalf",)])
        w_in_v = w_in.rearrange("(c p) n -> p c n", p=128)
        wstate = {"n": 0}

        def load_w(col0):
            slot = wstate["n"] % NW
            wstate["n"] += 1
            Sc.dma("pool", wslot[slot], w_in_v[:, :, col0:col0 + 256], writes=[("wslot", slot)])
            return slot

        def head_slots(h):
            return [load_w(m * 1024 + h * 256) for m in range(4)]

        def proj_units(h, tg, slots):
            sq, sk, sv, sg = slots
            tsl = slice(tg * 512, (tg + 1) * 512)
            units = []

            def qk_mm(nm, sl, j):
                def f():
                    mm_group(bank(j), [(wslot[sl][:, c, j * 128:(j + 1) * 128], uT[:, c, tsl]) for c in range(8)],
                             reads=[("wslot", sl), ("uT",)], writes=[PS(j)])
                return f

            def qk_rope(nm, dst):
                def f():
                    P0, P1 = bank(0), bank(1)
                    Sc.op("dve", lambda e: e.tensor_tensor(out=rt[0], in0=P0, in1=rcos[:, tsl], op=ALU.mult),
                          reads=[PS(0), ("rcos",)], writes=[("rt", 0)])
                    Sc.op("dve", lambda e: e.tensor_tensor(out=rt[1], in0=P1, in1=rsin[:, tsl], op=ALU.mult),
                          reads=[PS(1), ("rsin",)], writes=[("rt", 1)])
                    Sc.op("dve", lambda e: e.tensor_tensor(out=rt[2], in0=P1, in1=rcos[:, tsl], op=ALU.mult),
                          reads=[PS(1), ("rcos",)], writes=[("rt", 2)])
                    Sc.op("dve", lambda e: e.tensor_tensor(out=rt[3], in0=P0, in1=rsin[:, tsl], op=ALU.mult),
                          reads=[PS(0), ("rsin",)], writes=[("rt", 3)])
                    Sc.op("pool", lambda e: e.tensor_tensor(out=dst[:, 0, tsl], in0=rt[0], in1=rt[1], op=ALU.subtract),
                          reads=[("rt", 0), ("rt", 1)], writes=[(nm + "T", 0, tg)])
                    Sc.op("pool", lambda e: e.tensor_tensor(out=dst[:, 1, tsl], in0=rt[2], in1=rt[3], op=ALU.add),
                          reads=[("rt", 2), ("rt", 3)], writes=[(nm + "T", 1, tg)])
                return f

            def qk_units(nm, sl, dst):
                m1_ = qk_mm(nm, sl, 1)
                r_ = qk_rope(nm, dst)
                return [qk_mm(nm, sl, 0), (lambda: (m1_(), r_()))]

            def g_unit(j):
                def f():
                    mm_group(bank(j), [(wslot[sg][:, c, j * 128:(j + 1) * 128], uT[:, c, tsl]) for c in range(8)],
                             reads=[("wslot", sg), ("uT",)], writes=[PS(j)])
                    Sc.op("act", lambda e: e.activation(out=sgT[:, j, tsl], in_=bank(j), func=AF.Silu),
                          reads=[PS(j)], writes=[("sgT", j, tg)])
                return f

            def v_unit(t):
                def f():
                    rg_ = bank(2)[:, 0:256]
                    mm_group(rg_, [(uT[:, c, t * 128:(t + 1) * 128], wslot[sv][:, c, :]) for c in range(8)],
                             reads=[("wslot", sv), ("uT",)], writes=[PS(2)])
                    Sc.op("act", lambda e: e.activation(out=v_s[:, t, :], in_=rg_, func=AF.Copy, scale=cret[:, h:h + 1]),
                          reads=[PS(2), ("cret",)], writes=[("v_s", t)])
                return f

            g0_, g1_ = g_unit(0), g_unit(1)
            qu_ = qk_units("q", sq, qT)
            ku_ = qk_units("k", sk, kT)
            units += [qu_[0], v_unit(4 * tg), qu_[1], v_unit(4 * tg + 1), ku_[0], v_unit(4 * tg + 2), ku_[1], v_unit(4 * tg + 3)]
            units.append(lambda: (g0_(), g1_()))
            return units

        def ret_tail(h, n):
            p = n % 2
            tg = n // 4
            nsl = slice(n * 128, (n + 1) * 128)
            yT = bank_bf(7)[:, p * 256:(p + 1) * 256].rearrange("p (c t) -> p c t", c=2)
            transposes([(yT[:, c, :], yn[p][:, c * 128:(c + 1) * 128]) for c in range(2)],
                       reads=[("yn", p), ("ident_bf",)], writes=[("ps", 7, p)], ident=ident_bf)
            Sc.op("dve", lambda e: e.tensor_tensor(out=gatedT[:, 2 * h:2 * h + 2, nsl], in0=yT, in1=sgT[:, :, nsl], op=ALU.mult),
                  reads=[("ps", 7, p), ("sgT", 0, tg), ("sgT", 1, tg)], writes=[("gatedT", h, n)])

        def recur_units(h, n):
            p = n % 2
            tg = n // 4
            nsl = slice(n * 128, (n + 1) * 128)
            qk_keys = [("kT", 0, tg), ("kT", 1, tg), ("qT", 0, tg), ("qT", 1, tg)]
            scb = bank(4)[:, p * 128:(p + 1) * 128]
            ybk = 3 if p == 0 else 5
            yb = bank(ybk)[:, 0:256]

            def ua():
                mm_group(scb, [(kT[:, c, nsl], qT[:, c, nsl]) for c in range(2)], reads=qk_keys, writes=[("ps", 4, "s", p)])
                Sc.op("dve", lambda e: e.tensor_tensor(out=sm[p], in0=scb, in1=mask01, op=ALU.mult),
                      reads=[("ps", 4, "s", p), ("mask01",)], writes=[("sm", p)])
                if n < NT - 1:
                    kt = bank_bf(6)[:, 0:256]
                    transposes([(kt[:, c * 128:(c + 1) * 128], kT[:, c, nsl]) for c in range(2)],
                               reads=qk_keys[0:2] + [("ident_bf",)], writes=[PS(6)], ident=ident_bf)
                    Sc.op("act", lambda e: e.activation(out=ktok[p], in_=kt, func=AF.Copy),
                          reads=[PS(6)], writes=[("ktok", p)])

            def ukv():
                if n < NT - 1:
                    for c in range(2):
                        mm_group(bank(6)[:, c * 256:(c + 1) * 256], [(ktok[p][:, c * 128:(c + 1) * 128], v_s[:, n, :])],
                                 reads=[("ktok", p), ("v_s", n)], writes=[("ps", 6, c)])
                    if n == 0:
                        Sc.op("dve", lambda e: e.tensor_copy(out=Tst, in_=bank(6)), reads=[("ps", 6)], writes=[("Tst",)])
                    else:
                        Sc.op("dve", lambda e: e.scalar_tensor_tensor(out=Tst, in0=Tst, scalar=gC[h], in1=bank(6),
                                                                       op0=ALU.mult, op1=ALU.add),
                              reads=[("ps", 6), ("Tst",)], writes=[("Tst",)])
                    Sc.op("act", lambda e: e.activation(out=st_bf[p].rearrange("p a b -> p (a b)"), in_=Tst, func=AF.Copy, scale=gC[h]),
                          reads=[("Tst",)], writes=[("st_bf", p)])

            def uy():
                pairs = [(sm[p], v_s[:, n, :])]
                rds = [("sm", p), ("v_s", n)]
                if n > 0:
                    pairs += [(qT[:, c, nsl], st_bf[1 - p][:, c, :]) for c in range(2)]
                    rds += qk_keys[2:4] + [("st_bf", 1 - p)]
                mm_group(yb, pairs, reads=rds, writes=[PS(ybk)])

            def uc():
                Sc.op("dve", lambda e: e.bn_stats(out=st6[p][:, 0:6], in_=yb), reads=[PS(ybk)], writes=[("st6", p)])
                Sc.op("dve", lambda e: e.bn_aggr(out=mv[p][:, 0:2], in_=st6[p][:, 0:6]), reads=[("st6", p)], writes=[("mv", p, 0)])
                Sc.op("dve", lambda e: e.tensor_scalar(out=mv[p][:, 2:3], in0=mv[p][:, 1:2], scalar1=cret[:, 4 + h:5 + h], scalar2=None,
                                                       op0=ALU.add),
                      reads=[("mv", p, 0), ("cret",)], writes=[("mv", p, 2)])
                Sc.op("pool", lambda e: e.tensor_tensor(out=mv[p][:, 3:4], in0=mv[p][:, 2:3], in1=mhalf[:, 0:1], op=ALU.pow),
                      reads=[("mv", p, 2), ("mhalf",)], writes=[("mv", p, 3)])
                Sc.op("dve", lambda e: e.tensor_scalar(out=yn[p], in0=yb, scalar1=mv[p][:, 0:1], scalar2=mv[p][:, 3:4],
                                                       op0=ALU.subtract, op1=ALU.mult),
                      reads=[PS(ybk), ("mv", p)], writes=[("yn", p)])
                if n > 0:
                    ret_tail(h, n - 1)
                if n == NT - 1:
                    ret_tail(h, n)
            return dict(a=ua, kv=ukv, y=uy, c=uc)

        all_slots = {0: head_slots(0)}
        seq = [(h, tg) for h in range(RET_H) for tg in range(4)][:int(os.environ.get('MK_R', 16))]
        for u in proj_units(0, 0, all_slots[0])[:int(os.environ.get('MK_U', 99))]:
            u()
        if os.environ.get('MK_NOPF', '') == '':
            all_slots[1] = head_slots(1)
        for i, (h, tg) in enumerate(seq):
            if tg == 0 and h >= 1 and h + 1 < RET_H:
                all_slots[h + 1] = head_slots(h + 1)
            pu = []
            if i + 1 < len(seq):
                h2, tg2 = seq[i + 1]
                pu = proj_units(h2, tg2, all_slots[h2])
            ru = []
            for n in range(4 * tg, 4 * tg + 4):
                u_ = recur_units(h, n)
                ru += [u_["a"], u_["kv"], (lambda u_=u_: (u_["y"](), u_["c"]()))]
            k = 0
            for j, r in enumerate(ru):
                r()
                if j % 3 != 0 and k < len(pu):
                    pu[k]()
                    k += 1
            while k < len(pu):
                pu[k]()
                k += 1
        dump("gatedT", gatedT, [128, 8, S], BF16, ("gatedT",))
        A.release(m1)


        if os.environ.get("MK_STOP", "") == "1":
            A.release(m1)
            Sc.finish()
            block = es.enter_context(nc.Block())
            Sc.emit(block)
            return nc, dbg_outs
        m2 = A.mark()
        SCALE = float(192.0 ** -0.5)
        wuq = A.alloc("wuq", (3, 1536), BF16)
        wpad = [A.alloc("wpad", (3, 128), BF16) for _ in range(2)]
        wukv = A.alloc("wukv", (2, 2048), BF16)
        mcs = A.alloc("mcs", (S,), BF16)
        cqnT = A.alloc("cqnT", (3, S), BF16)
        ckvnT = A.alloc("ckvnT", (2, S), BF16)
        kpeT = A.alloc("kpeT", (S,), BF16)
        v_aug = A.alloc("v_aug", (NT, 4, 130), BF16)
        Sc.dma("pool", wuq, w_uq.rearrange("(c p) n -> p c n", p=128), writes=[("wuq",)])
        Sc.dma("pool", wukv, w_ukv.rearrange("(c p) n -> p c n", p=128), writes=[("wukv",)])
        wuq4 = wuq.rearrange("p c (h k) -> p c h k", h=8)
        Sc.dma("pool", mcs[0:64, :], c_mcos64, writes=[("mcs", 0)])
        Sc.dma("pool", mcs[64:128, :], c_msin64, writes=[("mcs", 1)])

        mA = A.mark()
        wl = A.alloc("wl", (8, 704), BF16)
        mcos_t = A.alloc("mcos_t", (NT, 32), F32)
        msin_t = A.alloc("msin_t", (NT, 32), F32)
        cqn = [A.alloc("cqn", (384,), BF16) for _ in range(2)]
        ckvn = [A.alloc("ckvn", (256,), BF16) for _ in range(2)]
        kper = [A.alloc("kper", (128,), BF16) for _ in range(2)]
        lst = [A.alloc("lst", (8,), F32) for _ in range(2)]
        kr = [A.alloc("kr", (4, 32), F32) for _ in range(2)]
        ljunk = A.alloc("ljunk", (384,), BF16)
        for i in range(2):
            Sc.op("dve", lambda e, i=i: e.memset(kper[i], 0.0), reads=[], writes=[("kper", i)])
        Sc.dma("pool", wl, w_in_v[:, :, 4096:4800], writes=[("wl",)])
        Sc.dma("sp", mcos_t, c_mcos_tok, writes=[("mcos_t",)])
        Sc.dma("sp", msin_t, c_msin_tok, writes=[("msin_t",)])
        _stop = os.environ.get("MK_STOP", "")
        for t in range(0 if _stop == "2pre" else int(os.environ.get("MK_NT", NT))):
            p = t % 2
            b0 = 4 * p
            tsl = slice(t * 128, (t + 1) * 128)
            mm_group(bank(b0)[:, 0:384], [(uT[:, c, tsl], wl[:, c, 0:384]) for c in range(8)],
                     reads=[("uT",), ("wl",)], writes=[PS(b0)])
            mm_group(bank(b0 + 1)[:, 0:320], [(uT[:, c, tsl], wl[:, c, 384:704]) for c in range(8)],
                     reads=[("uT",), ("wl",)], writes=[PS(b0 + 1)])
            cq_ps = bank(b0)[:, 0:384]
            ckv_ps = bank(b0 + 1)[:, 0:256]
            kx1 = bank(b0 + 1)[:, 256:288]
            kx2 = bank(b0 + 1)[:, 288:320]
            _A = os.environ.get("MK_A", "sq,rope,tr,ev")
            if "sq" not in _A:
                continue
            Sc.op("act", lambda e, p=p, cq_ps=cq_ps: e.activation(out=ljunk, in_=cq_ps, func=AF.Square, accum_out=lst[p][:, 0:1]),
                  reads=[PS(b0)], writes=[("ljunk",), ("lst", p, 0)])
            Sc.op("act", lambda e, p=p, ckv_ps=ckv_ps: e.activation(out=ljunk[:, 0:256], in_=ckv_ps, func=AF.Square, accum_out=lst[p][:, 1:2]),
                  reads=[PS(b0 + 1)], writes=[("ljunk",), ("lst", p, 1)])
            Sc.op("act", lambda e, p=p: e.activation(out=lst[p][:, 2:3], in_=lst[p][:, 0:1], func=AF.Sqrt, scale=1.0 / 384, bias=EPS),
                  reads=[("lst", p, 0)], writes=[("lst", p, 2)])
            Sc.op("act", lambda e, p=p: e.activation(out=lst[p][:, 3:4], in_=lst[p][:, 1:2], func=AF.Sqrt, scale=1.0 / 256, bias=EPS),
                  reads=[("lst", p, 1)], writes=[("lst", p, 3)])
            Sc.op("dve", lambda e, p=p: e.reciprocal(out=lst[p][:, 4:6], in_=lst[p][:, 2:4]),
                  reads=[("lst", p, 2), ("lst", p, 3)], writes=[("lst", p, 4)])
            Sc.op("act", lambda e, p=p, cq_ps=cq_ps: e.activation(out=cqn[p], in_=cq_ps, func=AF.Copy, scale=lst[p][:, 4:5]),
                  reads=[PS(b0), ("lst", p, 4)], writes=[("cqn", p)])
            Sc.op("act", lambda e, p=p, ckv_ps=ckv_ps: e.activation(out=ckvn[p], in_=ckv_ps, func=AF.Copy, scale=lst[p][:, 4 + 1:6]),
                  reads=[PS(b0 + 1), ("lst", p, 4)], writes=[("ckvn", p)])
            if "rope" not in _A:
                continue
            cs = mcos_t[:, t, :]
            sn = msin_t[:, t, :]
            Sc.op("dve", lambda e, p=p, kx1=kx1, cs=cs: e.tensor_tensor(out=kr[p][:, 0, :], in0=kx1, in1=cs, op=ALU.mult),
                  reads=[PS(b0 + 1), ("mcos_t",)], writes=[("kr", p, 0)])
            Sc.op("dve", lambda e, p=p, kx2=kx2, sn=sn: e.tensor_tensor(out=kr[p][:, 1, :], in0=kx2, in1=sn, op=ALU.mult),
                  reads=[PS(b0 + 1), ("msin_t",)], writes=[("kr", p, 1)])
            Sc.op("dve", lambda e, p=p, kx2=kx2, cs=cs: e.tensor_tensor(out=kr[p][:, 2, :], in0=kx2, in1=cs, op=ALU.mult),
                  reads=[PS(b0 + 1), ("mcos_t",)], writes=[("kr", p, 2)])
            Sc.op("dve", lambda e, p=p, kx1=kx1, sn=sn: e.tensor_tensor(out=kr[p][:, 3, :], in0=kx1, in1=sn, op=ALU.mult),
                  reads=[PS(b0 + 1), ("msin_t",)], writes=[("kr", p, 3)])
            Sc.op("dve", lambda e, p=p: e.tensor_tensor(out=kper[p][:, 0:32], in0=kr[p][:, 0, :], in1=kr[p][:, 1, :], op=ALU.subtract),
                  reads=[("kr", p)], writes=[("kper", p, 0)])
            Sc.op("dve", lambda e, p=p: e.tensor_tensor(out=kper[p][:, 64:96], in0=kr[p][:, 0, :], in1=kr[p][:, 1, :], op=ALU.subtract),
                  reads=[("kr", p)], writes=[("kper", p, 2)])
            Sc.op("dve", lambda e, p=p: e.tensor_tensor(out=kper[p][:, 96:128], in0=kr[p][:, 2, :], in1=kr[p][:, 3, :], op=ALU.add),
                  reads=[("kr", p)], writes=[("kper", p, 3)])
            Sc.op("dve", lambda e, p=p: e.tensor_tensor(out=kper[p][:, 32:64], in0=kr[p][:, 2, :], in1=kr[p][:, 3, :], op=ALU.add),
                  reads=[("kr", p)], writes=[("kper", p, 1)])
            if "tr" not in _A:
                continue
            pT = bank_bf(b0 + 2)[:, 0:768].rearrange("p (a b) -> p a b", a=6)
            items = [(pT[:, c, :], cqn[p][:, c * 128:(c + 1) * 128]) for c in range(3)]
            items += [(pT[:, 3 + c, :], ckvn[p][:, c * 128:(c + 1) * 128]) for c in range(2)]
            items += [(pT[:, 5, :], kper[p])]
            transposes(items, reads=[("cqn", p), ("ckvn", p), ("kper", p), ("ident_bf",)], writes=[PS(b0 + 2)], ident=ident_bf)
            if "ev" not in _A:
                continue
            for c in range(3):
                Sc.op("dve", lambda e, c=c, pT=pT, tsl=tsl: e.tensor_scalar(out=cqnT[:, c, tsl], in0=pT[:, c, :], scalar1=cols[:, 64 + c:65 + c],
                                                                           scalar2=None, op0=ALU.mult),
                      reads=[PS(b0 + 2), ("cols",)], writes=[("cqnT", c, t)])
            for c in range(2):
                Sc.op("dve", lambda e, c=c, pT=pT, tsl=tsl: e.tensor_scalar(out=ckvnT[:, c, tsl], in0=pT[:, 3 + c, :], scalar1=cols[:, 67 + c:68 + c],
                                                                           scalar2=None, op0=ALU.mult),
                      reads=[PS(b0 + 2), ("cols",)], writes=[("ckvnT", c, t)])
            if "nokpe" in _A:
                continue
            Sc.op("dve", lambda e, pT=pT, tsl=tsl: e.tensor_copy(out=kpeT[:, tsl], in_=pT[:, 5, :]),
                  reads=[PS(b0 + 2)], writes=[("kpeT", t)])
        dump("cqnT", cqnT, [128, 3, S], BF16, ("cqnT",))
        dump("ckvnT", ckvnT, [128, 2, S], BF16, ("ckvnT",))
        A.release(mA)

        _stop = os.environ.get("MK_STOP", "")
        qn = A.alloc("qn", (S,), BF16)
        kn = A.alloc("kn", (S,), BF16)
        qpe = A.alloc("qpe", (S,), BF16)
        qr = [A.alloc("qr", (512,), F32) for _ in range(2)]
        pTs = [A.alloc("pTs", (512,), BF16) for _ in range(6)]
        accP = [A.alloc("accP", (512,), F32) for _ in range(4)]
        ones_f = A.alloc("ones_f", (128,), F32)
        maskb = A.alloc("maskb", (128,), BF16)
        zeros_bf = A.alloc("zeros_bf", (512,), BF16)
        Sc.op("pool", lambda e: e.memset(zeros_bf, 0.0), reads=[], writes=[("zeros_bf",)])
        Sc.op("dve", lambda e: e.tensor_scalar(out=maskb, in0=mask01, scalar1=-1.0, scalar2=30000.0, op0=ALU.add, op1=ALU.mult),
              reads=[("mask01",)], writes=[("maskb",)])
        Sc.op("dve", lambda e: e.memset(ones_f, 1.0), reads=[], writes=[("ones_f",)])
        wukv4 = wukv.rearrange("p c (h k) -> p c h k", h=8)
        cnt_st = {"s": 0, "o": 0}

        def build_wpad(hh):
            wq_ = wpad[hh % 2]
            for c in range(3):
                Sc.op("act", lambda e, c=c: e.activation(out=wq_[:, c, 0:64], in_=wuq4[:, c, hh, 128:192], func=AF.Copy),
                      reads=[("wuq",)], writes=[("wpad", hh % 2, c, 0)])
                Sc.op("act", lambda e, c=c: e.activation(out=wq_[:, c, 64:96], in_=wuq4[:, c, hh, 160:192], func=AF.Copy, scale=-1.0),
                      reads=[("wuq",)], writes=[("wpad", hh % 2, c, 1)])
                Sc.op("act", lambda e, c=c: e.activation(out=wq_[:, c, 96:128], in_=wuq4[:, c, hh, 128:160], func=AF.Copy),
                      reads=[("wuq",)], writes=[("wpad", hh % 2, c, 2)])
        for hp in range(0 if _stop in ("2A", "2pre") else 2):
            Sc.op("dve", lambda e: e.memset(v_aug[:, :, :, 128:130], 1.0), reads=[], writes=[("v_aug",)])
            for t in range(NT):
                pb = t % 2
                tsl = slice(t * 128, (t + 1) * 128)
                mm_group(bank(pb), [(ckvnT[:, c, tsl], wukv4[:, c, 4 * hp:4 * hp + 4, 128:256]) for c in range(2)],
                         reads=[("ckvnT",), ("wukv",)], writes=[PS(pb)])
                Sc.op("act", lambda e, t=t, pb=pb: e.activation(out=v_aug[:, t, :, 0:128], in_=bank(pb).rearrange("p (h k) -> p h k", h=4),
                                                                 func=AF.Copy),
                      reads=[PS(pb)], writes=[("v_aug", t)])
            for hl in range(0 if _stop == "2V" else 4):
                h = 4 * hp + hl
                for tg in range(4):
                    tsl = slice(tg * 512, (tg + 1) * 512)
                    pb = 7 * (tg % 2)
                    mm_group(bank(pb), [(wuq[:, c, h * 192:h * 192 + 128], cqnT[:, c, tsl]) for c in range(3)],
                             reads=[("wuq",), ("cqnT",)], writes=[PS(pb)])
                    Sc.op("act", lambda e, pb=pb, tsl=tsl: e.activation(out=qn[:, tsl], in_=bank(pb), func=AF.Copy, scale=SCALE),
                          reads=[PS(pb)], writes=[("qn", tg)])
                for tg in range(4):
                    tsl = slice(tg * 512, (tg + 1) * 512)
                    pb = 7 * (tg % 2)
                    mm_group(bank(pb), [(wukv[:, c, h * 256:h * 256 + 128], ckvnT[:, c, tsl]) for c in range(2)],
                             reads=[("wukv",), ("ckvnT",)], writes=[PS(pb)])
                    Sc.op("act", lambda e, pb=pb, tsl=tsl: e.activation(out=kn[:, tsl], in_=bank(pb), func=AF.Copy),
                          reads=[PS(pb)], writes=[("kn", tg)])
                wp_ = wpad[h % 2]
                if h == 0:
                    build_wpad(0)
                for tg in range(4):
                    tsl = slice(tg * 512, (tg + 1) * 512)
                    pb = 7 * (tg % 2)
                    mm_group(bank(pb), [(wp_[:, c, :], cqnT[:, c, tsl]) for c in range(3)],
                             reads=[("wpad", h % 2), ("cqnT",)], writes=[PS(pb)])
                    Sc.op("dve", lambda e, tsl=tsl, pb=pb: e.tensor_tensor(out=qpe[:, tsl], in0=bank(pb), in1=mcs[:, tsl], op=ALU.mult),
                          reads=[PS(pb), ("mcs",)], writes=[("qpe", tg)])
                if h + 1 < MLA_H:
                    build_wpad(h + 1)
                its = [(G, jb) for G in range(0 if _stop == "2P" else 4) for jb in range(4 * G + 4)]
                info = []
                for (G, jb) in its:
                    td = jb - 4 * G
                    q0 = max(td, 0) * 128
                    k3 = cnt_st["s"] % 6
                    sb_ = 1 + cnt_st["s"] % 3
                    cnt_st["s"] += 1
                    info.append(dict(G=G, jb=jb, td=td, q0=q0, N=512 - q0, k3=k3, sb=sb_))

                def emit_st(it):
                    ksl = slice(it["jb"] * 128, (it["jb"] + 1) * 128)
                    qsl = slice(it["G"] * 512 + it["q0"], (it["G"] + 1) * 512)
                    sbk, N_ = it["sb"], it["N"]
                    diag = it["td"] >= 0

                    def stf(e):
                        e.matmul(out=bank(sbk)[:, 0:N_], lhsT=kn[:, ksl], rhs=qn[:, qsl], start=True, stop=False)
                        ins = e.matmul(out=bank(sbk)[:, 0:N_], lhsT=kpeT[:, ksl], rhs=qpe[:, qsl], start=False, stop=not diag)
                        if diag:
                            ins = e.matmul(out=bank(sbk)[:, 0:128], lhsT=ident_bf, rhs=maskb, start=False, stop=True)
                        return ins
                    Sc.op("pe", stf, reads=[("kn",), ("qn",), ("kpeT",), ("qpe",), ("ident_bf",), ("maskb",)], writes=[PS(sbk)])

                def emit_exp(it):
                    k3, sb, N = it["k3"], it["sb"], it["N"]
                    Sc.op("act", lambda e: e.activation(out=pTs[k3][:, 0:N], in_=bank(sb)[:, 0:N], func=AF.Exp),
                          reads=[PS(sb)], writes=[("pTs", k3)])

                def emit_pv(it, hl=hl):
                    k3, N, G, jb, q0 = it["k3"], it["N"], it["G"], it["jb"], it["q0"]
                    ob = 4 + G % 2
                    which = jb % 2
                    ai = 2 * (G % 2) + which
                    ap_ = accP[ai]
                    en_ = "pool" if which == 0 else "dve"

                    def pv(e):
                        return e.matmul(out=bank(ob)[:, q0:512], lhsT=v_aug[:, jb, hl, 0:128], rhs=pTs[k3][:, 0:N],
                                        start=(jb == 0), stop=(jb == 4 * G + 3))
                    Sc.op("pe", pv, reads=[("pTs", k3), ("v_aug",)], writes=[PS(ob)])
                    if jb < 2:
                        if q0 > 0:
                            Sc.op(en_, lambda e: e.memset(ap_[:, 0:q0], 0.0), reads=[], writes=[("accP", ai)])
                        if en_ == "pool":
                            Sc.op(en_, lambda e: e.tensor_tensor(out=ap_[:, q0:512], in0=pTs[k3][:, 0:N], in1=zeros_bf[:, 0:N], op=ALU.add),
                                  reads=[("pTs", k3), ("zeros_bf",)], writes=[("accP", ai)])
                        else:
                            Sc.op(en_, lambda e: e.tensor_copy(out=ap_[:, q0:512], in_=pTs[k3][:, 0:N]), reads=[("pTs", k3)], writes=[("accP", ai)])
                    else:
                        Sc.op(en_, lambda e: e.tensor_tensor(out=ap_[:, q0:512], in0=ap_[:, q0:512], in1=pTs[k3][:, 0:N], op=ALU.add),
                              reads=[("pTs", k3), ("accP", ai)], writes=[("accP", ai)])

                def fin_steps(G, h=h):
                    ob = 4 + G % 2

                    def s0():
                        mm_group(bank(6), [(ones_f, accP[2 * (G % 2)]), (ones_f, accP[2 * (G % 2) + 1])],
                                 reads=[("ones_f",), ("accP", 2 * (G % 2)), ("accP", 2 * (G % 2) + 1)], writes=[PS(6)])

                    def piece(j):
                        def f():
                            cs_ = slice(j * 128, (j + 1) * 128)
                            Sc.op("dve", lambda e: e.reciprocal(out=qr[0][:, cs_], in_=bank(6)[:, cs_]), reads=[PS(6)], writes=[("qr", 0, j)])
                            Sc.op("dve", lambda e: e.tensor_tensor(out=attnT[:, h, G * 512 + j * 128:G * 512 + (j + 1) * 128],
                                                                   in0=bank(ob)[:, cs_], in1=qr[0][:, cs_], op=ALU.mult),
                                  reads=[PS(ob), ("qr", 0, j)], writes=[("attnT", h, G, j)])
                        return f
                    return [s0] + [piece(j) for j in range(4)]

                pend = []
                for j in range(min(2, len(info))):
                    emit_st(info[j])
                for i, it in enumerate(info):
                    if i + 2 < len(info):
                        emit_st(info[i + 2])
                    emit_exp(it)
                    emit_pv(it)
                    if pend:
                        pend.pop(0)()
                    if it["jb"] == 4 * it["G"] + 3:
                        pend += [(lambda: None), (lambda: None)] + fin_steps(it["G"])
                while pend:
                    pend.pop(0)()
        dump("attnT", attnT, [128, 8, S], BF16, ("attnT",))
        A.release(m2)


        dump("gT2", gatedT, [128, 8, S], BF16, ("gatedT",))
        dump("aT2", attnT, [128, 8, S], BF16, ("attnT",))
        dump("uT2", uT, [128, 8, S], BF16, ("uT",))
        m3 = A.mark()
        mergedT = A.alloc("mergedT", (8, S), BF16)
        wout = A.alloc("wout", (8, D), BF16)
        Sc.dma("pool", wout, w_out.rearrange("(c p) n -> p c n", p=128), writes=[("wout",)])
        m3a = A.mark()
        wm = [[A.alloc("wm", (8, 128), BF16) for _ in range(4)] for _ in range(2)]
        sgA = [A.alloc("sgA", (512,), BF16) for _ in range(2)]
        sgB = [A.alloc("sgB", (512,), BF16) for _ in range(2)]
        mt1 = [A.alloc("mt1", (512,), F32) for _ in range(2)]
        mt2 = [A.alloc("mt2", (512,), F32) for _ in range(2)]
        w_ro_v = w_ret_o.rearrange("(c p) n -> p c n", p=128)
        w_mo_v = w_mla_o.rearrange("(c p) n -> p c n", p=128)

        def load_wm(fc):
            ws = fc % 2
            srcs = [w_ro_v[:, :, fc * 128:(fc + 1) * 128], w_mo_v[:, :, fc * 128:(fc + 1) * 128],
                    w_in_v[:, :, 4800 + fc * 128:4800 + (fc + 1) * 128], w_in_v[:, :, 5824 + fc * 128:5824 + (fc + 1) * 128]]
            for i in range(4):
                Sc.dma("pool", wm[ws][i], srcs[i], writes=[("wm", ws, i)])

        load_wm(0)
        for fc in range(8):
            ws = fc % 2
            if fc + 1 < 8:
                load_wm(fc + 1)
            for tg in range(4):
                it = fc * 4 + tg
                p = it % 2
                bs = p * 4
                tsl = slice(tg * 512, (tg + 1) * 512)
                srcT = [(gatedT, ("gatedT",)), (attnT, ("attnT",)), (uT, ("uT",)), (uT, ("uT",))]
                for i in range(4):
                    mm_group(bank(bs + i), [(wm[ws][i][:, c, :], srcT[i][0][:, c, tsl]) for c in range(8)],
                             reads=[("wm", ws, i), srcT[i][1]], writes=[PS(bs + i)])
                Sc.op("act", lambda e, p=p, bs=bs: e.activation(out=sgA[p], in_=bank(bs + 2), func=AF.Sigmoid),
                      reads=[PS(bs + 2)], writes=[("sgA", p)])
                Sc.op("act", lambda e, p=p, bs=bs: e.activation(out=sgB[p], in_=bank(bs + 3), func=AF.Sigmoid),
                      reads=[PS(bs + 3)], writes=[("sgB", p)])
                Sc.op("dve", lambda e, p=p, bs=bs: e.tensor_tensor(out=mt1[p], in0=bank(bs), in1=sgA[p], op=ALU.mult),
                      reads=[PS(bs), ("sgA", p)], writes=[("mt1", p)])
                Sc.op("dve", lambda e, p=p, bs=bs: e.tensor_tensor(out=mt2[p], in0=bank(bs + 1), in1=sgB[p], op=ALU.mult),
                      reads=[PS(bs + 1), ("sgB", p)], writes=[("mt2", p)])
                Sc.op("dve", lambda e, p=p, fc=fc, tsl=tsl: e.tensor_tensor(out=mergedT[:, fc, tsl], in0=mt1[p], in1=mt2[p], op=ALU.add),
                      reads=[("mt1", p), ("mt2", p)], writes=[("mergedT", fc, tg)])
        dump("mergedT", mergedT, [128, 8, S], BF16, ("mergedT",))
        dump("sgA1", sgA[1], [128, 512], BF16, ("sgA", 1))
        dump("mt11", mt1[1], [128, 512], F32, ("mt1", 1))
        dump("mt21", mt2[1], [128, 512], F32, ("mt2", 1))
        dump("wm1", wm[1][0], [128, 8, 128], BF16, ("wm", 1, 0))
        dump("gT3", gatedT, [128, 8, S], BF16, ("gatedT",))
        dump("aT3", attnT, [128, 8, S], BF16, ("attnT",))
        dump("uT3", uT, [128, 8, S], BF16, ("uT",))
        A.release(m3a)

        if os.environ.get("MK_STOP", "") == "3a":
            raise_stop = True
        else:
            raise_stop = False
        offG = A.offs["gatedT"]
        assert A.offs["attnT"] == offG + 32768
        acc = arena_t[:, offG // 4:offG // 4 + NT * D].rearrange("p (t d) -> p t d", t=NT)
        Sc.alias("acc", "gatedT")
        Sc.alias("acc", "attnT")
        xt = [A.alloc("xt", (D,), F32) for _ in range(3)]
        xn = [A.alloc("xn", (D,), BF16) for _ in range(3)]
        junk = A.alloc("junk", (D,), BF16)
        mtmp = [A.alloc("mtmp", (D,), F32) for _ in range(3)]
        def pre3b(t):
            p = t % 3
            tsl = slice(t * 128, (t + 1) * 128)
            Sc.dma("sp", xt[p], x[t * 128:(t + 1) * 128, :], writes=[("xt", p)])
            for half in range(2):
                hb = (t % 2) * 2 + half
                hs = slice(half * 512, (half + 1) * 512)
                mm_group(bank(hb), [(mergedT[:, c, tsl], wout[:, c, hs]) for c in range(8)],
                         reads=[("mergedT",), ("wout",)], writes=[PS(hb)])
                Sc.op("dve", lambda e, p=p, hb=hb, hs=hs: e.tensor_tensor(out=mtmp[p][:, hs], in0=bank(hb), in1=g1b[:, hs], op=ALU.mult),
                      reads=[PS(hb), ("g1b",)], writes=[("mtmp", p, half)])
                Sc.op("dve", lambda e, p=p, t=t, hs=hs: e.tensor_tensor(out=acc[:, t, hs], in0=mtmp[p][:, hs], in1=xt[p][:, hs], op=ALU.add),
                      reads=[("mtmp", p, half), ("xt", p)], writes=[("acc", t, half)])

        if not raise_stop:
            for t0_ in range(2):
                pre3b(t0_)
                norm_A(t0_, acc[:, t0_, :], ("acc", t0_))
            for t in range(NT):
                if t + 2 < NT:
                    pre3b(t + 2)
                    norm_A(t + 2, acc[:, t + 2, :], ("acc", t + 2))
                norm_B(t, A2, modc[:, 24:32], ("A2",), ("modc", 1), uT, "uT", pb=4 + t % 2)
        dump("h1", acc, [128, NT, D], F32, ("acc",))
        dump("u2T", uT, [128, 8, S], BF16, ("uT",))
        A.release(m3)

        m4 = A.mark()
        wr = A.alloc("wr", (8, 36), BF16)
        brt = A.alloc("brt", (36,), F32)
        gate = A.alloc("gate", (NT, 32), F32)
        hid = [A.alloc("hid", (2, S), BF16) for _ in range(2)]
        sa = [A.alloc("sa", (512,), BF16) for _ in range(2)]
        NWS = 3
        w1e = [A.alloc("w1e", (8, 256), BF16) for _ in range(NWS)]
        w3e = [A.alloc("w3e", (8, 256), BF16) for _ in range(NWS)]
        w2e = [A.alloc("w2e", (2, D), BF16) for _ in range(NWS)]
        w2s = [A.alloc("w2s", (2, D), F32) for _ in range(2)]
        junk = A.alloc("junk", (D,), BF16)
        fnb = A.alloc("fnb", (D,), F32)
        Sc.dma("sp", fnb, fnorm[0, :].partition_broadcast(128), writes=[("fnb",)])
        Sc.dma("pool", wr[:, :, 0:4], w_grp.rearrange("(c p) n -> p c n", p=128), writes=[("wr", 0)])
        Sc.dma("pool", wr[:, :, 4:36], w_exp.rearrange("(c p) n -> p c n", p=128), writes=[("wr", 1)])
        Sc.dma("sp", brt, b_rt[0, :].partition_broadcast(128), writes=[("brt",)])

        def load_expert(e):
            sl = e % NWS
            Sc.dma("pool", w1e[sl], w1[e].rearrange("(c p) f -> p c f", p=128), writes=[("w1e", sl)])
            Sc.dma("pool", w3e[sl], w3[e].rearrange("(c p) f -> p c f", p=128), writes=[("w3e", sl)])
            Sc.dma("sp", w2s[e % 2], w2[e].rearrange("(c p) d -> p c d", p=128), writes=[("w2s", e % 2)])
            for c in range(2):
                Sc.op("pool", lambda en, c=c, sl=sl, e=e: en.tensor_tensor(out=w2e[sl][:, c, :], in0=w2s[e % 2][:, c, :], in1=g2b, op=ALU.mult),
                      reads=[("w2s", e % 2), ("g2b",)], writes=[("w2e", sl, c)])

        load_expert(0)
        X = mybir.AxisListType.X
        LG = A.alloc("LG", (NT, 36), F32)
        r16 = A.alloc("r16", (12, NT), F32)
        r4a = A.alloc("r4a", (NT, 4), F32)
        r4b = A.alloc("r4b", (NT, 4), F32)
        rml = A.alloc("rml", (NT, 32), F32)
        rml2 = A.alloc("rml2", (NT, 32), F32)
        rm1 = A.alloc("rm1", (NT, 32), F32)
        rm2 = A.alloc("rm2", (NT, 32), F32)
        for t in range(0 if raise_stop else NT):
            p = t % 2
            tsl = slice(t * 128, (t + 1) * 128)
            lg = bank(p)[:, 0:36]
            mm_group(lg, [(uT[:, c, tsl], wr[:, c, :]) for c in range(8)], reads=[("uT",), ("wr",)], writes=[PS(p)])
            Sc.op("dve", lambda e, t=t, lg=lg: e.tensor_tensor(out=LG[:, t, :], in0=lg, in1=brt, op=ALU.add),
                  reads=[PS(p), ("brt",)], writes=[("LG", t)])
        if not raise_stop:
            Lg_ = LG[:, :, 0:4]
            Le_ = LG[:, :, 4:36].rearrange("p t (g e) -> p t g e", g=4)

            def b3(row, n):
                return r16[:, row, :].unsqueeze(2).to_broadcast([128, NT, n])

            def dv(fn, reads, writes):
                Sc.op("dve", fn, reads=reads, writes=writes)
            dv(lambda e: e.tensor_reduce(out=r16[:, 0, :], in_=Lg_, axis=X, op=ALU.max), [("LG",)], [("r16", 0)])
            dv(lambda e: e.tensor_tensor(out=r4a, in0=Lg_, in1=b3(0, 4), op=ALU.is_equal), [("LG",), ("r16", 0)], [("r4a",)])
            dv(lambda e: e.tensor_tensor(out=r4b, in0=Lg_, in1=b3(0, 4), op=ALU.subtract), [("LG",), ("r16", 0)], [("r4b",)])
            Sc.op("act", lambda e: e.activation(out=r4b, in_=r4b, func=AF.Exp), reads=[("r4b",)], writes=[("r4b",)])
            dv(lambda e: e.tensor_reduce(out=r16[:, 1, :], in_=r4b, axis=X, op=ALU.add), [("r4b",)], [("r16", 1)])
            dv(lambda e: e.reciprocal(out=r16[:, 2, :], in_=r16[:, 1, :]), [("r16", 1)], [("r16", 2)])
            dv(lambda e: e.tensor_scalar(out=r4a, in0=r4a, scalar1=-1.0, scalar2=1.0e30, op0=ALU.add, op1=ALU.mult), [("r4a",)], [("r4a",)])
            dv(lambda e: e.tensor_tensor(out=rml.rearrange("p t (g e) -> p t g e", g=4), in0=Le_,
                                         in1=r4a.unsqueeze(3).to_broadcast([128, NT, 4, 8]), op=ALU.add), [("LG",), ("r4a",)], [("rml",)])
            dv(lambda e: e.tensor_reduce(out=r16[:, 3, :], in_=rml, axis=X, op=ALU.max), [("rml",)], [("r16", 3)])
            dv(lambda e: e.tensor_tensor(out=rm1, in0=rml, in1=b3(3, 32), op=ALU.is_equal), [("rml",), ("r16", 3)], [("rm1",)])
            dv(lambda e: e.scalar_tensor_tensor(out=rml2, in0=rm1, scalar=-1.0e30, in1=rml, op0=ALU.mult, op1=ALU.add),
               [("rm1",), ("rml",)], [("rml2",)])
            dv(lambda e: e.tensor_reduce(out=r16[:, 4, :], in_=rml2, axis=X, op=ALU.max), [("rml2",)], [("r16", 4)])
            dv(lambda e: e.tensor_tensor(out=rm2, in0=rml2, in1=b3(4, 32), op=ALU.is_equal), [("rml2",), ("r16", 4)], [("rm2",)])
            dv(lambda e: e.tensor_tensor(out=r16[:, 5, :], in0=r16[:, 4, :], in1=r16[:, 3, :], op=ALU.subtract), [("r16", 3), ("r16", 4)], [("r16", 5)])
            Sc.op("act", lambda e: e.activation(out=r16[:, 6, :], in_=r16[:, 5, :], func=AF.Exp), reads=[("r16", 5)], writes=[("r16", 6)])
            dv(lambda e: e.tensor_scalar(out=r16[:, 7, :], in0=r16[:, 6, :], scalar1=1.0, scalar2=None, op0=ALU.add), [("r16", 6)], [("r16", 7)])
            dv(lambda e: e.reciprocal(out=r16[:, 8, :], in_=r16[:, 7, :]), [("r16", 7)], [("r16", 8)])
            dv(lambda e: e.tensor_tensor(out=r16[:, 9, :], in0=r16[:, 8, :], in1=r16[:, 2, :], op=ALU.mult), [("r16", 8), ("r16", 2)], [("r16", 9)])
            dv(lambda e: e.tensor_tensor(out=r16[:, 10, :], in0=r16[:, 9, :], in1=r16[:, 6, :], op=ALU.mult), [("r16", 9), ("r16", 6)], [("r16", 10)])
            dv(lambda e: e.tensor_tensor(out=rm1, in0=rm1, in1=b3(9, 32), op=ALU.mult), [("rm1",), ("r16", 9)], [("rm1",)])
            dv(lambda e: e.tensor_tensor(out=rm2, in0=rm2, in1=b3(10, 32), op=ALU.mult), [("rm2",), ("r16", 10)], [("rm2",)])
            dv(lambda e: e.tensor_tensor(out=gate, in0=rm1, in1=rm2, op=ALU.add), [("rm1",), ("rm2",)], [("gate",)])
        dump("gate", gate, [128, NT, 32], F32, ("gate",))

        n_exp = 0 if raise_stop else int(os.environ.get("MK_NEXP", N_EXP))

        def up_steps(ex):
            sl = ex % NWS
            hp_ = ex % 2
            steps = []
            for fch in range(2):
                for tg in range(4):
                    def f(fch=fch, tg=tg):
                        fs = slice(fch * 128, (fch + 1) * 128)
                        pp = (fch * 4 + tg) % 2
                        tsl = slice(tg * 512, (tg + 1) * 512)
                        mm_group(bank(pp), [(w1e[sl][:, c, fs], uT[:, c, tsl]) for c in range(8)],
                                 reads=[("w1e", sl), ("uT",)], writes=[PS(pp)])
                        mm_group(bank(2 + pp), [(w3e[sl][:, c, fs], uT[:, c, tsl]) for c in range(8)],
                                 reads=[("w3e", sl), ("uT",)], writes=[PS(2 + pp)])
                        Sc.op("act", lambda e: e.activation(out=sa[pp], in_=bank(pp), func=AF.Silu), reads=[PS(pp)], writes=[("sa", pp)])
                        Sc.op("dve", lambda e: e.tensor_tensor(out=hid[hp_][:, fch, tsl], in0=bank(2 + pp), in1=sa[pp], op=ALU.mult),
                              reads=[PS(2 + pp), ("sa", pp)], writes=[("hid", hp_, fch, tg)])
                    steps.append(f)
            return steps

        def down_steps(ex):
            sl = ex % NWS
            hp_ = ex % 2
            steps = []
            for t in range(NT):
                def f(t=t):
                    ob = 4 + (t % 2) * 2
                    tsl = slice(t * 128, (t + 1) * 128)
                    for half in range(2):
                        mm_group(bank(ob + half), [(hid[hp_][:, fch, tsl], w2e[sl][:, fch, half * 512:(half + 1) * 512]) for fch in range(2)],
                                 reads=[("hid", hp_, 0, t // 4), ("hid", hp_, 1, t // 4), ("w2e", sl)], writes=[PS(ob + half)])
                    Sc.op("dve", lambda e: e.scalar_tensor_tensor(
                        out=acc[:, t, :].rearrange("p (a b) -> p a b", a=2), in0=psum_t[:, ob:ob + 2, :], scalar=gate[:, t, ex:ex + 1],
                        in1=acc[:, t, :].rearrange("p (a b) -> p a b", a=2), op0=ALU.mult, op1=ALU.add),
                        reads=[PS(ob), PS(ob + 1), ("gate", t), ("acc", t)], writes=[("acc", t)])
                steps.append(f)
            return steps

        if n_exp > 0:
            if n_exp > 1:
                load_expert(1)
            for st_ in up_steps(0):
                st_()
        for ex in range(n_exp):
            if ex + 2 < n_exp:
                load_expert(ex + 2)
            dn = down_steps(ex)
            up = up_steps(ex + 1) if ex + 1 < n_exp else []
            for i, d_ in enumerate(dn):
                d_()
                if i % 2 == 1 and (i // 2) < len(up):
                    up[i // 2]()
        dump("h2", acc, [128, NT, D], F32, ("acc",))

        for t in range(0 if raise_stop else NT):
            Sc.op("act", lambda e, t=t: e.activation(out=junk, in_=acc[:, t, :], func=AF.Square, accum_out=stat[:, t:t + 1]),
                  reads=[("acc", t)], writes=[("junk",), ("stat", t)])
        if not raise_stop:
            Sc.op("act", lambda e: e.activation(out=stat[:, 16:32], in_=stat[:, 0:16], func=AF.Sqrt, scale=1.0 / D, bias=EPS),
                  reads=[("stat",)], writes=[("stat", "sq")])
            Sc.op("dve", lambda e: e.reciprocal(out=stat[:, 32:48], in_=stat[:, 16:32]), reads=[("stat", "sq")], writes=[("stat", "rs")])
        for t in range(0 if raise_stop else NT):
            p = t % 2
            Sc.op("dve", lambda e, t=t, p=p: e.scalar_tensor_tensor(out=w2s[p][:, 0, :], in0=acc[:, t, :], scalar=stat[:, 32 + t:33 + t], in1=fnb,
                                                                   op0=ALU.mult, op1=ALU.mult),
                  reads=[("acc", t), ("stat", "rs"), ("fnb",)], writes=[("w2s", p)])
            Sc.dma("sp", out_d[t * 128:(t + 1) * 128, :], w2s[p][:, 0, :], reads=[("w2s", p)], writes=[("out", t)], is_output=True)
        A.release(m4)

        Sc.finish()
        block = es.enter_context(nc.Block())
        Sc.emit(block)
        print(f"[build] instructions={Sc.nins} arena_peak={A.peak}")
    return nc, dbg_outs


_CACHE = {}


def _consts():
    idx = np.arange(128, dtype=np.float64)
    ident = np.eye(128, dtype=np.float32)
    mask = (idx[None, :] >= idx[:, None]).astype(np.float32)
    pos = np.arange(S, dtype=np.float32)
    inv_r = (np.float32(10000.0) ** (-np.arange(0, 256, 2, dtype=np.float32) / np.float32(256))).astype(np.float32)
    ang_r = pos[:, None] * inv_r[None, :]
    rcos = np.cos(ang_r).T.astype(np.float32).copy()
    rsin = np.sin(ang_r).T.astype(np.float32).copy()
    inv_m = (np.float32(10000.0) ** (-np.arange(0, 64, 2, dtype=np.float32) / np.float32(64))).astype(np.float32)
    ang_m = pos[:, None] * inv_m[None, :]
    mcos = np.cos(ang_m).astype(np.float32)
    msin = np.sin(ang_m).astype(np.float32)
    scale = np.float32(192.0 ** -0.5)
    mcos64 = (np.concatenate([mcos, mcos], axis=1).T * scale).astype(np.float32).copy()
    msin64 = (np.concatenate([msin, msin], axis=1).T * scale).astype(np.float32).copy()
    mcos_tok = mcos.reshape(NT, 128, 32).transpose(1, 0, 2).copy()
    msin_tok = msin.reshape(NT, 128, 32).transpose(1, 0, 2).copy()
    lg = np.log1p(-np.exp2(-5.0 - np.arange(RET_H, dtype=np.float64)))
    zp = np.exp(-lg[None, :] * (idx[:, None] + 1.0)) * (256.0 ** -0.5)
    eox2 = EPS * np.exp(-2.0 * lg[None, :] * (idx[:, None] + 1.0))
    cret = np.concatenate([zp, eox2], axis=1).astype(np.float32)
    return dict(c_ident=ident, c_mask=mask, c_rcos=rcos, c_rsin=rsin, c_mcos64=mcos64, c_msin64=msin64,
                c_mcos_tok=mcos_tok, c_msin_tok=msin_tok, c_ret=cret)


def make_in_maps(inputs):
    f = lambda a: np.ascontiguousarray(np.asarray(a, dtype=np.float32))
    g = {k: f(v) for k, v in inputs.items()}
    shared = dict(
        b_ada=g["b_ada"].reshape(1, 6 * D), w_ada=g["w_ada"][0], w_in=g["w_in"][0], w_ret_o=g["w_ret_o"][0],
        w_uq=g["w_uq"][0], w_ukv=g["w_ukv"][0], w_mla_o=g["w_mla_o"][0], w_out=g["w_out"][0],
        w_grp=g["w_grp"][0], w_exp=g["w_exp"][0],
        b_rt=np.concatenate([g["b_grp"][0], g["b_exp"][0]]).reshape(1, 36),
        w1=g["w1"][0].reshape(N_EXP, D, 256), w3=g["w3"][0].reshape(N_EXP, D, 256), w2=g["w2"][0].reshape(N_EXP, 256, D),
        final_norm=g["final_norm"].reshape(1, D),
    )
    shared.update(_consts())
    maps = []
    for b in range(NCORES):
        rows = np.concatenate([g["b_ada"].reshape(48, 128), g["norm1"].reshape(8, 128), g["norm2"].reshape(8, 128),
                               g["q_norm"].reshape(3, 128), g["kv_norm"].reshape(2, 128), g["c"][b].reshape(8, 128)], axis=0)
        m = dict(shared)
        m["x"] = g["x"][b]
        m["rows"] = np.ascontiguousarray(rows)
        maps.append(m)
    return maps


def kernel(**inputs):
    if "nc" not in _CACHE:
        _CACHE["nc"] = build_program()[0]
    nc = _CACHE["nc"]
    maps = make_in_maps(inputs)
    res = run_bass_kernel_spmd(nc, maps, core_ids=list(range(NCORES)))
    return np.stack([np.asarray(r["out"], dtype=np.float32) for r in res.results], axis=0)
```

```python
import contextlib
import os
import numpy as np
import concourse.bass as bass
import concourse.mybir as mybir
from concourse.bass_utils import run_bass_kernel_spmd

F32 = mybir.dt.float32
BF16 = mybir.dt.bfloat16
AF = mybir.ActivationFunctionType
ALU = mybir.AluOpType

D = 1024
S = 2048
NT = 16
NCORES = 8
EPS = 1e-6
RET_H = 4
MLA_H = 8
IN_W = 6848
N_EXP = 32

ENGS = ("pe", "act", "dve", "pool", "sp")
SAME_ENGINE_SYNC = True


def _conflict(k1, k2):
    n = min(len(k1), len(k2))
    return k1[:n] == k2[:n]


class Sched:
    def __init__(self, nc, es, ndma=12):
        self.nc = nc
        self.sem = {}
        for e in ENGS:
            self.sem[("c", e)] = es.enter_context(nc.semaphore(f"c_{e}"))
        self.cnt = {e: 0 for e in ENGS}
        self.ndma = ndma
        self.dcnt = {}
        self.drr = {}
        for q in ("sp", "pool", "act"):
            self.dcnt[q] = [0] * ndma
            self.drr[q] = 0
            for i in range(ndma):
                self.sem[("d", q, i)] = es.enter_context(nc.semaphore(f"d_{q}{i}"))
        self.seen = {e: {} for e in ENGS}
        self.prog = {e: [] for e in ENGS}
        self.lastw = {}
        self.readers = {}
        self.out_tokens = []
        self.nins = 0

    def _collect(self, eng, reads, writes):
        toks = set()
        for k in reads:
            for (k2, tok) in self.lastw.get(k[0], ()):
                if _conflict(k, k2):
                    toks.add(tok)
        for k in writes:
            for (k2, tok) in self.lastw.get(k[0], ()):
                if _conflict(k, k2):
                    toks.add(tok)
            for (k2, tok) in self.readers.get(k[0], ()):
                if _conflict(k, k2):
                    toks.add(tok)
        waits = []
        for (s, v) in sorted(toks, key=lambda t: (str(t[0]), t[1])):
            if s == ("c", eng) and (eng == "pe" or eng == "sp" or not SAME_ENGINE_SYNC):
                continue
            if self.seen[eng].get(s, 0) >= v:
                continue
            waits.append((s, v))
        best = {}
        for (s, v) in waits:
            best[s] = max(best.get(s, 0), v)
        for s, v in best.items():
            self.seen[eng][s] = v
        return list(best.items())

    def _commit(self, tok, reads, writes):
        for k in writes:
            lw = self.lastw.setdefault(k[0], [])
            lw[:] = [(k2, t) for (k2, t) in lw if not (len(k2) >= len(k) and k2[:len(k)] == k)]
            lw.append((k, tok))
            rd = self.readers.setdefault(k[0], [])
            rd[:] = [(k2, t) for (k2, t) in rd if not (len(k2) >= len(k) and k2[:len(k)] == k)]
        for k in reads:
            rd = self.readers.setdefault(k[0], [])
            rd[:] = [(k2, t) for (k2, t) in rd if not (k2 == k and t[0] == tok[0])]
            rd.append((k, tok))

    def op(self, eng, fn, reads=(), writes=()):
        reads = [tuple(k) if isinstance(k, (tuple, list)) else (k,) for k in reads]
        writes = [tuple(k) if isinstance(k, (tuple, list)) else (k,) for k in writes]
        reads = [k[:2] if k[0] == "ps" else k for k in reads]
        writes = [k[:2] if k[0] == "ps" else k for k in writes]
        waits = self._collect(eng, reads, writes)
        self.cnt[eng] += 1
        tok = (("c", eng), self.cnt[eng])
        self._commit(tok, reads, writes)
        self.prog[eng].append((waits, fn, ("c", eng), 1))
        self.nins += 1
        return tok

    def dma(self, q, out, in_, reads=(), writes=(), is_output=False):
        reads = [tuple(k) if isinstance(k, (tuple, list)) else (k,) for k in reads]
        writes = [tuple(k) if isinstance(k, (tuple, list)) else (k,) for k in writes]
        waits = self._collect(q, reads, writes)
        i = self.drr[q]
        self.drr[q] = (i + 1) % self.ndma
        s = ("d", q, i)
        if self.dcnt[q][i] > 0:
            v = 16 * self.dcnt[q][i]
            if self.seen[q].get(s, 0) < v:
                self.seen[q][s] = v
                waits = [w for w in waits if w[0] != s] + [(s, v)]
        self.dcnt[q][i] += 1
        tok = (s, 16 * self.dcnt[q][i])
        self._commit(tok, reads, writes)

        def fn(e, out=out, in_=in_):
            return e.dma_start(out=out, in_=in_)
        self.prog[q].append((waits, fn, s, 16))
        if is_output:
            self.out_tokens.append(tok)
        self.nins += 1
        return tok

    def alias(self, new, old):
        toks = set(t for (_, t) in self.lastw.get(old, ())) | set(t for (_, t) in self.readers.get(old, ()))
        lw = self.lastw.setdefault(new, [])
        for t in sorted(toks, key=lambda t: (str(t[0]), t[1])):
            lw.append(((new,), t))

    def barrier(self):
        snap = dict(self.cnt)
        for e in ENGS:
            waits = []
            for f in ENGS:
                if f == e or snap[f] == 0:
                    continue
                s = ("c", f)
                if self.seen[e].get(s, 0) < snap[f]:
                    self.seen[e][s] = snap[f]
                    waits.append((s, snap[f]))
            if waits:
                self.prog[e].append((waits, None, None, 0))

    def finish(self):
        waits = {}
        for (s, v) in self.out_tokens:
            waits[s] = max(waits.get(s, 0), v)
        for q in ("sp", "pool", "act"):
            for i in range(self.ndma):
                if self.dcnt[q][i] > 0:
                    waits[("d", q, i)] = 16 * self.dcnt[q][i]
        for e in ENGS:
            if self.cnt[e] > 0:
                waits[("c", e)] = self.cnt[e]
        self.prog["sp"].append((list(waits.items()), None, None, 0))

    def emit(self, block):
        nc = self.nc
        sem = self.sem

        def replay(eng_name):
            def run(e):
                for (waits, fn, s, inc) in self.prog[eng_name]:
                    for (ws, wv) in waits:
                        e.wait_ge(sem[ws], wv)
                    if fn is not None:
                        ins = fn(e)
                        ins.then_inc(sem[s], inc)
            return run

        block.tensor(replay("pe"))
        block.scalar(replay("act"))
        block.vector(replay("dve"))
        block.gpsimd(replay("pool"))
        block.sync(replay("sp"))


class Arena:
    def __init__(self, base_ap, nbytes, sched):
        self.base = base_ap
        self.nbytes = nbytes
        self.off = 0
        self.peak = 0
        self.sched = sched
        self.history = []

    def mark(self):
        return self.off

    def release(self, m):
        self.off = m

    def alloc(self, name, shape, dtype):
        n = int(np.prod(shape))
        esz = 4 if dtype == F32 else 2
        nb = (n * esz + 31) // 32 * 32
        assert self.off + nb <= self.nbytes, f"arena overflow {self.off}+{nb}>{self.nbytes}"
        lo, hi = self.off, self.off + nb
        olds = set(nm for (l, h, nm) in self.history if l < hi and lo < h and nm != name)
        for o in sorted(olds):
            self.sched.alias(name, o)
        self.history.append((lo, hi, name))
        self.offs = getattr(self, "offs", {})
        self.offs.setdefault(name, lo)
        a = self.base[:, self.off // 4:(self.off + nb) // 4]
        self.off += nb
        self.peak = max(self.peak, self.off)
        if dtype != F32:
            a = a.bitcast(dtype)
        a = a[:, 0:n]
        if len(shape) == 2:
            a = a.rearrange("p (a b) -> p a b", a=shape[0])
        elif len(shape) == 3:
            a = a.rearrange("p (a b c) -> p a b c", a=shape[0], b=shape[1])
        return a


def build_program(dbg=()):
    nc = bass.Bass("TRN2", target_bir_lowering=False)
    dbg = set(dbg)

    def din(name, shape, dt=F32):
        return nc.dram_tensor(name, list(shape), dt, kind="ExternalInput").ap()

    x = din("x", [S, D])
    rows_d = din("rows", [77, 128])
    b_ada = din("b_ada", [1, 6 * D])
    w_ada = din("w_ada", [D, 6 * D])
    w_in = din("w_in", [D, IN_W])
    w_ret_o = din("w_ret_o", [D, D])
    w_uq = din("w_uq", [384, 1536])
    w_ukv = din("w_ukv", [256, 2048])
    w_mla_o = din("w_mla_o", [D, D])
    w_out = din("w_out", [D, D])
    w_grp = din("w_grp", [D, 4])
    w_exp = din("w_exp", [D, 32])
    b_rt = din("b_rt", [1, 36])
    w1 = din("w1", [N_EXP, D, 256])
    w3 = din("w3", [N_EXP, D, 256])
    w2 = din("w2", [N_EXP, 256, D])
    fnorm = din("final_norm", [1, D])
    c_ident = din("c_ident", [128, 128])
    c_mask = din("c_mask", [128, 128])
    c_rcos = din("c_rcos", [128, S])
    c_rsin = din("c_rsin", [128, S])
    c_mcos64 = din("c_mcos64", [64, S])
    c_msin64 = din("c_msin64", [64, S])
    c_mcos_tok = din("c_mcos_tok", [128, NT, 32])
    c_msin_tok = din("c_msin_tok", [128, NT, 32])
    c_ret = din("c_ret", [128, 8])
    out_d = nc.dram_tensor("out", [S, D], F32, kind="ExternalOutput").ap()
    dbg_outs = {}

    log_gamma = [float(np.log1p(-np.exp2(-5.0 - h))) for h in range(RET_H)]
    gC = [float(np.exp(lg * 128.0)) for lg in log_gamma]

    with contextlib.ExitStack() as es:
        ARENA_BYTES = 204 * 1024
        arena_t = es.enter_context(nc.sbuf_tensor("arena", [128, ARENA_BYTES // 4], F32))
        psum_t = es.enter_context(nc.psum_tensor("psum", [128, 8, 512], F32))
        Sc = Sched(nc, es)
        A = Arena(arena_t[:, :], ARENA_BYTES, Sc)

        def bank(b):
            return psum_t[:, b, :]

        def bank_bf(b):
            return psum_t[:, b, :].bitcast(BF16)

        def PS(b):
            return ("ps", b)

        def dump(name, ap, shape, dt, key):
            if name not in dbg:
                return
            d = nc.dram_tensor("dbg_" + name, list(shape), dt, kind="ExternalOutput").ap()
            dbg_outs[name] = d
            Sc.dma("sp", d, ap, reads=[key], writes=[("dbgout", name)], is_output=True)

        def mm_group(out, pairs, reads, writes):
            def fn(e, out=out, pairs=list(pairs)):
                n = len(pairs)
                ins = None
                for i, (l, r) in enumerate(pairs):
                    ins = e.matmul(out=out, lhsT=l, rhs=r, start=(i == 0), stop=(i == n - 1))
                return ins
            return Sc.op("pe", fn, reads=reads, writes=writes)

        def transposes(items, reads, writes, ident):
            def fn(e, items=list(items), ident=ident):
                ins = None
                for (o, i) in items:
                    ins = e.transpose(out=o, in_=i, identity=ident)
                return ins
            return Sc.op("pe", fn, reads=reads, writes=writes)

        ident_bf = A.alloc("ident_bf", (128,), BF16)
        ident_f = A.alloc("ident_f", (128,), F32)
        mask01 = A.alloc("mask01", (128,), BF16)
        cols = A.alloc("cols", (80,), F32)
        modc = A.alloc("modc", (48,), F32)
        A1 = A.alloc("A1", (8,), F32)
        A2 = A.alloc("A2", (8,), F32)
        cact = A.alloc("cact", (8,), F32)
        cret = A.alloc("cret", (8,), F32)
        stat = A.alloc("stat", (64,), F32)
        g1b = A.alloc("g1b", (D,), F32)
        g2b = A.alloc("g2b", (D,), F32)
        uT = A.alloc("uT", (8, S), BF16)
        R_G = A.alloc("gatedT", (8, S), BF16)
        R_A = A.alloc("attnT", (8, S), BF16)
        gatedT = R_G
        attnT = R_A
        TMP0 = A.mark()

        Sc.dma("pool", ident_bf, c_ident, writes=[("ident_bf",)])
        Sc.dma("sp", ident_f, c_ident, writes=[("ident_f",)])
        Sc.dma("pool", mask01, c_mask, writes=[("mask01",)])
        Sc.dma("sp", cret, c_ret, writes=[("cret",)])

        m0 = A.mark()
        rows_in = A.alloc("rows_in", (128,), F32)
        cact_rep = A.alloc("cact_rep", (8, 128), F32)
        wa = [A.alloc("wa", (8, 512), F32) for _ in range(2)]
        xt = [A.alloc("xt", (D,), F32) for _ in range(2)]
        xn = [A.alloc("xn", (D,), BF16) for _ in range(2)]
        junk = A.alloc("junk", (D,), BF16)

        Sc.dma("sp", rows_in[0:77, :], rows_d, writes=[("rows_in",)])
        Sc.dma("sp", g1b, b_ada[0, 2 * D:3 * D].partition_broadcast(128), writes=[("g1b",)])
        Sc.dma("sp", g2b, b_ada[0, 5 * D:6 * D].partition_broadcast(128), writes=[("g2b",)])

        transposes([(bank(2)[:, 0:77], rows_in[0:77, :])], reads=[("rows_in",), ("ident_f",)], writes=[PS(2)],
                   ident=ident_f[0:77, 0:77])
        Sc.op("dve", lambda e: e.tensor_copy(out=cols[:, 0:77], in_=bank(2)[:, 0:77]), reads=[PS(2)], writes=[("cols",)])
        Sc.op("act", lambda e: e.activation(out=cact, in_=cols[:, 69:77], func=AF.Silu), reads=[("cols",)], writes=[("cact",)])
        for c in range(8):
            Sc.op("dve", lambda e, c=c: e.tensor_copy(out=cact_rep[:, c, :], in_=cact[:, c:c + 1].to_broadcast([128, 128])),
                  reads=[("cact",)], writes=[("cact_rep", c)])

        wa_view = w_ada.rearrange("(c p) n -> p c n", p=128)
        blk_order = [0, 1, 2, 3, 6, 7, 8, 9, 4, 5, 10, 11]
        for bi, blk in enumerate(blk_order):
            wb = wa[bi % 2]
            Sc.dma("sp", wb, wa_view[:, :, blk * 512:(blk + 1) * 512], writes=[("wa", bi % 2)])
            if blk in (4, 5, 10, 11):
                gb = g1b if blk in (4, 5) else g2b
                gk = ("g1b",) if blk in (4, 5) else ("g2b",)
                half = blk % 2 if blk in (4, 5) else (blk - 10)
                pb = 3 + (bi % 2)
                mm_group(bank(pb), [(cact_rep[:, c, :], wb[:, c, :]) for c in range(8)],
                         reads=[("cact_rep",), ("wa", bi % 2)], writes=[PS(pb)])
                Sc.op("dve", lambda e, gb=gb, half=half, pb=pb: e.tensor_tensor(
                    out=gb[:, half * 512:(half + 1) * 512], in0=bank(pb), in1=gb[:, half * 512:(half + 1) * 512], op=ALU.add),
                    reads=[PS(pb), gk], writes=[gk])
            else:
                for j in range(4):
                    J = blk * 4 + j
                    mm_group(bank(2)[:, 128 + J:129 + J], [(wb[:, c, j * 128:(j + 1) * 128], cact[:, c:c + 1]) for c in range(8)],
                             reads=[("cact",), ("wa", bi % 2)], writes=[("ps", 2, "col", J)])
            if bi == 3:
                Sc.op("dve", lambda e: e.tensor_tensor(out=modc[:, 0:16], in0=bank(2)[:, 128:144], in1=cols[:, 0:16], op=ALU.add),
                      reads=[("ps", 2, "col"), ("cols",)], writes=[("modc", 0)])
                Sc.op("dve", lambda e: e.scalar_tensor_tensor(out=A1, in0=modc[:, 8:16], scalar=1.0, in1=cols[:, 48:56],
                                                               op0=ALU.add, op1=ALU.mult),
                      reads=[("modc", 0), ("cols",)], writes=[("A1",)])
            if bi == 7:
                Sc.op("dve", lambda e: e.tensor_tensor(out=modc[:, 24:40], in0=bank(2)[:, 152:168], in1=cols[:, 24:40], op=ALU.add),
                      reads=[("ps", 2, "col"), ("cols",)], writes=[("modc", 1)])
                Sc.op("dve", lambda e: e.scalar_tensor_tensor(out=A2, in0=modc[:, 32:40], scalar=1.0, in1=cols[:, 56:64],
                                                               op0=ALU.add, op1=ALU.mult),
                      reads=[("modc", 1), ("cols",)], writes=[("A2",)])
        dump("modc", modc, [128, 48], F32, ("modc",))
        dump("g1b", g1b, [128, D], F32, ("g1b",))

        def norm_A(t, src_ap, src_key):
            b2 = t % len(xn)
            junk_ = junk
            xn_ = xn[b2]
            Sc.op("act", lambda e: e.activation(out=junk_, in_=src_ap, func=AF.Square, accum_out=stat[:, t:t + 1]),
                  reads=[src_key], writes=[("junk",), ("stat", t)])
            Sc.op("act", lambda e: e.activation(out=stat[:, 16 + t:17 + t], in_=stat[:, t:t + 1], func=AF.Sqrt,
                                                scale=1.0 / D, bias=EPS),
                  reads=[("stat", t)], writes=[("stat", 16 + t)])
            Sc.op("dve", lambda e: e.reciprocal(out=stat[:, 32 + t:33 + t], in_=stat[:, 16 + t:17 + t]),
                  reads=[("stat", 16 + t)], writes=[("stat", 32 + t)])
            Sc.op("act", lambda e: e.activation(out=xn_, in_=src_ap, func=AF.Copy, scale=stat[:, 32 + t:33 + t]),
                  reads=[src_key, ("stat", 32 + t)], writes=[("xn", b2)])

        def norm_B(t, Acol, shcol, Akey, shkey, dstT, dst_name, pb):
            b2 = t % len(xn)
            xn_ = xn[b2]
            pT = bank_bf(pb).rearrange("p (a b) -> p a b", a=8)
            transposes([(pT[:, c, :], xn_[:, c * 128:(c + 1) * 128]) for c in range(8)],
                       reads=[("xn", b2), ("ident_bf",)], writes=[PS(pb)], ident=ident_bf)
            for c in range(8):
                Sc.op("dve", lambda e, c=c: e.tensor_scalar(out=dstT[:, c, t * 128:(t + 1) * 128], in0=pT[:, c, :],
                                                            scalar1=Acol[:, c:c + 1], scalar2=shcol[:, c:c + 1],
                                                            op0=ALU.mult, op1=ALU.add),
                      reads=[PS(pb), Akey, shkey], writes=[(dst_name, c, t)])

        Sc.dma("sp", xt[0], x[0:128, :], writes=[("xt", 0)])
        norm_A(0, xt[0], ("xt", 0))
        for t in range(NT):
            if t + 1 < NT:
                Sc.dma("sp", xt[(t + 1) % 2], x[(t + 1) * 128:(t + 2) * 128, :], writes=[("xt", (t + 1) % 2)])
                norm_A(t + 1, xt[(t + 1) % 2], ("xt", (t + 1) % 2))
            norm_B(t, A1, modc[:, 0:8], ("A1",), ("modc", 0), uT, "uT", pb=t % 2)
        dump("uT", uT, [128, 8, S], BF16, ("uT",))
        A.release(m0)


        m1 = A.mark()
        rcos = A.alloc("rcos", (S,), BF16)
        rsin = A.alloc("rsin", (S,), BF16)
        Sc.dma("pool", rcos, c_rcos, writes=[("rcos",)])
        Sc.dma("pool", rsin, c_rsin, writes=[("rsin",)])
        NW = 8
        wslot = [A.alloc("wslot", (8, 256), BF16) for _ in range(NW)]
        qT = A.alloc("qT", (2, S), BF16)
        kT = A.alloc("kT", (2, S), BF16)
        dummy_ = A.alloc("dummy_", (512,), F32) if os.environ.get("MK_DUMMY") else None
        v_s = A.alloc("v_s", (NT, 256), BF16)
        sgT = A.alloc("sgT", (2, S), BF16)
        Tst = A.alloc("Tst", (512,), F32)
        st_bf = [A.alloc("st_bf", (2, 256), BF16) for _ in range(2)]
        sm = [A.alloc("sm", (128,), BF16) for _ in range(2)]
        ktok = [A.alloc("ktok", (256,), BF16) for _ in range(2)]
        yn = [A.alloc("yn", (256,), BF16) for _ in range(2)]
        rt = [A.alloc("rt", (512,), F32) for _ in range(4)]
        st6 = [A.alloc("st6", (8,), F32) for _ in range(2)]
        mv = [A.alloc("mv", (8,), F32) for _ in range(2)]
        mhalf = A.alloc("mhalf", (8,), F32)
        Sc.op("pool", lambda e: e.memset(mhalf, -0.5), reads=[], writes=[("mhalf",)])
        w_in_v = w_in.rearrange("(c p) n -> p c n", p=128)
        wstate = {"n": 0}

        def load_w(col0):
            slot = wstate["n"] % NW
            wstate["n"] += 1
            Sc.dma("pool", wslot[slot], w_in_v[:, :, col0:col0 + 256], writes=[("wslot", slot)])
            return slot

        def head_slots(h):
            return [load_w(m * 1024 + h * 256) for m in range(4)]

        def proj_units(h, tg, slots):
            sq, sk, sv, sg = slots
            tsl = slice(tg * 512, (tg + 1) * 512)
            units = []

            def qk_mm(nm, sl, j):
                def f():
                    mm_group(bank(j), [(wslot[sl][:, c, j * 128:(j + 1) * 128], uT[:, c, tsl]) for c in range(8)],
                             reads=[("wslot", sl), ("uT",)], writes=[PS(j)])
                return f

            def qk_rope(nm, dst):
                def f():
                    P0, P1 = bank(0), bank(1)
                    Sc.op("dve", lambda e: e.tensor_tensor(out=rt[0], in0=P0, in1=rcos[:, tsl], op=ALU.mult),
                          reads=[PS(0), ("rcos",)], writes=[("rt", 0)])
                    Sc.op("dve", lambda e: e.tensor_tensor(out=rt[1], in0=P1, in1=rsin[:, tsl], op=ALU.mult),
                          reads=[PS(1), ("rsin",)], writes=[("rt", 1)])
                    Sc.op("dve", lambda e: e.tensor_tensor(out=rt[2], in0=P1, in1=rcos[:, tsl], op=ALU.mult),
                          reads=[PS(1), ("rcos",)], writes=[("rt", 2)])
                    Sc.op("dve", lambda e: e.tensor_tensor(out=rt[3], in0=P0, in1=rsin[:, tsl], op=ALU.mult),
                          reads=[PS(0), ("rsin",)], writes=[("rt", 3)])
                    Sc.op("pool", lambda e: e.tensor_tensor(out=dst[:, 0, tsl], in0=rt[0], in1=rt[1], op=ALU.subtract),
                          reads=[("rt", 0), ("rt", 1)], writes=[(nm + "T", 0, tg)])
                    Sc.op("pool", lambda e: e.tensor_tensor(out=dst[:, 1, tsl], in0=rt[2], in1=rt[3], op=ALU.add),
                          reads=[("rt", 2), ("rt", 3)], writes=[(nm + "T", 1, tg)])
                return f

            def qk_units(nm, sl, dst):
                m1_ = qk_mm(nm, sl, 1)
                r_ = qk_rope(nm, dst)
                return [qk_mm(nm, sl, 0), (lambda: (m1_(), r_()))]

            def g_unit(j):
                def f():
                    mm_group(bank(j), [(wslot[sg][:, c, j * 128:(j + 1) * 128], uT[:, c, tsl]) for c in range(8)],
                             reads=[("wslot", sg), ("uT",)], writes=[PS(j)])
                    Sc.op("act", lambda e: e.activation(out=sgT[:, j, tsl], in_=bank(j), func=AF.Silu),
                          reads=[PS(j)], writes=[("sgT", j, tg)])
                return f

            def v_unit(t):
                def f():
                    rg_ = bank(2)[:, 0:256]
                    mm_group(rg_, [(uT[:, c, t * 128:(t + 1) * 128], wslot[sv][:, c, :]) for c in range(8)],
                             reads=[("wslot", sv), ("uT",)], writes=[PS(2)])
                    Sc.op("act", lambda e: e.activation(out=v_s[:, t, :], in_=rg_, func=AF.Copy, scale=cret[:, h:h + 1]),
                          reads=[PS(2), ("cret",)], writes=[("v_s", t)])
                return f

            g0_, g1_ = g_unit(0), g_unit(1)
            qu_ = qk_units("q", sq, qT)
            ku_ = qk_units("k", sk, kT)
            units += [qu_[0], v_unit(4 * tg), qu_[1], v_unit(4 * tg + 1), ku_[0], v_unit(4 * tg + 2), ku_[1], v_unit(4 * tg + 3)]
            units.append(lambda: (g0_(), g1_()))
            return units

        def ret_tail(h, n):
            p = n % 2
            tg = n // 4
            nsl = slice(n * 128, (n + 1) * 128)
            yT = bank_bf(7)[:, p * 256:(p + 1) * 256].rearrange("p (c t) -> p c t", c=2)
            transposes([(yT[:, c, :], yn[p][:, c * 128:(c + 1) * 128]) for c in range(2)],
                       reads=[("yn", p), ("ident_bf",)], writes=[("ps", 7, p)], ident=ident_bf)
            Sc.op("dve", lambda e: e.tensor_tensor(out=gatedT[:, 2 * h:2 * h + 2, nsl], in0=yT, in1=sgT[:, :, nsl], op=ALU.mult),
                  reads=[("ps", 7, p), ("sgT", 0, tg), ("sgT", 1, tg)], writes=[("gatedT", h, n)])

        def recur_units(h, n):
            p = n % 2
            tg = n // 4
            nsl = slice(n * 128, (n + 1) * 128)
            qk_keys = [("kT", 0, tg), ("kT", 1, tg), ("qT", 0, tg), ("qT", 1, tg)]
            scb = bank(4)[:, p * 128:(p + 1) * 128]
            ybk = 3 if p == 0 else 5
            yb = bank(ybk)[:, 0:256]

            def ua():
                mm_group(scb, [(kT[:, c, nsl], qT[:, c, nsl]) for c in range(2)], reads=qk_keys, writes=[("ps", 4, "s", p)])
                Sc.op("dve", lambda e: e.tensor_tensor(out=sm[p], in0=scb, in1=mask01, op=ALU.mult),
                      reads=[("ps", 4, "s", p), ("mask01",)], writes=[("sm", p)])
                if n < NT - 1:
                    kt = bank_bf(6)[:, 0:256]
                    transposes([(kt[:, c * 128:(c + 1) * 128], kT[:, c, nsl]) for c in range(2)],
                               reads=qk_keys[0:2] + [("ident_bf",)], writes=[PS(6)], ident=ident_bf)
                    Sc.op("act", lambda e: e.activation(out=ktok[p], in_=kt, func=AF.Copy),
                          reads=[PS(6)], writes=[("ktok", p)])

            def ukv():
                if n < NT - 1:
                    for c in range(2):
                        mm_group(bank(6)[:, c * 256:(c + 1) * 256], [(ktok[p][:, c * 128:(c + 1) * 128], v_s[:, n, :])],
                                 reads=[("ktok", p), ("v_s", n)], writes=[("ps", 6, c)])
                    if n == 0:
                        Sc.op("dve", lambda e: e.tensor_copy(out=Tst, in_=bank(6)), reads=[("ps", 6)], writes=[("Tst",)])
                    else:
                        Sc.op("dve", lambda e: e.scalar_tensor_tensor(out=Tst, in0=Tst, scalar=gC[h], in1=bank(6),
                                                                       op0=ALU.mult, op1=ALU.add),
                              reads=[("ps", 6), ("Tst",)], writes=[("Tst",)])
                    Sc.op("act", lambda e: e.activation(out=st_bf[p].rearrange("p a b -> p (a b)"), in_=Tst, func=AF.Copy, scale=gC[h]),
                          reads=[("Tst",)], writes=[("st_bf", p)])

            def uy():
                pairs = [(sm[p], v_s[:, n, :])]
                rds = [("sm", p), ("v_s", n)]
                if n > 0:
                    pairs += [(qT[:, c, nsl], st_bf[1 - p][:, c, :]) for c in range(2)]
                    rds += qk_keys[2:4] + [("st_bf", 1 - p)]
                mm_group(yb, pairs, reads=rds, writes=[PS(ybk)])

            def uc():
                Sc.op("dve", lambda e: e.bn_stats(out=st6[p][:, 0:6], in_=yb), reads=[PS(ybk)], writes=[("st6", p)])
                Sc.op("dve", lambda e: e.bn_aggr(out=mv[p][:, 0:2], in_=st6[p][:, 0:6]), reads=[("st6", p)], writes=[("mv", p, 0)])
                Sc.op("dve", lambda e: e.tensor_scalar(out=mv[p][:, 2:3], in0=mv[p][:, 1:2], scalar1=cret[:, 4 + h:5 + h], scalar2=None,
                                                       op0=ALU.add),
                      reads=[("mv", p, 0), ("cret",)], writes=[("mv", p, 2)])
                Sc.op("pool", lambda e: e.tensor_tensor(out=mv[p][:, 3:4], in0=mv[p][:, 2:3], in1=mhalf[:, 0:1], op=ALU.pow),
                      reads=[("mv", p, 2), ("mhalf",)], writes=[("mv", p, 3)])
                Sc.op("dve", lambda e: e.tensor_scalar(out=yn[p], in0=yb, scalar1=mv[p][:, 0:1], scalar2=mv[p][:, 3:4],
                                                       op0=ALU.subtract, op1=ALU.mult),
                      reads=[PS(ybk), ("mv", p)], writes=[("yn", p)])
                if n > 0:
                    ret_tail(h, n - 1)
                if n == NT - 1:
                    ret_tail(h, n)
            return dict(a=ua, kv=ukv, y=uy, c=uc)

        all_slots = {0: head_slots(0)}
        seq = [(h, tg) for h in range(RET_H) for tg in range(4)][:int(os.environ.get('MK_R', 16))]
        for u in proj_units(0, 0, all_slots[0])[:int(os.environ.get('MK_U', 99))]:
            u()
        if os.environ.get('MK_NOPF', '') == '':
            all_slots[1] = head_slots(1)
        for i, (h, tg) in enumerate(seq):
            if tg == 0 and h >= 1 and h + 1 < RET_H:
                all_slots[h + 1] = head_slots(h + 1)
            pu = []
            if i + 1 < len(seq):
                h2, tg2 = seq[i + 1]
                pu = proj_units(h2, tg2, all_slots[h2])
            ru = []
            for n in range(4 * tg, 4 * tg + 4):
                u_ = recur_units(h, n)
                ru += [u_["a"], u_["kv"], (lambda u_=u_: (u_["y"](), u_["c"]()))]
            k = 0
            for j, r in enumerate(ru):
                r()
                if j % 3 != 0 and k < len(pu):
                    pu[k]()
                    k += 1
            while k < len(pu):
                pu[k]()
                k += 1
        dump("gatedT", gatedT, [128, 8, S], BF16, ("gatedT",))
        A.release(m1)


        if os.environ.get("MK_STOP", "") == "1":
            A.release(m1)
            Sc.finish()
            block = es.enter_context(nc.Block())
            Sc.emit(block)
            return nc, dbg_outs
        m2 = A.mark()
        SCALE = float(192.0 ** -0.5)
        wuq = A.alloc("wuq", (3, 1536), BF16)
        wpad = [A.alloc("wpad", (3, 128), BF16) for _ in range(2)]
        wukv = A.alloc("wukv", (2, 2048), BF16)
        mcs = A.alloc("mcs", (S,), BF16)
        cqnT = A.alloc("cqnT", (3, S), BF16)
        ckvnT = A.alloc("ckvnT", (2, S), BF16)
        kpeT = A.alloc("kpeT", (S,), BF16)
        v_aug = A.alloc("v_aug", (NT, 4, 130), BF16)
        Sc.dma("pool", wuq, w_uq.rearrange("(c p) n -> p c n", p=128), writes=[("wuq",)])
        Sc.dma("pool", wukv, w_ukv.rearrange("(c p) n -> p c n", p=128), writes=[("wukv",)])
        wuq4 = wuq.rearrange("p c (h k) -> p c h k", h=8)
        Sc.dma("pool", mcs[0:64, :], c_mcos64, writes=[("mcs", 0)])
        Sc.dma("pool", mcs[64:128, :], c_msin64, writes=[("mcs", 1)])

        mA = A.mark()
        wl = A.alloc("wl", (8, 704), BF16)
        mcos_t = A.alloc("mcos_t", (NT, 32), F32)
        msin_t = A.alloc("msin_t", (NT, 32), F32)
        cqn = [A.alloc("cqn", (384,), BF16) for _ in range(2)]
        ckvn = [A.alloc("ckvn", (256,), BF16) for _ in range(2)]
        kper = [A.alloc("kper", (128,), BF16) for _ in range(2)]
        lst = [A.alloc("lst", (8,), F32) for _ in range(2)]
        kr = [A.alloc("kr", (4, 32), F32) for _ in range(2)]
        ljunk = A.alloc("ljunk", (384,), BF16)
        for i in range(2):
            Sc.op("dve", lambda e, i=i: e.memset(kper[i], 0.0), reads=[], writes=[("kper", i)])
        Sc.dma("pool", wl, w_in_v[:, :, 4096:4800], writes=[("wl",)])
        Sc.dma("sp", mcos_t, c_mcos_tok, writes=[("mcos_t",)])
        Sc.dma("sp", msin_t, c_msin_tok, writes=[("msin_t",)])
        _stop = os.environ.get("MK_STOP", "")
        for t in range(0 if _stop == "2pre" else int(os.environ.get("MK_NT", NT))):
            p = t % 2
            b0 = 4 * p
            tsl = slice(t * 128, (t + 1) * 128)
            mm_group(bank(b0)[:, 0:384], [(uT[:, c, tsl], wl[:, c, 0:384]) for c in range(8)],
                     reads=[("uT",), ("wl",)], writes=[PS(b0)])
            mm_group(bank(b0 + 1)[:, 0:320], [(uT[:, c, tsl], wl[:, c, 384:704]) for c in range(8)],
                     reads=[("uT",), ("wl",)], writes=[PS(b0 + 1)])
            cq_ps = bank(b0)[:, 0:384]
            ckv_ps = bank(b0 + 1)[:, 0:256]
            kx1 = bank(b0 + 1)[:, 256:288]
            kx2 = bank(b0 + 1)[:, 288:320]
            _A = os.environ.get("MK_A", "sq,rope,tr,ev")
            if "sq" not in _A:
                continue
            Sc.op("act", lambda e, p=p, cq_ps=cq_ps: e.activation(out=ljunk, in_=cq_ps, func=AF.Square, accum_out=lst[p][:, 0:1]),
                  reads=[PS(b0)], writes=[("ljunk",), ("lst", p, 0)])
            Sc.op("act", lambda e, p=p, ckv_ps=ckv_ps: e.activation(out=ljunk[:, 0:256], in_=ckv_ps, func=AF.Square, accum_out=lst[p][:, 1:2]),
                  reads=[PS(b0 + 1)], writes=[("ljunk",), ("lst", p, 1)])
            Sc.op("act", lambda e, p=p: e.activation(out=lst[p][:, 2:3], in_=lst[p][:, 0:1], func=AF.Sqrt, scale=1.0 / 384, bias=EPS),
                  reads=[("lst", p, 0)], writes=[("lst", p, 2)])
            Sc.op("act", lambda e, p=p: e.activation(out=lst[p][:, 3:4], in_=lst[p][:, 1:2], func=AF.Sqrt, scale=1.0 / 256, bias=EPS),
                  reads=[("lst", p, 1)], writes=[("lst", p, 3)])
            Sc.op("dve", lambda e, p=p: e.reciprocal(out=lst[p][:, 4:6], in_=lst[p][:, 2:4]),
                  reads=[("lst", p, 2), ("lst", p, 3)], writes=[("lst", p, 4)])
            Sc.op("act", lambda e, p=p, cq_ps=cq_ps: e.activation(out=cqn[p], in_=cq_ps, func=AF.Copy, scale=lst[p][:, 4:5]),
                  reads=[PS(b0), ("lst", p, 4)], writes=[("cqn", p)])
            Sc.op("act", lambda e, p=p, ckv_ps=ckv_ps: e.activation(out=ckvn[p], in_=ckv_ps, func=AF.Copy, scale=lst[p][:, 4 + 1:6]),
                  reads=[PS(b0 + 1), ("lst", p, 4)], writes=[("ckvn", p)])
            if "rope" not in _A:
                continue
            cs = mcos_t[:, t, :]
            sn = msin_t[:, t, :]
            Sc.op("dve", lambda e, p=p, kx1=kx1, cs=cs: e.tensor_tensor(out=kr[p][:, 0, :], in0=kx1, in1=cs, op=ALU.mult),
                  reads=[PS(b0 + 1), ("mcos_t",)], writes=[("kr", p, 0)])
            Sc.op("dve", lambda e, p=p, kx2=kx2, sn=sn: e.tensor_tensor(out=kr[p][:, 1, :], in0=kx2, in1=sn, op=ALU.mult),
                  reads=[PS(b0 + 1), ("msin_t",)], writes=[("kr", p, 1)])
            Sc.op("dve", lambda e, p=p, kx2=kx2, cs=cs: e.tensor_tensor(out=kr[p][:, 2, :], in0=kx2, in1=cs, op=ALU.mult),
                  reads=[PS(b0 + 1), ("mcos_t",)], writes=[("kr", p, 2)])
            Sc.op("dve", lambda e, p=p, kx1=kx1, sn=sn: e.tensor_tensor(out=kr[p][:, 3, :], in0=kx1, in1=sn, op=ALU.mult),
                  reads=[PS(b0 + 1), ("msin_t",)], writes=[("kr", p, 3)])
            Sc.op("dve", lambda e, p=p: e.tensor_tensor(out=kper[p][:, 0:32], in0=kr[p][:, 0, :], in1=kr[p][:, 1, :], op=ALU.subtract),
                  reads=[("kr", p)], writes=[("kper", p, 0)])
            Sc.op("dve", lambda e, p=p: e.tensor_tensor(out=kper[p][:, 64:96], in0=kr[p][:, 0, :], in1=kr[p][:, 1, :], op=ALU.subtract),
                  reads=[("kr", p)], writes=[("kper", p, 2)])
            Sc.op("dve", lambda e, p=p: e.tensor_tensor(out=kper[p][:, 96:128], in0=kr[p][:, 2, :], in1=kr[p][:, 3, :], op=ALU.add),
                  reads=[("kr", p)], writes=[("kper", p, 3)])
            Sc.op("dve", lambda e, p=p: e.tensor_tensor(out=kper[p][:, 32:64], in0=kr[p][:, 2, :], in1=kr[p][:, 3, :], op=ALU.add),
                  reads=[("kr", p)], writes=[("kper", p, 1)])
            if "tr" not in _A:
                continue
            pT = bank_bf(b0 + 2)[:, 0:768].rearrange("p (a b) -> p a b", a=6)
            items = [(pT[:, c, :], cqn[p][:, c * 128:(c + 1) * 128]) for c in range(3)]
            items += [(pT[:, 3 + c, :], ckvn[p][:, c * 128:(c + 1) * 128]) for c in range(2)]
            items += [(pT[:, 5, :], kper[p])]
            transposes(items, reads=[("cqn", p), ("ckvn", p), ("kper", p), ("ident_bf",)], writes=[PS(b0 + 2)], ident=ident_bf)
            if "ev" not in _A:
                continue
            for c in range(3):
                Sc.op("dve", lambda e, c=c, pT=pT, tsl=tsl: e.tensor_scalar(out=cqnT[:, c, tsl], in0=pT[:, c, :], scalar1=cols[:, 64 + c:65 + c],
                                                                           scalar2=None, op0=ALU.mult),
                      reads=[PS(b0 + 2), ("cols",)], writes=[("cqnT", c, t)])
            for c in range(2):
                Sc.op("dve", lambda e, c=c, pT=pT, tsl=tsl: e.tensor_scalar(out=ckvnT[:, c, tsl], in0=pT[:, 3 + c, :], scalar1=cols[:, 67 + c:68 + c],
                                                                           scalar2=None, op0=ALU.mult),
                      reads=[PS(b0 + 2), ("cols",)], writes=[("ckvnT", c, t)])
            if "nokpe" in _A:
                continue
            Sc.op("dve", lambda e, pT=pT, tsl=tsl: e.tensor_copy(out=kpeT[:, tsl], in_=pT[:, 5, :]),
                  reads=[PS(b0 + 2)], writes=[("kpeT", t)])
        dump("cqnT", cqnT, [128, 3, S], BF16, ("cqnT",))
        dump("ckvnT", ckvnT, [128, 2, S], BF16, ("ckvnT",))
        A.release(mA)

        _stop = os.environ.get("MK_STOP", "")
        qn = A.alloc("qn", (S,), BF16)
        kn = A.alloc("kn", (S,), BF16)
        qpe = A.alloc("qpe", (S,), BF16)
        qr = [A.alloc("qr", (512,), F32) for _ in range(2)]
        pTs = [A.alloc("pTs", (512,), BF16) for _ in range(6)]
        accP = [A.alloc("accP", (512,), F32) for _ in range(4)]
        ones_f = A.alloc("ones_f", (128,), F32)
        maskb = A.alloc("maskb", (128,), BF16)
        zeros_bf = A.alloc("zeros_bf", (512,), BF16)
        Sc.op("pool", lambda e: e.memset(zeros_bf, 0.0), reads=[], writes=[("zeros_bf",)])
        Sc.op("dve", lambda e: e.tensor_scalar(out=maskb, in0=mask01, scalar1=-1.0, scalar2=30000.0, op0=ALU.add, op1=ALU.mult),
              reads=[("mask01",)], writes=[("maskb",)])
        Sc.op("dve", lambda e: e.memset(ones_f, 1.0), reads=[], writes=[("ones_f",)])
        wukv4 = wukv.rearrange("p c (h k) -> p c h k", h=8)
        cnt_st = {"s": 0, "o": 0}
        pend_all = []

        def build_wpad(hh):
            wq_ = wpad[hh % 2]
            for c in range(3):
                Sc.op("act", lambda e, c=c: e.activation(out=wq_[:, c, 0:64], in_=wuq4[:, c, hh, 128:192], func=AF.Copy),
                      reads=[("wuq",)], writes=[("wpad", hh % 2, c, 0)])
                Sc.op("act", lambda e, c=c: e.activation(out=wq_[:, c, 64:96], in_=wuq4[:, c, hh, 160:192], func=AF.Copy, scale=-1.0),
                      reads=[("wuq",)], writes=[("wpad", hh % 2, c, 1)])
                Sc.op("act", lambda e, c=c: e.activation(out=wq_[:, c, 96:128], in_=wuq4[:, c, hh, 128:160], func=AF.Copy),
                      reads=[("wuq",)], writes=[("wpad", hh % 2, c, 2)])
        for hp in range(0 if _stop in ("2A", "2pre") else 2):
            Sc.op("dve", lambda e: e.memset(v_aug[:, :, :, 128:130], 1.0), reads=[], writes=[("v_aug",)])
            for t in range(NT):
                pb = t % 2
                tsl = slice(t * 128, (t + 1) * 128)
                mm_group(bank(pb), [(ckvnT[:, c, tsl], wukv4[:, c, 4 * hp:4 * hp + 4, 128:256]) for c in range(2)],
                         reads=[("ckvnT",), ("wukv",)], writes=[PS(pb)])
                Sc.op("act", lambda e, t=t, pb=pb: e.activation(out=v_aug[:, t, :, 0:128], in_=bank(pb).rearrange("p (h k) -> p h k", h=4),
                                                                 func=AF.Copy),
                      reads=[PS(pb)], writes=[("v_aug", t)])
            for hl in range(0 if _stop == "2V" else 4):
                h = 4 * hp + hl
                for tg in range(4):
                    tsl = slice(tg * 512, (tg + 1) * 512)
                    pb = 7 * (tg % 2)
                    mm_group(bank(pb), [(wuq[:, c, h * 192:h * 192 + 128], cqnT[:, c, tsl]) for c in range(3)],
                             reads=[("wuq",), ("cqnT",)], writes=[PS(pb)])
                    Sc.op("act", lambda e, pb=pb, tsl=tsl: e.activation(out=qn[:, tsl], in_=bank(pb), func=AF.Copy, scale=SCALE),
                          reads=[PS(pb)], writes=[("qn", tg)])
                for tg in range(4):
                    tsl = slice(tg * 512, (tg + 1) * 512)
                    pb = 7 * (tg % 2)
                    mm_group(bank(pb), [(wukv[:, c, h * 256:h * 256 + 128], ckvnT[:, c, tsl]) for c in range(2)],
                             reads=[("wukv",), ("ckvnT",)], writes=[PS(pb)])
                    Sc.op("act", lambda e, pb=pb, tsl=tsl: e.activation(out=kn[:, tsl], in_=bank(pb), func=AF.Copy),
                          reads=[PS(pb)], writes=[("kn", tg)])
                wp_ = wpad[h % 2]
                if h == 0:
                    build_wpad(0)
                for tg in range(4):
                    tsl = slice(tg * 512, (tg + 1) * 512)
                    pb = 7 * (tg % 2)
                    mm_group(bank(pb), [(wp_[:, c, :], cqnT[:, c, tsl]) for c in range(3)],
                             reads=[("wpad", h % 2), ("cqnT",)], writes=[PS(pb)])
                    Sc.op("dve", lambda e, tsl=tsl, pb=pb: e.tensor_tensor(out=qpe[:, tsl], in0=bank(pb), in1=mcs[:, tsl], op=ALU.mult),
                          reads=[PS(pb), ("mcs",)], writes=[("qpe", tg)])
                if h + 1 < MLA_H:
                    build_wpad(h + 1)
                its = [(G, jb) for G in range(0 if _stop == "2P" else 4) for jb in range(4 * G + 4)]
                info = []
                for (G, jb) in its:
                    td = jb - 4 * G
                    q0 = max(td, 0) * 128
                    k3 = cnt_st["s"] % 6
                    sb_ = 1 + cnt_st["s"] % 3
                    cnt_st["s"] += 1
                    info.append(dict(G=G, jb=jb, td=td, q0=q0, N=512 - q0, k3=k3, sb=sb_))

                def emit_st(it):
                    ksl = slice(it["jb"] * 128, (it["jb"] + 1) * 128)
                    qsl = slice(it["G"] * 512 + it["q0"], (it["G"] + 1) * 512)
                    sbk, N_ = it["sb"], it["N"]
                    diag = it["td"] >= 0

                    def stf(e):
                        e.matmul(out=bank(sbk)[:, 0:N_], lhsT=kn[:, ksl], rhs=qn[:, qsl], start=True, stop=False)
                        ins = e.matmul(out=bank(sbk)[:, 0:N_], lhsT=kpeT[:, ksl], rhs=qpe[:, qsl], start=False, stop=not diag)
                        if diag:
                            ins = e.matmul(out=bank(sbk)[:, 0:128], lhsT=ident_bf, rhs=maskb, start=False, stop=True)
                        return ins
                    Sc.op("pe", stf, reads=[("kn",), ("qn",), ("kpeT",), ("qpe",), ("ident_bf",), ("maskb",)], writes=[PS(sbk)])

                def emit_exp(it):
                    k3, sb, N = it["k3"], it["sb"], it["N"]
                    Sc.op("act", lambda e: e.activation(out=pTs[k3][:, 0:N], in_=bank(sb)[:, 0:N], func=AF.Exp),
                          reads=[PS(sb)], writes=[("pTs", k3)])

                def emit_pv(it, hl=hl):
                    k3, N, G, jb, q0 = it["k3"], it["N"], it["G"], it["jb"], it["q0"]
                    ob = 4 + G % 2
                    which = jb % 2
                    ai = 2 * (G % 2) + which
                    ap_ = accP[ai]
                    en_ = "pool" if which == 0 else "dve"

                    def pv(e):
                        return e.matmul(out=bank(ob)[:, q0:512], lhsT=v_aug[:, jb, hl, 0:128], rhs=pTs[k3][:, 0:N],
                                        start=(jb == 0), stop=(jb == 4 * G + 3))
                    Sc.op("pe", pv, reads=[("pTs", k3), ("v_aug",)], writes=[PS(ob)])
                    if jb < 2:
                        if q0 > 0:
                            Sc.op(en_, lambda e: e.memset(ap_[:, 0:q0], 0.0), reads=[], writes=[("accP", ai)])
                        if en_ == "pool":
                            Sc.op(en_, lambda e: e.tensor_tensor(out=ap_[:, q0:512], in0=pTs[k3][:, 0:N], in1=zeros_bf[:, 0:N], op=ALU.add),
                                  reads=[("pTs", k3), ("zeros_bf",)], writes=[("accP", ai)])
                        else:
                            Sc.op(en_, lambda e: e.tensor_copy(out=ap_[:, q0:512], in_=pTs[k3][:, 0:N]), reads=[("pTs", k3)], writes=[("accP", ai)])
                    else:
                        Sc.op(en_, lambda e: e.tensor_tensor(out=ap_[:, q0:512], in0=ap_[:, q0:512], in1=pTs[k3][:, 0:N], op=ALU.add),
                              reads=[("pTs", k3), ("accP", ai)], writes=[("accP", ai)])

                def fin_steps(G, h=h):
                    ob = 4 + G % 2

                    def s0():
                        mm_group(bank(6), [(ones_f, accP[2 * (G % 2)]), (ones_f, accP[2 * (G % 2) + 1])],
                                 reads=[("ones_f",), ("accP", 2 * (G % 2)), ("accP", 2 * (G % 2) + 1)], writes=[PS(6)])

                    def piece(j):
                        def f():
                            cs_ = slice(j * 128, (j + 1) * 128)
                            Sc.op("dve", lambda e: e.reciprocal(out=qr[0][:, cs_], in_=bank(6)[:, cs_]), reads=[PS(6)], writes=[("qr", 0, j)])
                            Sc.op("dve", lambda e: e.tensor_tensor(out=attnT[:, h, G * 512 + j * 128:G * 512 + (j + 1) * 128],
                                                                   in0=bank(ob)[:, cs_], in1=qr[0][:, cs_], op=ALU.mult),
                                  reads=[PS(ob), ("qr", 0, j)], writes=[("attnT", h, G, j)])
                        return f
                    return [s0] + [piece(j) for j in range(4)]

                while pend_all:
                    pend_all.pop(0)()
                pend = pend_all
                for j in range(min(2, len(info))):
                    emit_st(info[j])
                for i, it in enumerate(info):
                    if i + 2 < len(info):
                        emit_st(info[i + 2])
                    emit_exp(it)
                    emit_pv(it)
                    if pend:
                        pend.pop(0)()
                    if it["jb"] == 4 * it["G"] + 3:
                        pend += [(lambda: None), (lambda: None)] + fin_steps(it["G"])
        while pend_all:
            pend_all.pop(0)()
        dump("attnT", attnT, [128, 8, S], BF16, ("attnT",))
        A.release(m2)


        dump("gT2", gatedT, [128, 8, S], BF16, ("gatedT",))
        dump("aT2", attnT, [128, 8, S], BF16, ("attnT",))
        dump("uT2", uT, [128, 8, S], BF16, ("uT",))
        m3 = A.mark()
        mergedT = A.alloc("mergedT", (8, S), BF16)
        wout = A.alloc("wout", (8, D), BF16)
        Sc.dma("pool", wout, w_out.rearrange("(c p) n -> p c n", p=128), writes=[("wout",)])
        m3a = A.mark()
        wm = [[A.alloc("wm", (8, 128), BF16) for _ in range(4)] for _ in range(2)]
        sgA = [A.alloc("sgA", (512,), BF16) for _ in range(2)]
        sgB = [A.alloc("sgB", (512,), BF16) for _ in range(2)]
        mt1 = [A.alloc("mt1", (512,), F32) for _ in range(2)]
        mt2 = [A.alloc("mt2", (512,), F32) for _ in range(2)]
        w_ro_v = w_ret_o.rearrange("(c p) n -> p c n", p=128)
        w_mo_v = w_mla_o.rearrange("(c p) n -> p c n", p=128)

        def load_wm(fc):
            ws = fc % 2
            srcs = [w_ro_v[:, :, fc * 128:(fc + 1) * 128], w_mo_v[:, :, fc * 128:(fc + 1) * 128],
                    w_in_v[:, :, 4800 + fc * 128:4800 + (fc + 1) * 128], w_in_v[:, :, 5824 + fc * 128:5824 + (fc + 1) * 128]]
            for i in range(4):
                Sc.dma("pool", wm[ws][i], srcs[i], writes=[("wm", ws, i)])

        load_wm(0)
        for fc in range(8):
            ws = fc % 2
            if fc + 1 < 8:
                load_wm(fc + 1)
            for tg in range(4):
                it = fc * 4 + tg
                p = it % 2
                bs = p * 4
                tsl = slice(tg * 512, (tg + 1) * 512)
                srcT = [(gatedT, ("gatedT",)), (attnT, ("attnT",)), (uT, ("uT",)), (uT, ("uT",))]
                for i in range(4):
                    mm_group(bank(bs + i), [(wm[ws][i][:, c, :], srcT[i][0][:, c, tsl]) for c in range(8)],
                             reads=[("wm", ws, i), srcT[i][1]], writes=[PS(bs + i)])
                Sc.op("act", lambda e, p=p, bs=bs: e.activation(out=sgA[p], in_=bank(bs + 2), func=AF.Sigmoid),
                      reads=[PS(bs + 2)], writes=[("sgA", p)])
                Sc.op("act", lambda e, p=p, bs=bs: e.activation(out=sgB[p], in_=bank(bs + 3), func=AF.Sigmoid),
                      reads=[PS(bs + 3)], writes=[("sgB", p)])
                Sc.op("dve", lambda e, p=p, bs=bs: e.tensor_tensor(out=mt1[p], in0=bank(bs), in1=sgA[p], op=ALU.mult),
                      reads=[PS(bs), ("sgA", p)], writes=[("mt1", p)])
                Sc.op("dve", lambda e, p=p, bs=bs: e.tensor_tensor(out=mt2[p], in0=bank(bs + 1), in1=sgB[p], op=ALU.mult),
                      reads=[PS(bs + 1), ("sgB", p)], writes=[("mt2", p)])
                Sc.op("dve", lambda e, p=p, fc=fc, tsl=tsl: e.tensor_tensor(out=mergedT[:, fc, tsl], in0=mt1[p], in1=mt2[p], op=ALU.add),
                      reads=[("mt1", p), ("mt2", p)], writes=[("mergedT", fc, tg)])
        dump("mergedT", mergedT, [128, 8, S], BF16, ("mergedT",))
        dump("sgA1", sgA[1], [128, 512], BF16, ("sgA", 1))
        dump("mt11", mt1[1], [128, 512], F32, ("mt1", 1))
        dump("mt21", mt2[1], [128, 512], F32, ("mt2", 1))
        dump("wm1", wm[1][0], [128, 8, 128], BF16, ("wm", 1, 0))
        dump("gT3", gatedT, [128, 8, S], BF16, ("gatedT",))
        dump("aT3", attnT, [128, 8, S], BF16, ("attnT",))
        dump("uT3", uT, [128, 8, S], BF16, ("uT",))
        A.release(m3a)

        if os.environ.get("MK_STOP", "") == "3a":
            raise_stop = True
        else:
            raise_stop = False
        offG = A.offs["gatedT"]
        assert A.offs["attnT"] == offG + 32768
        acc = arena_t[:, offG // 4:offG // 4 + NT * D].rearrange("p (t d) -> p t d", t=NT)
        Sc.alias("acc", "gatedT")
        Sc.alias("acc", "attnT")
        xt = [A.alloc("xt", (D,), F32) for _ in range(3)]
        xn = [A.alloc("xn", (D,), BF16) for _ in range(3)]
        junk = A.alloc("junk", (D,), BF16)
        mtmp = [A.alloc("mtmp", (D,), F32) for _ in range(3)]
        def pre3b(t):
            p = t % 3
            tsl = slice(t * 128, (t + 1) * 128)
            Sc.dma("sp", xt[p], x[t * 128:(t + 1) * 128, :], writes=[("xt", p)])
            for half in range(2):
                hb = (t % 2) * 2 + half
                hs = slice(half * 512, (half + 1) * 512)
                mm_group(bank(hb), [(mergedT[:, c, tsl], wout[:, c, hs]) for c in range(8)],
                         reads=[("mergedT",), ("wout",)], writes=[PS(hb)])
                Sc.op("dve", lambda e, p=p, hb=hb, hs=hs: e.tensor_tensor(out=mtmp[p][:, hs], in0=bank(hb), in1=g1b[:, hs], op=ALU.mult),
                      reads=[PS(hb), ("g1b",)], writes=[("mtmp", p, half)])
                Sc.op("dve", lambda e, p=p, t=t, hs=hs: e.tensor_tensor(out=acc[:, t, hs], in0=mtmp[p][:, hs], in1=xt[p][:, hs], op=ALU.add),
                      reads=[("mtmp", p, half), ("xt", p)], writes=[("acc", t, half)])

        if not raise_stop:
            for t0_ in range(2):
                pre3b(t0_)
                norm_A(t0_, acc[:, t0_, :], ("acc", t0_))
            for t in range(NT):
                if t + 2 < NT:
                    pre3b(t + 2)
                    norm_A(t + 2, acc[:, t + 2, :], ("acc", t + 2))
                norm_B(t, A2, modc[:, 24:32], ("A2",), ("modc", 1), uT, "uT", pb=4 + t % 2)
        dump("h1", acc, [128, NT, D], F32, ("acc",))
        dump("u2T", uT, [128, 8, S], BF16, ("uT",))
        A.release(m3)

        m4 = A.mark()
        wr = A.alloc("wr", (8, 36), BF16)
        brt = A.alloc("brt", (36,), F32)
        gate = A.alloc("gate", (NT, 32), F32)
        hid = [A.alloc("hid", (2, S), BF16) for _ in range(2)]
        sa = [A.alloc("sa", (512,), BF16) for _ in range(2)]
        NWS = 3
        w1e = [A.alloc("w1e", (8, 256), BF16) for _ in range(NWS)]
        w3e = [A.alloc("w3e", (8, 256), BF16) for _ in range(NWS)]
        w2e = [A.alloc("w2e", (2, D), BF16) for _ in range(NWS)]
        w2s = [A.alloc("w2s", (2, D), F32) for _ in range(2)]
        junk = A.alloc("junk", (D,), BF16)
        fnb = A.alloc("fnb", (D,), F32)
        Sc.dma("sp", fnb, fnorm[0, :].partition_broadcast(128), writes=[("fnb",)])
        Sc.dma("pool", wr[:, :, 0:4], w_grp.rearrange("(c p) n -> p c n", p=128), writes=[("wr", 0)])
        Sc.dma("pool", wr[:, :, 4:36], w_exp.rearrange("(c p) n -> p c n", p=128), writes=[("wr", 1)])
        Sc.dma("sp", brt, b_rt[0, :].partition_broadcast(128), writes=[("brt",)])

        def load_expert(e):
            sl = e % NWS
            Sc.dma("pool", w1e[sl], w1[e].rearrange("(c p) f -> p c f", p=128), writes=[("w1e", sl)])
            Sc.dma("pool", w3e[sl], w3[e].rearrange("(c p) f -> p c f", p=128), writes=[("w3e", sl)])
            Sc.dma("sp", w2s[e % 2], w2[e].rearrange("(c p) d -> p c d", p=128), writes=[("w2s", e % 2)])
            for c in range(2):
                Sc.op("pool", lambda en, c=c, sl=sl, e=e: en.tensor_tensor(out=w2e[sl][:, c, :], in0=w2s[e % 2][:, c, :], in1=g2b, op=ALU.mult),
                      reads=[("w2s", e % 2), ("g2b",)], writes=[("w2e", sl, c)])

        load_expert(0)
        X = mybir.AxisListType.X
        LG = A.alloc("LG", (NT, 36), F32)
        r16 = A.alloc("r16", (12, NT), F32)
        r4a = A.alloc("r4a", (NT, 4), F32)
        r4b = A.alloc("r4b", (NT, 4), F32)
        rml = A.alloc("rml", (NT, 32), F32)
        rml2 = A.alloc("rml2", (NT, 32), F32)
        rm1 = A.alloc("rm1", (NT, 32), F32)
        rm2 = A.alloc("rm2", (NT, 32), F32)
        for t in range(0 if raise_stop else NT):
            p = t % 2
            tsl = slice(t * 128, (t + 1) * 128)
            lg = bank(p)[:, 0:36]
            mm_group(lg, [(uT[:, c, tsl], wr[:, c, :]) for c in range(8)], reads=[("uT",), ("wr",)], writes=[PS(p)])
            Sc.op("dve", lambda e, t=t, lg=lg: e.tensor_tensor(out=LG[:, t, :], in0=lg, in1=brt, op=ALU.add),
                  reads=[PS(p), ("brt",)], writes=[("LG", t)])
        if not raise_stop:
            Lg_ = LG[:, :, 0:4]
            Le_ = LG[:, :, 4:36].rearrange("p t (g e) -> p t g e", g=4)

            def b3(row, n):
                return r16[:, row, :].unsqueeze(2).to_broadcast([128, NT, n])

            def dv(fn, reads, writes):
                Sc.op("dve", fn, reads=reads, writes=writes)
            dv(lambda e: e.tensor_reduce(out=r16[:, 0, :], in_=Lg_, axis=X, op=ALU.max), [("LG",)], [("r16", 0)])
            dv(lambda e: e.tensor_tensor(out=r4a, in0=Lg_, in1=b3(0, 4), op=ALU.is_equal), [("LG",), ("r16", 0)], [("r4a",)])
            dv(lambda e: e.tensor_tensor(out=r4b, in0=Lg_, in1=b3(0, 4), op=ALU.subtract), [("LG",), ("r16", 0)], [("r4b",)])
            Sc.op("act", lambda e: e.activation(out=r4b, in_=r4b, func=AF.Exp), reads=[("r4b",)], writes=[("r4b",)])
            dv(lambda e: e.tensor_reduce(out=r16[:, 1, :], in_=r4b, axis=X, op=ALU.add), [("r4b",)], [("r16", 1)])
            dv(lambda e: e.reciprocal(out=r16[:, 2, :], in_=r16[:, 1, :]), [("r16", 1)], [("r16", 2)])
            dv(lambda e: e.tensor_scalar(out=r4a, in0=r4a, scalar1=-1.0, scalar2=1.0e30, op0=ALU.add, op1=ALU.mult), [("r4a",)], [("r4a",)])
            dv(lambda e: e.tensor_tensor(out=rml.rearrange("p t (g e) -> p t g e", g=4), in0=Le_,
                                         in1=r4a.unsqueeze(3).to_broadcast([128, NT, 4, 8]), op=ALU.add), [("LG",), ("r4a",)], [("rml",)])
            dv(lambda e: e.tensor_reduce(out=r16[:, 3, :], in_=rml, axis=X, op=ALU.max), [("rml",)], [("r16", 3)])
            dv(lambda e: e.tensor_tensor(out=rm1, in0=rml, in1=b3(3, 32), op=ALU.is_equal), [("rml",), ("r16", 3)], [("rm1",)])
            dv(lambda e: e.scalar_tensor_tensor(out=rml2, in0=rm1, scalar=-1.0e30, in1=rml, op0=ALU.mult, op1=ALU.add),
               [("rm1",), ("rml",)], [("rml2",)])
            dv(lambda e: e.tensor_reduce(out=r16[:, 4, :], in_=rml2, axis=X, op=ALU.max), [("rml2",)], [("r16", 4)])
            dv(lambda e: e.tensor_tensor(out=rm2, in0=rml2, in1=b3(4, 32), op=ALU.is_equal), [("rml2",), ("r16", 4)], [("rm2",)])
            dv(lambda e: e.tensor_tensor(out=r16[:, 5, :], in0=r16[:, 4, :], in1=r16[:, 3, :], op=ALU.subtract), [("r16", 3), ("r16", 4)], [("r16", 5)])
            Sc.op("act", lambda e: e.activation(out=r16[:, 6, :], in_=r16[:, 5, :], func=AF.Exp), reads=[("r16", 5)], writes=[("r16", 6)])
            dv(lambda e: e.tensor_scalar(out=r16[:, 7, :], in0=r16[:, 6, :], scalar1=1.0, scalar2=None, op0=ALU.add), [("r16", 6)], [("r16", 7)])
            dv(lambda e: e.reciprocal(out=r16[:, 8, :], in_=r16[:, 7, :]), [("r16", 7)], [("r16", 8)])
            dv(lambda e: e.tensor_tensor(out=r16[:, 9, :], in0=r16[:, 8, :], in1=r16[:, 2, :], op=ALU.mult), [("r16", 8), ("r16", 2)], [("r16", 9)])
            dv(lambda e: e.tensor_tensor(out=r16[:, 10, :], in0=r16[:, 9, :], in1=r16[:, 6, :], op=ALU.mult), [("r16", 9), ("r16", 6)], [("r16", 10)])
            dv(lambda e: e.tensor_tensor(out=rm1, in0=rm1, in1=b3(9, 32), op=ALU.mult), [("rm1",), ("r16", 9)], [("rm1",)])
            dv(lambda e: e.tensor_tensor(out=rm2, in0=rm2, in1=b3(10, 32), op=ALU.mult), [("rm2",), ("r16", 10)], [("rm2",)])
            dv(lambda e: e.tensor_tensor(out=gate, in0=rm1, in1=rm2, op=ALU.add), [("rm1",), ("rm2",)], [("gate",)])
        dump("gate", gate, [128, NT, 32], F32, ("gate",))

        n_exp = 0 if raise_stop else int(os.environ.get("MK_NEXP", N_EXP))

        def up_steps(ex):
            sl = ex % NWS
            hp_ = ex % 2
            steps = []
            for fch in range(2):
                for tg in range(4):
                    def f(fch=fch, tg=tg):
                        fs = slice(fch * 128, (fch + 1) * 128)
                        pp = (fch * 4 + tg) % 2
                        tsl = slice(tg * 512, (tg + 1) * 512)
                        mm_group(bank(pp), [(w1e[sl][:, c, fs], uT[:, c, tsl]) for c in range(8)],
                                 reads=[("w1e", sl), ("uT",)], writes=[PS(pp)])
                        mm_group(bank(2 + pp), [(w3e[sl][:, c, fs], uT[:, c, tsl]) for c in range(8)],
                                 reads=[("w3e", sl), ("uT",)], writes=[PS(2 + pp)])
                        Sc.op("act", lambda e: e.activation(out=sa[pp], in_=bank(pp), func=AF.Silu), reads=[PS(pp)], writes=[("sa", pp)])
                        Sc.op("dve", lambda e: e.tensor_tensor(out=hid[hp_][:, fch, tsl], in0=bank(2 + pp), in1=sa[pp], op=ALU.mult),
                              reads=[PS(2 + pp), ("sa", pp)], writes=[("hid", hp_, fch, tg)])
                    steps.append(f)
            return steps

        def down_steps(ex):
            sl = ex % NWS
            hp_ = ex % 2
            steps = []
            for t in range(NT):
                def f(t=t):
                    ob = 4 + (t % 2) * 2
                    tsl = slice(t * 128, (t + 1) * 128)
                    for half in range(2):
                        mm_group(bank(ob + half), [(hid[hp_][:, fch, tsl], w2e[sl][:, fch, half * 512:(half + 1) * 512]) for fch in range(2)],
                                 reads=[("hid", hp_, 0, t // 4), ("hid", hp_, 1, t // 4), ("w2e", sl)], writes=[PS(ob + half)])
                    Sc.op("dve", lambda e: e.scalar_tensor_tensor(
                        out=acc[:, t, :].rearrange("p (a b) -> p a b", a=2), in0=psum_t[:, ob:ob + 2, :], scalar=gate[:, t, ex:ex + 1],
                        in1=acc[:, t, :].rearrange("p (a b) -> p a b", a=2), op0=ALU.mult, op1=ALU.add),
                        reads=[PS(ob), PS(ob + 1), ("gate", t), ("acc", t)], writes=[("acc", t)])
                steps.append(f)
            return steps

        if n_exp > 0:
            if n_exp > 1:
                load_expert(1)
            for st_ in up_steps(0):
                st_()
        for ex in range(n_exp):
            if ex + 2 < n_exp:
                load_expert(ex + 2)
            dn = down_steps(ex)
            up = up_steps(ex + 1) if ex + 1 < n_exp else []
            for i, d_ in enumerate(dn):
                d_()
                if i % 2 == 1 and (i // 2) < len(up):
                    up[i // 2]()
        dump("h2", acc, [128, NT, D], F32, ("acc",))

        for t in range(0 if raise_stop else NT):
            Sc.op("act", lambda e, t=t: e.activation(out=junk, in_=acc[:, t, :], func=AF.Square, accum_out=stat[:, t:t + 1]),
                  reads=[("acc", t)], writes=[("junk",), ("stat", t)])
        if not raise_stop:
            Sc.op("act", lambda e: e.activation(out=stat[:, 16:32], in_=stat[:, 0:16], func=AF.Sqrt, scale=1.0 / D, bias=EPS),
                  reads=[("stat",)], writes=[("stat", "sq")])
            Sc.op("dve", lambda e: e.reciprocal(out=stat[:, 32:48], in_=stat[:, 16:32]), reads=[("stat", "sq")], writes=[("stat", "rs")])
        for t in range(0 if raise_stop else NT):
            p = t % 2
            Sc.op("dve", lambda e, t=t, p=p: e.scalar_tensor_tensor(out=w2s[p][:, 0, :], in0=acc[:, t, :], scalar=stat[:, 32 + t:33 + t], in1=fnb,
                                                                   op0=ALU.mult, op1=ALU.mult),
                  reads=[("acc", t), ("stat", "rs"), ("fnb",)], writes=[("w2s", p)])
            Sc.dma("sp", out_d[t * 128:(t + 1) * 128, :], w2s[p][:, 0, :], reads=[("w2s", p)], writes=[("out", t)], is_output=True)
        A.release(m4)

        Sc.finish()
        block = es.enter_context(nc.Block())
        Sc.emit(block)
        print(f"[build] instructions={Sc.nins} arena_peak={A.peak}")
    return nc, dbg_outs


_CACHE = {}


def _consts():
    idx = np.arange(128, dtype=np.float64)
    ident = np.eye(128, dtype=np.float32)
    mask = (idx[None, :] >= idx[:, None]).astype(np.float32)
    pos = np.arange(S, dtype=np.float32)
    inv_r = (np.float32(10000.0) ** (-np.arange(0, 256, 2, dtype=np.float32) / np.float32(256))).astype(np.float32)
    ang_r = pos[:, None] * inv_r[None, :]
    rcos = np.cos(ang_r).T.astype(np.float32).copy()
    rsin = np.sin(ang_r).T.astype(np.float32).copy()
    inv_m = (np.float32(10000.0) ** (-np.arange(0, 64, 2, dtype=np.float32) / np.float32(64))).astype(np.float32)
    ang_m = pos[:, None] * inv_m[None, :]
    mcos = np.cos(ang_m).astype(np.float32)
    msin = np.sin(ang_m).astype(np.float32)
    scale = np.float32(192.0 ** -0.5)
    mcos64 = (np.concatenate([mcos, mcos], axis=1).T * scale).astype(np.float32).copy()
    msin64 = (np.concatenate([msin, msin], axis=1).T * scale).astype(np.float32).copy()
    mcos_tok = mcos.reshape(NT, 128, 32).transpose(1, 0, 2).copy()
    msin_tok = msin.reshape(NT, 128, 32).transpose(1, 0, 2).copy()
    lg = np.log1p(-np.exp2(-5.0 - np.arange(RET_H, dtype=np.float64)))
    zp = np.exp(-lg[None, :] * (idx[:, None] + 1.0)) * (256.0 ** -0.5)
    eox2 = EPS * np.exp(-2.0 * lg[None, :] * (idx[:, None] + 1.0))
    cret = np.concatenate([zp, eox2], axis=1).astype(np.float32)
    return dict(c_ident=ident, c_mask=mask, c_rcos=rcos, c_rsin=rsin, c_mcos64=mcos64, c_msin64=msin64,
                c_mcos_tok=mcos_tok, c_msin_tok=msin_tok, c_ret=cret)


def make_in_maps(inputs):
    f = lambda a: np.ascontiguousarray(np.asarray(a, dtype=np.float32))
    g = {k: f(v) for k, v in inputs.items()}
    shared = dict(
        b_ada=g["b_ada"].reshape(1, 6 * D), w_ada=g["w_ada"][0], w_in=g["w_in"][0], w_ret_o=g["w_ret_o"][0],
        w_uq=g["w_uq"][0], w_ukv=g["w_ukv"][0], w_mla_o=g["w_mla_o"][0], w_out=g["w_out"][0],
        w_grp=g["w_grp"][0], w_exp=g["w_exp"][0],
        b_rt=np.concatenate([g["b_grp"][0], g["b_exp"][0]]).reshape(1, 36),
        w1=g["w1"][0].reshape(N_EXP, D, 256), w3=g["w3"][0].reshape(N_EXP, D, 256), w2=g["w2"][0].reshape(N_EXP, 256, D),
        final_norm=g["final_norm"].reshape(1, D),
    )
    shared.update(_consts())
    maps = []
    for b in range(NCORES):
        rows = np.concatenate([g["b_ada"].reshape(48, 128), g["norm1"].reshape(8, 128), g["norm2"].reshape(8, 128),
                               g["q_norm"].reshape(3, 128), g["kv_norm"].reshape(2, 128), g["c"][b].reshape(8, 128)], axis=0)
        m = dict(shared)
        m["x"] = g["x"][b]
        m["rows"] = np.ascontiguousarray(rows)
        maps.append(m)
    return maps


def kernel(**inputs):
    if "nc" not in _CACHE:
        _CACHE["nc"] = build_program()[0]
    nc = _CACHE["nc"]
    maps = make_in_maps(inputs)
    res = run_bass_kernel_spmd(nc, maps, core_ids=list(range(NCORES)))
    return np.stack([np.asarray(r["out"], dtype=np.float32) for r in res.results], axis=0)
```
